# Optimizing a Trainium2 kernel written in Bass

```python
import jax, jax.numpy as jnp
from jax import lax
import numpy as np

D_MODEL = 2048
BATCH = 8
SEQ = 2048
DEPTH = 1

M_HEADS = 4
M_HEAD_DIM = 256
M_WIDTH = M_HEADS * M_HEAD_DIM
M_CHUNK = 64
CONV_WIDTH = 4
A_HEADS = 16
A_KV_HEADS = 4
A_GROUP = A_HEADS // A_KV_HEADS
A_HEAD_DIM = 64
A_WIDTH = A_HEADS * A_HEAD_DIM
A_KV_WIDTH = A_KV_HEADS * A_HEAD_DIM
WINDOW = 128
ROPE_THETA = 10000.0
N_BRANCHES = 2
D_IN = 4 * M_WIDTH + 2 * M_HEADS + A_WIDTH + 2 * A_KV_WIDTH + N_BRANCHES * D_MODEL
N_GROUPS = 8
EXPERTS_PER_GROUP = 8
N_EXPERTS = N_GROUPS * EXPERTS_PER_GROUP
TOP_K = 2
D_EXPERT = 768
MOE_BLOCK = 128
EPS = 1e-6

kernel_name = "hybrid_mlstm_swa_hmoe_layer"


def rms_norm(x, g):
    xf = x.astype(jnp.float32)
    y = xf * lax.rsqrt(jnp.mean(xf * xf, axis=-1, keepdims=True) + EPS)
    return (y * g.astype(jnp.float32)).astype(x.dtype)


def rope(x, pos):
    half = x.shape[-1] // 2
    freqs = ROPE_THETA ** (-jnp.arange(half, dtype=jnp.float32) / half)
    ang = pos.astype(jnp.float32)[:, None] * freqs[None, :]
    cos = jnp.cos(ang)[None, :, None, :]
    sin = jnp.sin(ang)[None, :, None, :]
    xf = x.astype(jnp.float32)
    x1, x2 = xf[..., :half], xf[..., half:]
    return jnp.concatenate([x1 * cos - x2 * sin, x2 * cos + x1 * sin], axis=-1).astype(x.dtype)


def causal_dwconv(x, w):
    K = w.shape[0]
    S = x.shape[1]
    xp = jnp.pad(x, ((0, 0), (K - 1, 0), (0, 0)))
    y = xp[:, 0:S, :] * w[0]
    for j in range(1, K):
        y = y + xp[:, j:j + S, :] * w[j]
    return y


def mlstm_chunkwise(q, k, v, li, lf):
    B, H, S, dk = q.shape
    dv = v.shape[-1]
    L = M_CHUNK
    NC = S // L

    def to_chunks(t):
        t = t.reshape(B, H, NC, L, *t.shape[3:])
        return jnp.moveaxis(t, 2, 0)

    causal = jnp.tril(jnp.ones((L, L), dtype=bool))

    def step(carry, xs):
        C, n, m = carry
        qc, kc, vc, lic, lfc = xs
        b = jnp.cumsum(lfc, axis=-1)
        a = b + m[..., None]
        D = jnp.where(causal, b[..., :, None] - b[..., None, :] + lic[..., None, :], -jnp.inf)
        m_t = jnp.maximum(a, jnp.max(D, axis=-1))
        w_inter = jnp.exp(a - m_t)
        P = jnp.einsum('bhtd,bhsd->bhts', qc, kc) * jnp.exp(D - m_t[..., None])
        num = w_inter[..., None] * jnp.einsum('bhtd,bhde->bhte', qc, C) + jnp.einsum('bhts,bhse->bhte', P, vc)
        qn = w_inter * jnp.einsum('bhtd,bhd->bht', qc, n) + jnp.sum(P, axis=-1)
        den = jnp.maximum(jnp.abs(qn), jnp.exp(-m_t))
        h = num / den[..., None]
        bL = b[..., -1]
        g = bL[..., None] - b + lic
        m_new = jnp.maximum(bL + m, jnp.max(g, axis=-1))
        decay = jnp.exp(bL + m - m_new)
        wk = jnp.exp(g - m_new[..., None])
        C_new = decay[..., None, None] * C + jnp.einsum('bhs,bhsd,bhse->bhde', wk, kc, vc)
        n_new = decay[..., None] * n + jnp.einsum('bhs,bhsd->bhd', wk, kc)
        return (C_new, n_new, m_new), h

    init = (jnp.zeros((B, H, dk, dv), jnp.float32),
            jnp.zeros((B, H, dk), jnp.float32),
            jnp.zeros((B, H), jnp.float32))
    xs = (to_chunks(q), to_chunks(k), to_chunks(v), to_chunks(li), to_chunks(lf))
    _, h = lax.scan(step, init, xs)
    return jnp.moveaxis(h, 0, 2).reshape(B, H, S, dv)


def sliding_window_attention(q, k, v, sinks):
    B, S, _, dh = q.shape
    Bk = WINDOW
    NB = S // Bk
    qb = q.reshape(B, NB, Bk, A_KV_HEADS, A_GROUP, dh)

    def band(t):
        tb = t.reshape(B, NB, Bk, A_KV_HEADS, dh)
        prev = jnp.pad(tb, ((0, 0), (1, 0), (0, 0), (0, 0), (0, 0)))[:, :NB]
        return jnp.concatenate([prev, tb], axis=2)

    kb, vb = band(k), band(v)
    s = jnp.einsum('bnqhgd,bnkhd->bnhgqk', qb, kb).astype(jnp.float32) * (dh ** -0.5)
    qpos = jnp.arange(Bk)[:, None] + Bk
    kpos = jnp.arange(2 * Bk)[None, :]
    rel = qpos - kpos
    allowed = (rel >= 0) & (rel < WINDOW)
    key_abs = (jnp.arange(NB) * Bk - Bk)[:, None] + kpos
    mask = allowed[None] & (key_abs >= 0)[:, None, :]
    s = jnp.where(mask[None, :, None, None, :, :], s, -jnp.inf)
    sink = sinks.astype(jnp.float32).reshape(A_KV_HEADS, A_GROUP)[None, None, :, :, None]
    mx = jnp.maximum(jnp.max(s, axis=-1), sink)
    p = jnp.exp(s - mx[..., None])
    den = jnp.sum(p, axis=-1) + jnp.exp(sink - mx)
    p = (p / den[..., None]).astype(v.dtype)
    o = jnp.einsum('bnhgqk,bnkhd->bnqhgd', p, vb)
    return o.reshape(B, S, A_HEADS, dh)


def hierarchical_moe(x, w_group, b_group, w_expert, b_expert, w_gate, w_up, w_down):
    T, D = x.shape
    g_logits = jnp.matmul(x, w_group).astype(jnp.float32) + b_group.astype(jnp.float32)
    g_prob = jax.nn.softmax(g_logits, axis=-1)
    g_p, g_idx = lax.top_k(g_prob, 1)
    e_logits = (jnp.matmul(x, w_expert).astype(jnp.float32) + b_expert.astype(jnp.float32))
    e_logits = e_logits.reshape(T, N_GROUPS, EXPERTS_PER_GROUP)[jnp.arange(T), g_idx[:, 0]]
    e_prob = jax.nn.softmax(e_logits, axis=-1)
    e_p, e_loc = lax.top_k(e_prob, TOP_K)
    e_p = e_p / jnp.sum(e_p, axis=-1, keepdims=True)
    gate = g_p * e_p
    eid = g_idx * EXPERTS_PER_GROUP + e_loc

    M = T * TOP_K
    eid_f = eid.reshape(M).astype(jnp.int32)
    tok_f = jnp.repeat(jnp.arange(T, dtype=jnp.int32), TOP_K)
    w_f = gate.reshape(M)
    order = jnp.argsort(eid_f)
    e_s, tok_s, w_s = eid_f[order], tok_f[order], w_f[order]
    counts = jnp.bincount(eid_f, length=N_EXPERTS).astype(jnp.int32)
    padded = (counts + MOE_BLOCK - 1) // MOE_BLOCK * MOE_BLOCK
    start = jnp.cumsum(counts) - counts
    pend = jnp.cumsum(padded)
    pstart = pend - padded
    dest = pstart[e_s] + jnp.arange(M, dtype=jnp.int32) - start[e_s]
    n_blocks = -(-M // MOE_BLOCK) + N_EXPERTS
    P = n_blocks * MOE_BLOCK
    row_tok = jnp.zeros((P,), jnp.int32).at[dest].set(tok_s)
    row_w = jnp.zeros((P,), jnp.float32).at[dest].set(w_s)
    blk_e = jnp.minimum(jnp.searchsorted(pend, jnp.arange(n_blocks, dtype=jnp.int32) * MOE_BLOCK, side='right'),
                        N_EXPERTS - 1)
    xr = x[row_tok].reshape(n_blocks, MOE_BLOCK, D)

    def expert_block(args):
        xb, e = args
        h = jax.nn.silu(jnp.matmul(xb, w_gate[e])) * jnp.matmul(xb, w_up[e])
        return jnp.matmul(h, w_down[e])

    yr = lax.map(expert_block, (xr, blk_e)).reshape(P, D)
    y = jax.ops.segment_sum(yr.astype(jnp.float32) * row_w[:, None], row_tok, num_segments=T)
    return y.astype(x.dtype)


def setup_inputs(seed: int = 0) -> dict:
    key = jax.random.key(seed)
    ks = jax.random.split(key, 24)
    f32 = jnp.float32
    nrm = lambda k, shape, scale: jax.random.normal(k, shape, f32) * scale
    return {
        "x": nrm(ks[0], (BATCH, SEQ, D_MODEL), 1.0),
        "g_mix": 1.0 + nrm(ks[1], (DEPTH, D_MODEL), 0.02),
        "w_in": nrm(ks[2], (DEPTH, D_MODEL, D_IN), D_MODEL ** -0.5),
        "conv_qk": nrm(ks[3], (DEPTH, CONV_WIDTH, 2 * M_WIDTH), CONV_WIDTH ** -0.5),
        "b_igate": nrm(ks[4], (DEPTH, M_HEADS), 0.1),
        "b_fgate": jnp.linspace(3.0, 6.0, M_HEADS, dtype=f32)[None, :] + nrm(ks[5], (DEPTH, M_HEADS), 0.1),
        "g_mlstm": 1.0 + nrm(ks[6], (DEPTH, M_HEADS, M_HEAD_DIM), 0.02),
        "g_q": 1.0 + nrm(ks[7], (DEPTH, A_HEAD_DIM), 0.02),
        "g_k": 1.0 + nrm(ks[8], (DEPTH, A_HEAD_DIM), 0.02),
        "sinks": nrm(ks[9], (DEPTH, A_HEADS), 0.5),
        "w_proj_m": nrm(ks[10], (DEPTH, M_WIDTH, D_MODEL), M_WIDTH ** -0.5),
        "w_proj_a": nrm(ks[11], (DEPTH, A_WIDTH, D_MODEL), A_WIDTH ** -0.5),
        "w_out": nrm(ks[12], (DEPTH, D_MODEL, D_MODEL), D_MODEL ** -0.5),
        "g_ffn": 1.0 + nrm(ks[13], (DEPTH, D_MODEL), 0.02),
        "w_group": nrm(ks[14], (DEPTH, D_MODEL, N_GROUPS), D_MODEL ** -0.5),
        "b_group": nrm(ks[15], (DEPTH, N_GROUPS), 0.01),
        "w_expert": nrm(ks[16], (DEPTH, D_MODEL, N_EXPERTS), D_MODEL ** -0.5),
        "b_expert": nrm(ks[17], (DEPTH, N_EXPERTS), 0.01),
        "w_gate": nrm(ks[18], (DEPTH, N_EXPERTS, D_MODEL, D_EXPERT), D_MODEL ** -0.5),
        "w_up": nrm(ks[19], (DEPTH, N_EXPERTS, D_MODEL, D_EXPERT), D_MODEL ** -0.5),
        "w_down": nrm(ks[20], (DEPTH, N_EXPERTS, D_EXPERT, D_MODEL), D_EXPERT ** -0.5),
    }


def reference(x, g_mix, w_in, conv_qk, b_igate, b_fgate, g_mlstm, g_q, g_k, sinks,
              w_proj_m, w_proj_a, w_out, g_ffn, w_group, b_group, w_expert, b_expert,
              w_gate, w_up, w_down):
    B, S, D = x.shape
    f32 = jnp.float32
    sizes = (M_WIDTH, M_WIDTH, M_WIDTH, M_WIDTH, M_HEADS, M_HEADS,
             A_WIDTH, A_KV_WIDTH, A_KV_WIDTH, D_MODEL, D_MODEL)
    cuts = [int(c) for c in np.cumsum(sizes)[:-1]]
    pos = jnp.arange(S, dtype=jnp.int32)
    for l in range(DEPTH):
        h = rms_norm(x, g_mix[l])
        z = jnp.matmul(h, w_in[l])
        mq, mk, mv, mo, mi, mf, aq, ak, av, gm, ga = jnp.split(z, cuts, axis=-1)

        qk = jax.nn.silu(causal_dwconv(jnp.concatenate([mq, mk], axis=-1), conv_qk[l]))
        mq, mk = jnp.split(qk, 2, axis=-1)
        heads = lambda t: t.reshape(B, S, M_HEADS, M_HEAD_DIM).transpose(0, 2, 1, 3).astype(f32)
        li = (mi + b_igate[l]).astype(f32).transpose(0, 2, 1)
        lf = jax.nn.log_sigmoid((mf + b_fgate[l]).astype(f32)).transpose(0, 2, 1)
        hm = mlstm_chunkwise(heads(mq), heads(mk) * (M_HEAD_DIM ** -0.5), heads(mv), li, lf)
        hm = rms_norm(hm.transpose(0, 2, 1, 3), g_mlstm[l])
        hm = hm.reshape(B, S, M_WIDTH).astype(x.dtype) * jax.nn.sigmoid(mo)

        aq = rope(rms_norm(aq.reshape(B, S, A_HEADS, A_HEAD_DIM), g_q[l]), pos)
        ak = rope(rms_norm(ak.reshape(B, S, A_KV_HEADS, A_HEAD_DIM), g_k[l]), pos)
        av = av.reshape(B, S, A_KV_HEADS, A_HEAD_DIM)
        ha = sliding_window_attention(aq, ak, av, sinks[l]).reshape(B, S, A_WIDTH)

        mixed = (jax.nn.sigmoid(gm) * jnp.matmul(hm, w_proj_m[l])
                 + jax.nn.sigmoid(ga) * jnp.matmul(ha, w_proj_a[l]))
        x = x + jnp.matmul(mixed, w_out[l])

        hf = rms_norm(x, g_ffn[l]).reshape(B * S, D)
        y = hierarchical_moe(hf, w_group[l], b_group[l], w_expert[l], b_expert[l],
                             w_gate[l], w_up[l], w_down[l])
        x = x + y.reshape(B, S, D)
    return x
```

```python
import math
import numpy as np
import ml_dtypes
from contextlib import ExitStack
import concourse.bass as bass
import concourse.mybir as mybir
from concourse.bass_utils import run_bass_kernel_spmd

F32 = mybir.dt.float32
BF16 = mybir.dt.bfloat16
I32 = mybir.dt.int32
U8 = mybir.dt.uint8
AF = mybir.ActivationFunctionType
ALU = mybir.AluOpType
AX = mybir.AxisListType

S_LEN = 2048
D = 2048
D_IN = 9736
NT = 16
TB = 512
NTB = 4
EPS = 1e-6
N_EXP = 64
D_EXP = 768
C_MQ, C_MK, C_MV, C_MO, C_G, C_AQ, C_AK, C_AV, C_GM, C_GA = 0, 1024, 2048, 3072, 4096, 4104, 5128, 5384, 5640, 7688
LN16 = math.log(16.0)

ENGS = ("pe", "act", "dve", "pool", "sp")


class Sched:
    NDSEM = 20

    def __init__(self, nc, es):
        self.nc = nc
        self.q = {e: [] for e in ENGS}
        self.oprecs = {e: [] for e in ENGS}
        self.cnt = {e: 0 for e in ENGS}
        self.sem = {e: es.enter_context(nc.semaphore("s_" + e)) for e in ENGS}
        self.dsem, self.dcount, self.dnext = {}, {}, {}
        for e in ("sp", "act", "pool"):
            self.dsem[e] = [es.enter_context(nc.semaphore("d_%s%d" % (e, i))) for i in range(self.NDSEM)]
            self.dcount[e] = [0] * self.NDSEM
            self.dnext[e] = 0
        self.seen = {e: {} for e in ENGS}
        self.res = {}
        self.ninst = 0

    def _need(self, eng, key, val, waits):
        if eng == "pe" and key == "pe":
            return
        if self.seen[eng].get(key, 0) >= val:
            return
        self.seen[eng][key] = val
        waits[key] = max(waits.get(key, 0), val)
        if isinstance(key, str):
            self.oprecs[key][val - 1]["sig"] = True

    def _deps(self, eng, reads, writes):
        waits = {}
        for r in reads:
            ent = self.res.get(r)
            if ent and ent[0]:
                self._need(eng, ent[0][0], ent[0][1], waits)
            if ent and r.startswith("ps"):
                for k, v in ent[1].items():
                    if k != eng:
                        self._need(eng, k, v, waits)
        for r in writes:
            ent = self.res.get(r)
            if ent:
                if ent[0]:
                    self._need(eng, ent[0][0], ent[0][1], waits)
                for k, v in ent[1].items():
                    self._need(eng, k, v, waits)
        return waits

    def _commit(self, tok, reads, writes):
        for r in reads:
            ent = self.res.setdefault(r, [None, {}])
            ent[1][tok[0]] = max(ent[1].get(tok[0], 0), tok[1])
        for r in writes:
            self.res[r] = [tok, {}]

    def op(self, eng, fn, reads=(), writes=()):
        waits = self._deps(eng, reads, writes)
        self.cnt[eng] += 1
        tok = (eng, self.cnt[eng])
        self._commit(tok, reads, writes)
        rec = {"kind": "op", "fn": fn, "waits": waits, "sig": False}
        self.q[eng].append(rec)
        self.oprecs[eng].append(rec)
        self.ninst += 1 + len(waits)
        return tok

    def dma(self, eng, fn, reads=(), writes=()):
        waits = self._deps(eng, reads, writes)
        i = self.dnext[eng]
        self.dnext[eng] = (i + 1) % self.NDSEM
        key = (eng, i)
        prev = self.dcount[eng][i]
        if prev:
            self._need(eng, key, prev, waits)
        self.dcount[eng][i] = prev + 16
        tok = (key, prev + 16)
        self._commit(tok, reads, writes)
        self.q[eng].append({"kind": "dma", "fn": fn, "waits": waits, "dsem": self.dsem[eng][i]})
        self.ninst += 1 + len(waits)
        return tok

    def barrier(self, engines=ENGS):
        toks = [(e, self.cnt[e]) for e in ENGS if self.cnt[e]]
        for e in self.dsem:
            for i in range(self.NDSEM):
                if self.dcount[e][i]:
                    toks.append(((e, i), self.dcount[e][i]))
        for eng in engines:
            waits = {}
            for k, v in toks:
                self._need(eng, k, v, waits)
            self.q[eng].append({"kind": "wait", "waits": waits})

    def emit(self):
        nc = self.nc
        sigmap = {}
        for eng in ENGS:
            cum, m = 0, []
            for rec in self.oprecs[eng]:
                if rec["sig"]:
                    cum += 1
                m.append(cum)
            sigmap[eng] = m
        self.nsig = {e: (sigmap[e][-1] if sigmap[e] else 0) for e in ENGS}

        def replay(eng, e):
            sem = self.sem[eng]
            for rec in self.q[eng]:
                for k, v in rec["waits"].items():
                    if isinstance(k, str):
                        e.wait_ge(self.sem[k], sigmap[k][v - 1])
                    else:
                        e.wait_ge(self.dsem[k[0]][k[1]], v)
                if rec["kind"] == "op":
                    ins = rec["fn"](e)
                    if rec["sig"]:
                        ins.then_inc(sem, 1)
                elif rec["kind"] == "dma":
                    rec["fn"](e).then_inc(rec["dsem"], 16)

        with nc.Block() as block:
            @block.sync
            def _(e):
                replay("sp", e)

            @block.tensor
            def _(e):
                replay("pe", e)

            @block.scalar
            def _(e):
                replay("act", e)

            @block.vector
            def _(e):
                replay("dve", e)

            @block.gpsimd
            def _(e):
                replay("pool", e)


class Arena:
    def __init__(self, nc, es, nbytes):
        self.t = es.enter_context(nc.sbuf_tensor("arena", [128, nbytes], U8))
        self.off = 0
        self.cap = nbytes
        self.peak = 0

    def alloc(self, shape, dt):
        esz = {F32: 4, BF16: 2, I32: 4, U8: 1}[dt]
        n = esz
        for s in shape:
            n *= s
        off = (self.off + 63) // 64 * 64
        assert off + n <= self.cap, ("arena overflow", off, n, self.cap)
        self.off = off + n
        self.peak = max(self.peak, self.off)
        ap = self.t[:, off:off + n]
        if dt != U8:
            ap = ap.bitcast(dt)
        if len(shape) == 2:
            ap = ap.rearrange("p (a b) -> p a b", b=shape[1])
        elif len(shape) == 3:
            ap = ap.rearrange("p (a b c) -> p a b c", b=shape[1], c=shape[2])
        return ap

    def mark(self):
        return self.off

    def release(self, m):
        self.off = m


def build_nc(debug=None, stop=None, moe=True):
    nc = bass.Bass("TRN2", target_bir_lowering=False)
    dram = lambda name, shape, dt, kind="ExternalInput": nc.dram_tensor(name, list(shape), dt, kind=kind).ap()
    x = dram("x", [S_LEN, D], F32)
    w_in = dram("w_in", [D, D_IN], F32)
    convw = dram("convw", [128, 64], F32)
    gmixT = dram("gmixT", [128, 16], F32)
    gbias = dram("gbias", [128, 8], F32)
    gml = dram("gml", [128, 1024], F32)
    gqk = dram("gqk", [128, 2], F32)
    sinks = dram("sinks", [128, 16], F32)
    w_pm = dram("w_proj_m", [1024, D], F32)
    w_pa = dram("w_proj_a", [1024, D], F32)
    w_out = dram("w_out", [D, D], F32)
    gffn = dram("gffn", [128, D], F32)
    wroute = dram("wroute", [D, 72], F32)
    broute = dram("broute", [128, 72], F32)
    if moe:
        w_gate = dram("w_gate", [N_EXP, D, D_EXP], F32)
        w_up = dram("w_up", [N_EXP, D, D_EXP], F32)
        w_down = dram("w_down", [N_EXP, D_EXP, D], F32)
    cmat = dram("cmat", [128, 6, 128], F32)
    ctab = dram("ctab", [128, 2, S_LEN], F32)
    eoff = dram("eoff", [128, 64], F32)
    out = dram("out", [S_LEN, D], F32, kind="ExternalOutput")
    x1s = dram("x1s", [S_LEN, D], F32, kind="Internal")
    xr = dram("xr", [N_EXP * 128, D], BF16, kind="Internal")
    yr = dram("yr", [N_EXP * 128, D], BF16, kind="Internal")
    dbg = {}
    if debug:
        for name, shape, dt in debug:
            dbg[name] = dram(name, shape, dt, kind="ExternalOutput")

    if "d_x1" in dbg:
        x1s = dbg["d_x1"]
    with ExitStack() as es:
        S = Sched(nc, es)
        A = Arena(nc, es, 206 * 1024)
        PSA = es.enter_context(nc.psum_tensor("psA", [128, 8, 512], F32))
        ps = lambda b: PSA[:, b, :]
        psb = lambda b: PSA[:, b, :].bitcast(BF16)
        PK = lambda b: "ps%d" % b
        out_toks = []

        def OP(eng, fn, reads=(), writes=()):
            return S.op(eng, fn, reads, writes)

        def dbg_dump(name, src_ap, reads, dst=None):
            if name in dbg:
                d = dbg[name] if dst is None else dst
                out_toks.append(S.dma("sp", lambda e: e.dma_start(out=d, in_=src_ap), reads=reads, writes=["dbg." + name + str(len(out_toks))]))

        cm_f = A.alloc((6, 128), F32)
        cm_b = A.alloc((6, 128), BF16)
        ones_f = A.alloc((128,), F32)
        ones_b = A.alloc((128,), BF16)
        convw_t = A.alloc((16, 4), F32)
        gmixT_t = A.alloc((16,), F32)
        gbias_t = A.alloc((8,), F32)
        gqk_t = A.alloc((2,), F32)
        esk_t = A.alloc((16,), F32)
        ident_f, tri_f, stri_f = cm_f[:, 0, :], cm_f[:, 1, :], cm_f[:, 2, :]
        ident_b, tri_b, stri_b, swap_b, bd_b, gt_b = (cm_b[:, i, :] for i in range(6))
        S.dma("sp", lambda e: e.dma_start(out=cm_f, in_=cmat), writes=["cm_f"])
        S.dma("sp", lambda e: e.dma_start(out=convw_t, in_=convw.rearrange("p (c j) -> p c j", j=4)), writes=["convw"])
        S.dma("sp", lambda e: e.dma_start(out=gmixT_t, in_=gmixT), writes=["gmixT"])
        S.dma("sp", lambda e: e.dma_start(out=gbias_t, in_=gbias), writes=["gbias"])
        S.dma("sp", lambda e: e.dma_start(out=gqk_t, in_=gqk), writes=["gqk"])
        S.dma("sp", lambda e: e.dma_start(out=esk_t, in_=sinks), writes=["esk"])
        OP("dve", lambda e: e.tensor_copy(cm_b, cm_f), reads=["cm_f"], writes=["cm_b"])
        OP("dve", lambda e: e.memset(ones_f, 1.0), writes=["ones_f"])
        OP("dve", lambda e: e.memset(ones_b, 1.0), writes=["ones_b"])
        OP("act", lambda e: e.activation(esk_t, esk_t, AF.Exp), reads=["esk"], writes=["esk"])
        CONST_R = ["cm_f", "cm_b", "ones_f", "ones_b", "convw", "gmixT", "gbias", "gqk", "esk"]

        XT = [A.alloc((D,), F32) for _ in range(2)]
        zt = XT[1].bitcast(BF16)
        xr_z = xr.rearrange("(a p c) d -> a p (c d)", p=128, c=2)
        XR_Z = ["xr.z%d" % a for a in range(32)]

        def zero_fill():
            OP("dve", lambda e: e.memset(zt, 0.0), writes=["xt1"])
            for a in range(32):
                S.dma("sp", lambda e, a=a: e.dma_start(out=xr_z[a], in_=zt), reads=["xt1"], writes=["xr.z%d" % a])
        p1_mark = A.mark()
        gml_t = A.alloc((1024,), F32)
        S.dma("sp", lambda e: e.dma_start(out=gml_t, in_=gml), writes=["gml"])
        hb = A.alloc((D,), BF16)
        hT = A.alloc((16, TB), BF16)
        NW = 6
        WB = [A.alloc((8192,), U8) for _ in range(NW)]
        wstate = {"i": 0}
        ss_t = A.alloc((NT,), F32)
        rs_t = A.alloc((NT,), F32)
        ZC = [A.alloc((TB + 3,), F32) for _ in range(2)]
        ACC = [A.alloc((TB,), F32) for _ in range(2)]
        hal = A.alloc((16, 3), F32)
        mqkT = A.alloc((16, TB), BF16)
        v_ext = A.alloc((4, 4, 258), BF16)
        gs = A.alloc((4, 1024), BF16)
        graw = A.alloc((4, 8), F32)
        tab = A.alloc((2, TB), F32)
        aqT = A.alloc((8, TB), BF16)
        akT = A.alloc((2, 128 + TB), BF16)
        avd = A.alloc((5, 4, 128), BF16)
        hmT = A.alloc((8, TB), BF16)
        haT = A.alloc((8, TB), BF16)
        Cf = A.alloc((4, 2, 257), F32)
        Cb = A.alloc((4, 2, 258), BF16)
        f16 = lambda: A.alloc((16,), F32)
        v3 = lambda t: t.rearrange("p (a b) -> p a b", b=4)
        lfn, negB, u_t, euS, euE, dec, flr, dt1, dt2, dt3, dt4, Dg = (f16() for _ in range(12))
        NB_all = A.alloc((5, 4), F32)
        U_all = A.alloc((5, 4), F32)
        cm_s = A.alloc((1,), F32)
        kp = [A.alloc((256,), BF16) for _ in range(2)]
        PT = [A.alloc((128,), BF16) for _ in range(2)]
        msc = A.alloc((4, 8), F32)
        hsq = A.alloc((256,), BF16)
        ytok = [A.alloc((1024,), BF16) for _ in range(2)]
        sqb = A.alloc((TB,), BF16)
        rstd_a = ZC[0][:, 0:TB]
        xn_a = A.alloc((TB,), BF16)
        t1_a = ACC[0]
        t2_a = ACC[1]
        PTa = [A.alloc((TB,), BF16) for _ in range(2)]
        PTb = [A.alloc((TB,), BF16) for _ in range(2)]
        rd_a = [A.alloc((4, 128), F32) for _ in range(2)]
        sg_m, sg_a = PTa, PTb
        tm1 = ACC
        tm2 = [ZC[0][:, 0:TB], ZC[1][:, 0:TB]]
        mixT = mqkT
        XP = [A.alloc((256,), F32) for _ in range(3)]
        XO = [A.alloc((256,), F32) for _ in range(3)]

        OP("dve", lambda e: e.memset(hal, 0.0), writes=["hal"])
        OP("dve", lambda e: e.memset(v_ext, 1.0), writes=["v_ext"])
        OP("dve", lambda e: e.memset(Cf, 0.0), writes=["Cf"])
        OP("dve", lambda e: e.memset(Cb, 0.0), writes=["Cb"])
        OP("dve", lambda e: e.memset(NB_all, 0.0), writes=["NB_all"])
        OP("dve", lambda e: e.memset(U_all, 0.0), writes=["U_all"])
        OP("dve", lambda e: e.memset(ss_t, 0.0), writes=["ss"])
        OP("dve", lambda e: e.memset(akT, 0.0), writes=["akT"])
        OP("dve", lambda e: e.memset(avd, 0.0), writes=["avd"])

        def load_w(src2d, kch, ncols, runs=None):
            i = wstate["i"]
            wstate["i"] = (i + 1) % NW
            key = "wb%d" % i
            assert kch * ncols * 2 <= 8192
            view = WB[i][:, 0:kch * ncols * 2].bitcast(BF16).rearrange("p (k c) -> p k c", c=ncols)
            if runs is None:
                S.dma("pool", lambda e: e.dma_start(out=view, in_=src2d.rearrange("(k p) c -> p k c", p=128)), writes=[key])
            else:
                for (dc0, src) in runs:
                    n = src.shape[1]
                    S.dma("pool", lambda e, dc0=dc0, src=src, n=n: e.dma_start(out=view[:, :, dc0:dc0 + n], in_=src.rearrange("(k p) c -> p k c", p=128)), writes=[key])
            return view, key

        pbank = {"i": 0}

        def next_bank(lo=0, n=4):
            b = lo + pbank["i"] % n
            pbank["i"] += 1
            return b

        for tb in range(NTB):
            t0 = tb * TB
            for j in range(4):
                tt = tb * 4 + j
                xt = XT[tt % 2]
                xk = "xt%d" % (tt % 2)
                S.dma("sp", lambda e, xt=xt, tt=tt: e.dma_start(out=xt, in_=x[tt * 128:(tt + 1) * 128, :]), writes=[xk])
                OP("act", lambda e, xt=xt, tt=tt: e.activation(hb, xt, AF.Square, accum_out=ss_t[:, tt:tt + 1]), reads=[xk, "ss"], writes=["hb", "ss%d" % tt])
                OP("act", lambda e, tt=tt: e.activation(rs_t[:, tt:tt + 1], ss_t[:, tt:tt + 1], AF.Ln, scale=1.0 / D, bias=EPS), reads=["ss%d" % tt], writes=["rs%d" % tt])
                OP("act", lambda e, tt=tt: e.activation(rs_t[:, tt:tt + 1], rs_t[:, tt:tt + 1], AF.Exp, scale=-0.5), reads=["rs%d" % tt], writes=["rs%d" % tt])
                OP("dve", lambda e, xt=xt, tt=tt: e.tensor_scalar(hb, xt, rs_t[:, tt:tt + 1], None, op0=ALU.mult), reads=[xk, "rs%d" % tt], writes=["hb"])
                for half in range(2):
                    b = half
                    for kk in range(8):
                        k = half * 8 + kk
                        OP("pe", lambda e, b=b, kk=kk, k=k: e.transpose(psb(b)[:, kk * 128:(kk + 1) * 128], hb[:, k * 128:(k + 1) * 128], ident_b),
                           reads=["hb", "cm_b"], writes=[PK(b)])
                    OP("dve", lambda e, b=b, half=half, j=j: e.tensor_tensor(
                        hT[:, half * 8:(half + 1) * 8, j * 128:(j + 1) * 128],
                        psb(b).rearrange("p (k t) -> p k t", t=128),
                        gmixT_t[:, half * 8:(half + 1) * 8].unsqueeze(2).to_broadcast([128, 8, 128]), op=ALU.mult),
                        reads=[PK(b), "gmixT"], writes=["hT"])
            S.dma("sp", lambda e, t0=t0: e.dma_start(out=tab, in_=ctab[:, :, t0:t0 + TB]), writes=["tab"])

            def proj_fm(wv, wk, c0, ncol_chunk=128):
                b = next_bank(0, 4)
                for k in range(16):
                    OP("pe", lambda e, b=b, k=k, c0=c0: e.matmul(ps(b), wv[:, k, c0:c0 + 128], hT[:, k, :], start=(k == 0), stop=(k == 15)),
                       reads=[wk, "hT"], writes=[PK(b)])
                return b

            def proj_tm(wv, wk, j, ncols):
                b = next_bank(0, 4)
                for k in range(16):
                    OP("pe", lambda e, b=b, k=k, j=j: e.matmul(ps(b)[:, 0:ncols], hT[:, k, j * 128:(j + 1) * 128], wv[:, k, 0:ncols], start=(k == 0), stop=(k == 15)),
                       reads=[wk, "hT"], writes=[PK(b)])
                return b

            for g8 in range(8):
                wv, wk = load_w(w_in[:, g8 * 256:(g8 + 1) * 256], 16, 256)
                for c2 in range(2):
                    cj = g8 * 2 + c2
                    b = proj_fm(wv, wk, c2 * 128)
                    zc, zk = ZC[cj % 2], "zc%d" % (cj % 2)
                    acc, ak_ = ACC[cj % 2], "acc%d" % (cj % 2)
                    OP("act", lambda e, zc=zc, cj=cj: e.activation(zc[:, 0:3], hal[:, cj, :], AF.Copy), reads=["hal%d" % cj, "hal"], writes=[zk + "h"])
                    OP("act", lambda e, zc=zc, b=b: e.activation(zc[:, 3:TB + 3], ps(b), AF.Copy), reads=[PK(b)], writes=[zk])
                    OP("act", lambda e, zc=zc, cj=cj: e.activation(hal[:, cj, :], zc[:, TB:TB + 3], AF.Copy), reads=[zk], writes=["hal%d" % cj])
                    OP("dve", lambda e, zc=zc, acc=acc, cj=cj: e.tensor_scalar(acc, zc[:, 3:TB + 3], convw_t[:, cj, 3:4], None, op0=ALU.mult),
                       reads=[zk, zk + "h", "convw"], writes=[ak_])
                    for tap in (2, 1, 0):
                        OP("dve", lambda e, zc=zc, acc=acc, cj=cj, tap=tap: e.scalar_tensor_tensor(acc, zc[:, tap:tap + TB], convw_t[:, cj, tap:tap + 1], acc, op0=ALU.mult, op1=ALU.add),
                           reads=[zk, zk + "h", ak_], writes=[ak_])
                    OP("act", lambda e, acc=acc, cj=cj: e.activation(mqkT[:, cj, :], acc, AF.Silu), reads=[ak_], writes=["mqkT%d" % cj])
            for hh in range(4):
                wv, wk = load_w(w_in[:, C_MV + hh * 256:C_MV + (hh + 1) * 256], 16, 256)
                for j in range(4):
                    b = proj_tm(wv, wk, j, 256)
                    OP("act", lambda e, b=b, j=j, hh=hh: e.activation(v_ext[:, j, hh, 0:256], ps(b)[:, 0:256], AF.Copy), reads=[PK(b)], writes=["v%d_%d" % (j, hh)])
            for hh in range(4):
                wv, wk = load_w(w_in[:, C_MO + hh * 256:C_MO + (hh + 1) * 256], 16, 256)
                for j in range(4):
                    b = proj_tm(wv, wk, j, 256)
                    OP("act", lambda e, b=b, j=j, hh=hh: e.activation(gs[:, j, hh * 256:(hh + 1) * 256], ps(b)[:, 0:256], AF.Sigmoid), reads=[PK(b)], writes=["gs%d_%d" % (j, hh)])
                    OP("dve", lambda e, j=j, hh=hh: e.tensor_tensor(gs[:, j, hh * 256:(hh + 1) * 256], gs[:, j, hh * 256:(hh + 1) * 256], gml_t[:, hh * 256:(hh + 1) * 256], op=ALU.mult),
                       reads=["gs%d_%d" % (j, hh), "gml"], writes=["gs%d_%d" % (j, hh)])
            wv, wk = load_w(w_in[:, C_G:C_G + 8], 16, 8)
            for j in range(4):
                b = proj_tm(wv, wk, j, 8)
                OP("dve", lambda e, b=b, j=j: e.tensor_tensor(graw[:, j, :], ps(b)[:, 0:8], gbias_t, op=ALU.add), reads=[PK(b), "gbias"], writes=["graw%d" % j])

            def qk_post(b, gcol, dst, dkey, dsl):
                OP("act", lambda e: e.activation(sqb, ps(b), AF.Square), reads=[PK(b)], writes=["sqb"])
                OP("pe", lambda e: e.matmul(ps(4), bd_b, sqb, start=True, stop=True), reads=["sqb", "cm_b"], writes=[PK(4)])
                OP("act", lambda e: e.activation(rstd_a, ps(4), AF.Ln, scale=1.0 / 64, bias=EPS), reads=[PK(4)], writes=["zc0", "zc0h"])
                OP("act", lambda e: e.activation(rstd_a, rstd_a, AF.Exp, scale=-0.5), reads=["zc0"], writes=["zc0", "zc0h"])
                OP("dve", lambda e: e.scalar_tensor_tensor(xn_a, ps(b), gqk_t[:, gcol:gcol + 1], rstd_a, op0=ALU.mult, op1=ALU.mult),
                   reads=[PK(b), "gqk", "zc0"], writes=["xn_a"])
                OP("pe", lambda e: e.matmul(ps(5), swap_b, xn_a, start=True, stop=True), reads=["xn_a", "cm_b"], writes=[PK(5)])
                OP("dve", lambda e: e.tensor_tensor(t1_a, xn_a, tab[:, 0, :], op=ALU.mult), reads=["xn_a", "tab"], writes=["acc0"])
                OP("dve", lambda e: e.tensor_tensor(t2_a, ps(5), tab[:, 1, :], op=ALU.mult), reads=[PK(5), "tab"], writes=["acc1"])
                OP("dve", lambda e: e.tensor_tensor(dst, t1_a, t2_a, op=ALU.add), reads=["acc0", "acc1"], writes=[dkey])

            if tb > 0:
                OP("act", lambda e: e.activation(akT[:, :, 0:128], akT[:, :, TB:TB + 128], AF.Copy), reads=["akT"], writes=["akT"])
                OP("act", lambda e: e.activation(avd[:, 0, :, :], avd[:, 4, :, :], AF.Copy), reads=["avd"], writes=["avd"])
            wv, wk = load_w(w_in[:, C_AK:C_AK + 256], 16, 256)
            for i in range(2):
                b = proj_fm(wv, wk, i * 128)
                qk_post(b, 1, akT[:, i, 128:128 + TB], "akT", None)
            wv, wk = load_w(w_in[:, C_AV:C_AV + 256], 16, 256)
            for j in range(4):
                b = proj_tm(wv, wk, j, 256)
                for dup in range(2):
                    OP("act", lambda e, b=b, j=j, dup=dup: e.activation(avd[:, 1 + j, :, dup * 64:(dup + 1) * 64], ps(b)[:, 0:256].rearrange("p (g c) -> p g c", c=64), AF.Copy),
                       reads=[PK(b)], writes=["avd"])
            for a in range(4):
                runs = []
                for c2 in range(2):
                    ci = a * 2 + c2
                    i, r = ci // 4, ci % 4
                    h0, h1 = 8 * i + r, 8 * i + 4 + r
                    runs.append((c2 * 128, w_in[:, C_AQ + h0 * 64:C_AQ + (h0 + 1) * 64]))
                    runs.append((c2 * 128 + 64, w_in[:, C_AQ + h1 * 64:C_AQ + (h1 + 1) * 64]))
                wv, wk = load_w(None, 16, 256, runs=runs)
                for c2 in range(2):
                    ci = a * 2 + c2
                    b = proj_fm(wv, wk, c2 * 128)
                    qk_post(b, 0, aqT[:, ci, :], "aqT%d" % ci, None)

            if "d_mqkT" in dbg:
                dbg_dump("d_mqkT", mqkT, ["mqkT%d" % c for c in range(16)], dst=dbg["d_mqkT"][:, :, t0:t0 + TB])
            if "d_v" in dbg:
                dbg_dump("d_v", v_ext, ["v%d_%d" % (j, hh) for j in range(4) for hh in range(4)], dst=dbg["d_v"][:, tb * 4:(tb + 1) * 4, :, :])
            if "d_gs" in dbg:
                dbg_dump("d_gs", gs, ["gs%d_%d" % (j, hh) for j in range(4) for hh in range(4)], dst=dbg["d_gs"][:, tb * 4:(tb + 1) * 4, :])
            if "d_graw" in dbg:
                dbg_dump("d_graw", graw, ["graw%d" % j for j in range(4)], dst=dbg["d_graw"][:, tb * 4:(tb + 1) * 4, :])
            if "d_aqT" in dbg:
                dbg_dump("d_aqT", aqT, ["aqT%d" % c for c in range(8)], dst=dbg["d_aqT"][:, :, t0:t0 + TB])
            if "d_akT" in dbg:
                dbg_dump("d_akT", akT[:, :, 128:128 + TB], ["akT"], dst=dbg["d_akT"][:, :, t0:t0 + TB])
            if stop == "1b":
                continue

            if tb == 0:
                zero_fill()
            GR = ["graw%d" % j for j in range(4)]
            OP("act", lambda e: e.activation(v3(lfn), graw[:, :, 4:8], AF.Exp, scale=-1.0), reads=GR, writes=["lfn"])
            OP("act", lambda e: e.activation(lfn, lfn, AF.Ln, bias=1.0), reads=["lfn"], writes=["lfn"])
            OP("pe", lambda e: e.matmul(ps(4)[:, 0:16], tri_f, lfn, start=True, stop=True), reads=["lfn", "cm_f"], writes=[PK(4)])
            OP("pe", lambda e: e.matmul(ps(4)[:, 16:32], ones_f, lfn, start=True, stop=True), reads=["lfn", "ones_f"], writes=[PK(4)])
            if tb > 0:
                OP("dve", lambda e: e.tensor_copy(NB_all[:, 0, :], NB_all[:, 4, :]), reads=["NB_all"], writes=["NB_all"])
                OP("dve", lambda e: e.tensor_copy(U_all[:, 0, :], U_all[:, 4, :]), reads=["U_all"], writes=["U_all"])
            for j in range(4):
                OP("dve", lambda e, j=j: e.tensor_tensor(NB_all[:, j + 1, :], NB_all[:, j, :], ps(4)[:, 16 + j * 4:20 + j * 4], op=ALU.add), reads=["NB_all", PK(4)], writes=["NB_all"])
            OP("dve", lambda e: e.tensor_tensor(v3(negB), v3(ps(4)[:, 0:16]), NB_all[:, 0:4, :], op=ALU.add), reads=[PK(4), "NB_all"], writes=["negB"])
            OP("dve", lambda e: e.tensor_tensor(v3(u_t), graw[:, :, 0:4], v3(negB), op=ALU.add), reads=GR + ["negB"], writes=["u_t"])
            OP("pe", lambda e: e.transpose(ps(4)[0:16, 64:192], u_t, ident_f), reads=["u_t", "cm_f"], writes=[PK(4)])
            OP("dve", lambda e: e.reduce_max(cm_s[0:16, :], ps(4)[0:16, 64:192], axis=AX.X), reads=[PK(4)], writes=["cm_s"])
            OP("dve", lambda e: e.tensor_scalar(Dg[0:16, :], ident_f[0:16, 0:16], cm_s[0:16, 0:1], None, op0=ALU.mult), reads=["cm_s", "cm_f"], writes=["Dg"])
            OP("pe", lambda e: e.matmul(ps(4)[:, 32:48], ones_f[0:16, :], Dg[0:16, :], start=True, stop=True), reads=["Dg", "ones_f"], writes=[PK(4)])
            for j in range(4):
                OP("dve", lambda e, j=j: e.tensor_tensor(U_all[:, j + 1, :], U_all[:, j, :], ps(4)[:, 32 + j * 4:36 + j * 4], op=ALU.max), reads=["U_all", PK(4)], writes=["U_all"])
            Us, Ue = U_all[:, 0:4, :], U_all[:, 1:5, :]
            OP("dve", lambda e: e.scalar_tensor_tensor(v3(dt1), v3(u_t), -LN16, Us, op0=ALU.add, op1=ALU.subtract), reads=["u_t", "U_all"], writes=["dt1"])
            OP("act", lambda e: e.activation(euS, dt1, AF.Exp), reads=["dt1"], writes=["euS"])
            OP("dve", lambda e: e.scalar_tensor_tensor(v3(dt2), v3(u_t), -LN16, Ue, op0=ALU.add, op1=ALU.subtract), reads=["u_t", "U_all"], writes=["dt2"])
            OP("act", lambda e: e.activation(euE, dt2, AF.Exp), reads=["dt2"], writes=["euE"])
            OP("dve", lambda e: e.tensor_tensor(v3(dt3), Us, Ue, op=ALU.subtract), reads=["U_all"], writes=["dt3"])
            OP("act", lambda e: e.activation(dec, dt3, AF.Exp), reads=["dt3"], writes=["dec"])
            OP("dve", lambda e: e.tensor_tensor(v3(dt4), v3(negB), Us, op=ALU.subtract), reads=["negB", "U_all"], writes=["dt4"])
            OP("act", lambda e: e.activation(flr, dt4, AF.Exp), reads=["dt4"], writes=["flr"])

            def mlstm_unit(j, h):
                yt_, yk = ytok[j % 2], "ytok%d" % (j % 2)
                tsl = slice(j * 128, (j + 1) * 128)
                col = j * 4 + h
                qc = (2 * h, 2 * h + 1)
                kc = (8 + 2 * h, 9 + 2 * h)
                QR = ["mqkT%d" % c for c in qc]
                KR = ["mqkT%d" % c for c in kc]
                VK = "v%d_%d" % (j, h)
                kp_, kpk = kp[h % 2], "kp%d" % (h % 2)
                PT_, ptk = PT[h % 2], "PT%d" % (h % 2)
                nb = 2 + (h % 2)
                for half in range(2):
                    OP("pe", lambda e, half=half: e.transpose(psb(0)[:, half * 128:(half + 1) * 128], mqkT[:, kc[half], tsl], ident_b),
                       reads=[KR[half], "cm_b"], writes=[PK(0)])
                OP("act", lambda e: e.activation(kp_, psb(0)[:, 0:256], AF.Copy, scale=euE[:, col:col + 1]), reads=[PK(0), "euE"], writes=[kpk])
                for half in range(2):
                    OP("pe", lambda e, half=half: e.matmul(ps(1)[:, 0:128], mqkT[:, kc[half], tsl], mqkT[:, qc[half], tsl], start=(half == 0), stop=(half == 1)),
                       reads=[KR[half], QR[half]], writes=[PK(1)])
                OP("dve", lambda e: e.scalar_tensor_tensor(PT_, ps(1)[:, 0:128], euS[:, col:col + 1], tri_b, op0=ALU.mult, op1=ALU.mult),
                   reads=[PK(1), "euS", "cm_b"], writes=[ptk])
                for half in range(2):
                    OP("pe", lambda e, half=half: e.matmul(ps(nb)[:, 0:257], mqkT[:, qc[half], tsl], Cb[:, h, half, 0:257], start=(half == 0), stop=False),
                       reads=[QR[half], "Cb%d" % h], writes=[PK(nb)])
                OP("pe", lambda e: e.matmul(ps(nb)[:, 0:257], PT_, v_ext[:, j, h, 0:257], start=False, stop=True),
                   reads=[ptk, VK, "v_ext"], writes=[PK(nb)])
                for half in range(2):
                    OP("pe", lambda e, half=half: e.matmul(PSA[:, 4 + half, 0:257], kp_[:, half * 128:(half + 1) * 128], v_ext[:, j, h, 0:257], start=True, stop=True),
                       reads=[kpk, VK, "v_ext"], writes=[PK(4 + half)])
                OP("dve", lambda e: e.scalar_tensor_tensor(Cf[:, h, :, :], Cf[:, h, :, :], dec[:, col:col + 1], PSA[:, 4:6, 0:257], op0=ALU.mult, op1=ALU.add),
                   reads=["Cf", "Cf%d" % h, "dec", PK(4), PK(5)], writes=["Cf%d" % h])
                OP("act", lambda e: e.activation(Cb[:, h, :, 0:257], Cf[:, h, :, :], AF.Copy), reads=["Cf%d" % h, "Cb"], writes=["Cb%d" % h])
                m = msc[:, h, :]
                mk_ = "msc%d" % h
                OP("act", lambda e: e.activation(m[:, 0:1], ps(nb)[:, 256:257], AF.Abs), reads=[PK(nb)], writes=[mk_])
                OP("dve", lambda e: e.tensor_scalar(m[:, 0:1], m[:, 0:1], flr[:, col:col + 1], None, op0=ALU.max), reads=[mk_, "flr"], writes=[mk_])
                OP("dve", lambda e: e.reciprocal(m[:, 1:2], m[:, 0:1]), reads=[mk_], writes=[mk_])
                OP("dve", lambda e: e.memset(m[:, 2:3], 0.0), reads=[mk_], writes=[mk_])
                OP("act", lambda e: e.activation(hsq, ps(nb)[:, 0:256], AF.Square, scale=m[:, 1:2], accum_out=m[:, 2:3]), reads=[PK(nb), mk_], writes=[mk_, "hsq"])
                OP("act", lambda e: e.activation(m[:, 3:4], m[:, 2:3], AF.Ln, scale=1.0 / 256, bias=EPS), reads=[mk_], writes=[mk_])
                OP("act", lambda e: e.activation(m[:, 3:4], m[:, 3:4], AF.Exp, scale=-0.5), reads=[mk_], writes=[mk_])
                OP("dve", lambda e: e.tensor_tensor(m[:, 4:5], m[:, 1:2], m[:, 3:4], op=ALU.mult), reads=[mk_], writes=[mk_])
                OP("dve", lambda e: e.scalar_tensor_tensor(yt_[:, h * 256:(h + 1) * 256], ps(nb)[:, 0:256], m[:, 4:5], gs[:, j, h * 256:(h + 1) * 256], op0=ALU.mult, op1=ALU.mult),
                   reads=[PK(nb), mk_, "gs%d_%d" % (j, h)], writes=[yk])

            def mlstm_tile_end(j):
                yt_, yk = ytok[j % 2], "ytok%d" % (j % 2)
                tsl = slice(j * 128, (j + 1) * 128)
                for c in range(8):
                    OP("pe", lambda e, c=c: e.transpose(psb(0)[:, c * 128:(c + 1) * 128], yt_[:, c * 128:(c + 1) * 128], ident_b), reads=[yk, "cm_b"], writes=[PK(0)])
                OP("act", lambda e: e.activation(hmT[:, :, tsl], psb(0).rearrange("p (c t) -> p c t", t=128), AF.Copy), reads=[PK(0)], writes=["hmT"])

            AQR = ["aqT%d" % c for c in range(8)]

            def swa_unit(j, g):
                n = tb * 4 + j
                tsl = slice(j * 128, (j + 1) * 128)
                i, hf = g // 2, g % 2
                pr = slice(hf * 64, hf * 64 + 64)
                si = (j * 4 + g) % 2
                has_prev = n > 0
                pa_, pb_, rd = PTa[si], PTb[si], rd_a[si]
                pak, pbk, rdk = "PTa%d" % si, "PTb%d" % si, "rd%d" % si
                qv = aqT[pr, 4 * i:4 * i + 4, tsl]
                m3 = lambda t: t.rearrange("p (r q) -> p r q", q=128)
                if has_prev:
                    OP("pe", lambda e: e.matmul(ps(1), akT[pr, i, j * 128:(j + 1) * 128], qv, start=True, stop=True), reads=["akT"] + AQR, writes=[PK(1)])
                    OP("act", lambda e: e.activation(pa_, ps(1), AF.Exp, scale=0.125), reads=[PK(1)], writes=[pak])
                    OP("dve", lambda e: e.tensor_tensor(m3(pa_), m3(pa_), gt_b.unsqueeze(1).to_broadcast([128, 4, 128]), op=ALU.mult), reads=[pak, "cm_b"], writes=[pak])
                OP("pe", lambda e: e.matmul(ps(1), akT[pr, i, (j + 1) * 128:(j + 2) * 128], qv, start=True, stop=True), reads=["akT"] + AQR, writes=[PK(1)])
                OP("act", lambda e: e.activation(pb_, ps(1), AF.Exp, scale=0.125), reads=[PK(1)], writes=[pbk])
                OP("dve", lambda e: e.tensor_tensor(m3(pb_), m3(pb_), tri_b.unsqueeze(1).to_broadcast([128, 4, 128]), op=ALU.mult), reads=[pbk, "cm_b"], writes=[pbk])
                for (bk, lhs_fn, lk) in ((6, lambda jj: avd[:, jj, g, :], "avd"), (7, lambda jj: ones_b, "ones_b")):
                    if has_prev:
                        OP("pe", lambda e, bk=bk, lhs_fn=lhs_fn: e.matmul(ps(bk), lhs_fn(j), pa_, start=True, stop=False), reads=[lk, pak], writes=[PK(bk)])
                    OP("pe", lambda e, bk=bk, lhs_fn=lhs_fn: e.matmul(ps(bk), lhs_fn(j + 1), pb_, start=(not has_prev), stop=True), reads=[lk, pbk], writes=[PK(bk)])
                for r in range(4):
                    OP("act", lambda e, r=r: e.activation(rd[:, r, :], ps(7)[:, r * 128:(r + 1) * 128], AF.Ln, bias=esk_t[:, g * 4 + r:g * 4 + r + 1]), reads=[PK(7), "esk"], writes=[rdk])
                OP("act", lambda e: e.activation(rd, rd, AF.Exp, scale=-1.0), reads=[rdk], writes=[rdk])
                for par in range(2):
                    hp_ = slice(par * 64, par * 64 + 64)
                    OP("dve", lambda e, hp_=hp_, par=par: e.tensor_tensor(haT[hp_, 2 * g:2 * g + 2, tsl], m3(ps(6))[hp_, par::2, :], rd[hp_, par::2, :], op=ALU.mult),
                       reads=[PK(6), rdk], writes=["haT"])

            for j in range(4):
                import os as _os2
                _mm = _os2.environ.get("K_MIX", "both")
                for idx in range(4):
                    if _mm in ("both", "m"):
                        mlstm_unit(j, idx)
                    if _mm in ("both", "s"):
                        swa_unit(j, idx)
                if _mm in ("both", "m"):
                    mlstm_tile_end(j)
            if "d_hmT" in dbg:
                dbg_dump("d_hmT", hmT, ["hmT"], dst=dbg["d_hmT"][:, :, t0:t0 + TB])
            if "d_haT" in dbg:
                dbg_dump("d_haT", haT, ["haT"], dst=dbg["d_haT"][:, :, t0:t0 + TB])
            if stop in ("1c", "1d"):
                continue

            for pc in range(8):
                wgm, kgm = load_w(w_in[:, C_GM + pc * 256:C_GM + (pc + 1) * 256], 16, 256)
                wga, kga = load_w(w_in[:, C_GA + pc * 256:C_GA + (pc + 1) * 256], 16, 256)
                wpm, kpm = load_w(w_pm[:, pc * 256:(pc + 1) * 256], 8, 256)
                wpa, kpa = load_w(w_pa[:, pc * 256:(pc + 1) * 256], 8, 256)
                for c2 in range(2):
                    c = pc * 2 + c2
                    si = c % 2
                    bo = 4 * si
                    cs_ = slice(c2 * 128, (c2 + 1) * 128)
                    for (bk, wv_, wk_, src, skey, nk) in ((bo, wgm, kgm, hT, "hT", 16), (bo + 1, wga, kga, hT, "hT", 16),
                                                        (bo + 2, wpm, kpm, hmT, "hmT", 8), (bo + 3, wpa, kpa, haT, "haT", 8)):
                        for k in range(nk):
                            OP("pe", lambda e, bk=bk, wv_=wv_, k=k, cs_=cs_, src=src, nk=nk: e.matmul(ps(bk), wv_[:, k, cs_], src[:, k, :], start=(k == 0), stop=(k == nk - 1)),
                               reads=[wk_, skey], writes=[PK(bk)])
                    OP("act", lambda e, si=si, bo=bo: e.activation(sg_m[si], ps(bo), AF.Sigmoid), reads=[PK(bo)], writes=["PTa%d" % si])
                    OP("act", lambda e, si=si, bo=bo: e.activation(sg_a[si], ps(bo + 1), AF.Sigmoid), reads=[PK(bo + 1)], writes=["PTb%d" % si])
                    OP("dve", lambda e, si=si, bo=bo: e.tensor_tensor(tm1[si], sg_m[si], ps(bo + 2), op=ALU.mult), reads=["PTa%d" % si, PK(bo + 2)], writes=["acc%d" % si])
                    OP("dve", lambda e, si=si, bo=bo: e.tensor_tensor(tm2[si], sg_a[si], ps(bo + 3), op=ALU.mult), reads=["PTb%d" % si, PK(bo + 3)], writes=["zc%d" % si, "zc%dh" % si])
                    OP("dve", lambda e, si=si, c=c: e.tensor_tensor(mixT[:, c, :], tm1[si], tm2[si], op=ALU.add), reads=["acc%d" % si, "zc%d" % si], writes=["mqkT%d" % c])

            MXR = ["mqkT%d" % c for c in range(16)]
            for n8 in range(8):
                wo, wok = load_w(w_out[:, n8 * 256:(n8 + 1) * 256], 16, 256)
                for j in range(4):
                    tt = tb * 4 + j
                    b = next_bank(0, 8)
                    xi = (n8 * 4 + j) % 3
                    for k in range(16):
                        OP("pe", lambda e, b=b, k=k, j=j, wo=wo: e.matmul(ps(b)[:, 0:256], mixT[:, k, j * 128:(j + 1) * 128], wo[:, k, :], start=(k == 0), stop=(k == 15)),
                           reads=[wok] + MXR, writes=[PK(b)])
                    S.dma("sp", lambda e, xi=xi, tt=tt, n8=n8: e.dma_start(out=XP[xi], in_=x[tt * 128:(tt + 1) * 128, n8 * 256:(n8 + 1) * 256]), writes=["xp%d" % xi])
                    OP("dve", lambda e, xi=xi, b=b: e.tensor_tensor(XO[xi], ps(b)[:, 0:256], XP[xi], op=ALU.add), reads=[PK(b), "xp%d" % xi], writes=["xo%d" % xi])
                    tk = S.dma("sp", lambda e, xi=xi, tt=tt, n8=n8: e.dma_start(out=x1s[tt * 128:(tt + 1) * 128, n8 * 256:(n8 + 1) * 256], in_=XO[xi]), reads=["xo%d" % xi], writes=["x1s.%d.%d" % (tt, n8)])
                    if "d_x1" in dbg:
                        out_toks.append(tk)


        if stop in (None, "R", "E"):
            S.barrier()
            A.release(p1_mark)
            dest_i = A.alloc((NT, 2), I32)
            gate_t = A.alloc((NT, 2), F32)
            r_mark = A.mark()
            gffn_t = A.alloc((D,), F32)
            wr_t = A.alloc((16, 72), F32)
            br_t = A.alloc((72,), F32)
            eoff_t = A.alloc((64,), F32)
            cnt = A.alloc((64,), F32)
            ss2 = A.alloc((NT,), F32)
            rs2 = A.alloc((NT,), F32)
            X1T = [A.alloc((D,), F32) for _ in range(2)]
            HFB = [A.alloc((D,), BF16) for _ in range(2)]
            hfT = A.alloc((16, 128), F32)
            lg = A.alloc((72,), F32)
            sm = A.alloc((16,), F32)
            goh = A.alloc((8,), F32)
            gex = A.alloc((8,), F32)
            esel = A.alloc((8,), F32)
            esel2 = A.alloc((8,), F32)
            oh = [A.alloc((8,), F32) for _ in range(2)]
            t64 = A.alloc((64,), F32)
            Ek = [A.alloc((64,), F32) for _ in range(2)]
            Mb = A.alloc((64,), BF16)
            posb = A.alloc((64,), F32)
            dstf = A.alloc((2,), F32)
            S.dma("sp", lambda e: e.dma_start(out=gffn_t, in_=gffn), writes=["gffn"])
            S.dma("sp", lambda e: e.dma_start(out=wr_t, in_=wroute.rearrange("(k p) c -> p k c", p=128)), writes=["wr"])
            S.dma("sp", lambda e: e.dma_start(out=br_t, in_=broute), writes=["br"])
            S.dma("sp", lambda e: e.dma_start(out=eoff_t, in_=eoff), writes=["eoff"])
            OP("dve", lambda e: e.memset(cnt, 0.0), writes=["cnt"])
            OP("dve", lambda e: e.memset(ss2, 0.0), writes=["ss2"])
            g3 = lambda t: t.rearrange("p (g e) -> p g e", e=8)
            scat_keys = []
            for tt in range(NT):
                xt, xk = X1T[tt % 2], "x1t%d" % (tt % 2)
                hfb, hk = HFB[tt % 2], "hfb%d" % (tt % 2)
                S.dma("sp", lambda e, xt=xt, tt=tt: e.dma_start(out=xt, in_=x1s[tt * 128:(tt + 1) * 128, :]), reads=["x1s.%d.%d" % (tt, n8) for n8 in range(8)], writes=[xk])
                OP("act", lambda e, xt=xt, tt=tt, hfb=hfb: e.activation(hfb, xt, AF.Square, accum_out=ss2[:, tt:tt + 1]), reads=[xk, "ss2"], writes=[hk, "ss2_%d" % tt])
                OP("act", lambda e, tt=tt: e.activation(rs2[:, tt:tt + 1], ss2[:, tt:tt + 1], AF.Ln, scale=1.0 / D, bias=EPS), reads=["ss2_%d" % tt], writes=["rs2_%d" % tt])
                OP("act", lambda e, tt=tt: e.activation(rs2[:, tt:tt + 1], rs2[:, tt:tt + 1], AF.Exp, scale=-0.5), reads=["rs2_%d" % tt], writes=["rs2_%d" % tt])
                OP("dve", lambda e, xt=xt, tt=tt: e.scalar_tensor_tensor(xt, xt, rs2[:, tt:tt + 1], gffn_t, op0=ALU.mult, op1=ALU.mult), reads=[xk, "rs2_%d" % tt, "gffn"], writes=[xk])
                OP("act", lambda e, xt=xt, hfb=hfb: e.activation(hfb, xt, AF.Copy), reads=[xk], writes=[hk])
                for q4 in range(4):
                    for kk in range(4):
                        k = q4 * 4 + kk
                        OP("pe", lambda e, q4=q4, kk=kk, k=k, xt=xt: e.transpose(ps(q4)[:, kk * 128:(kk + 1) * 128], xt[:, k * 128:(k + 1) * 128], ident_f), reads=[xk, "cm_f"], writes=[PK(q4)])
                    eng = "act" if q4 % 2 == 0 else "dve"
                    if eng == "act":
                        OP("act", lambda e, q4=q4: e.activation(hfT[:, q4 * 4:(q4 + 1) * 4, :], ps(q4).rearrange("p (k t) -> p k t", t=128), AF.Copy), reads=[PK(q4)], writes=["hfT%d" % q4])
                    else:
                        OP("dve", lambda e, q4=q4: e.tensor_copy(hfT[:, q4 * 4:(q4 + 1) * 4, :], ps(q4).rearrange("p (k t) -> p k t", t=128)), reads=[PK(q4)], writes=["hfT%d" % q4])
                for k in range(16):
                    OP("pe", lambda e, k=k: e.matmul(ps(4)[:, 0:72], hfT[:, k, :], wr_t[:, k, :], start=(k == 0), stop=(k == 15)), reads=["hfT%d" % (k // 4), "wr"], writes=[PK(4)])
                OP("dve", lambda e: e.tensor_tensor(lg, ps(4)[:, 0:72], br_t, op=ALU.add), reads=[PK(4), "br"], writes=["lg"])
                OP("dve", lambda e: e.reduce_max(sm[:, 0:1], lg[:, 0:8], axis=AX.X), reads=["lg"], writes=["sm0"])
                OP("dve", lambda e: e.tensor_scalar(goh, lg[:, 0:8], sm[:, 0:1], None, op0=ALU.is_equal), reads=["lg", "sm0"], writes=["goh"])
                OP("dve", lambda e: e.tensor_scalar(sm[:, 1:2], sm[:, 0:1], -1.0, None, op0=ALU.mult), reads=["sm0"], writes=["sm1"])
                OP("dve", lambda e: e.memset(sm[:, 2:3], 0.0), writes=["sm2"])
                OP("act", lambda e: e.activation(gex, lg[:, 0:8], AF.Exp, bias=sm[:, 1:2], accum_out=sm[:, 2:3]), reads=["lg", "sm1", "sm2"], writes=["gex", "sm2"])
                OP("dve", lambda e: e.reciprocal(sm[:, 3:4], sm[:, 2:3]), reads=["sm2"], writes=["sm3"])
                OP("dve", lambda e: e.tensor_tensor(g3(t64), g3(lg[:, 8:72]), goh.unsqueeze(2).to_broadcast([128, 8, 8]), op=ALU.mult), reads=["lg", "goh"], writes=["t64"])
                OP("dve", lambda e: e.reduce_sum(esel, t64.rearrange("p (g e) -> p e g", e=8), axis=AX.X), reads=["t64"], writes=["esel"])
                OP("dve", lambda e: e.reduce_max(sm[:, 4:5], esel, axis=AX.X), reads=["esel"], writes=["sm4"])
                OP("dve", lambda e: e.tensor_scalar(oh[0], esel, sm[:, 4:5], None, op0=ALU.is_equal), reads=["esel", "sm4"], writes=["oh0"])
                OP("dve", lambda e: e.scalar_tensor_tensor(esel2, oh[0], -1e30, esel, op0=ALU.mult, op1=ALU.add), reads=["oh0", "esel"], writes=["esel2"])
                OP("dve", lambda e: e.reduce_max(sm[:, 5:6], esel2, axis=AX.X), reads=["esel2"], writes=["sm5"])
                OP("dve", lambda e: e.tensor_scalar(oh[1], esel2, sm[:, 5:6], None, op0=ALU.is_equal), reads=["esel2", "sm5"], writes=["oh1"])
                OP("dve", lambda e: e.tensor_tensor(sm[:, 6:7], sm[:, 4:5], sm[:, 5:6], op=ALU.subtract), reads=["sm4", "sm5"], writes=["sm6"])
                OP("act", lambda e: e.activation(sm[:, 7:8], sm[:, 6:7], AF.Sigmoid), reads=["sm6"], writes=["sm7"])
                OP("dve", lambda e: e.tensor_scalar(sm[:, 8:9], sm[:, 7:8], -1.0, 1.0, op0=ALU.mult, op1=ALU.add), reads=["sm7"], writes=["sm8"])
                OP("dve", lambda e, tt=tt: e.tensor_tensor(gate_t[:, tt, 0:1], sm[:, 7:8], sm[:, 3:4], op=ALU.mult), reads=["sm7", "sm3"], writes=["gate%d" % tt])
                OP("dve", lambda e, tt=tt: e.tensor_tensor(gate_t[:, tt, 1:2], sm[:, 8:9], sm[:, 3:4], op=ALU.mult), reads=["sm8", "sm3", "gate%d" % tt], writes=["gate%d" % tt])
                for kk in range(2):
                    OP("dve", lambda e, kk=kk: e.tensor_tensor(g3(Ek[kk]), goh.unsqueeze(2).to_broadcast([128, 8, 8]), oh[kk].unsqueeze(1).to_broadcast([128, 8, 8]), op=ALU.mult),
                       reads=["goh", "oh%d" % kk], writes=["Ek%d" % kk])
                OP("dve", lambda e: e.tensor_tensor(Mb, Ek[0], Ek[1], op=ALU.add), reads=["Ek0", "Ek1"], writes=["Mb"])
                OP("pe", lambda e: e.matmul(ps(5)[:, 0:64], stri_b, Mb, start=True, stop=True), reads=["Mb", "cm_b"], writes=[PK(5)])
                OP("pe", lambda e: e.matmul(ps(5)[:, 64:128], ones_b, Mb, start=True, stop=True), reads=["Mb", "ones_b"], writes=[PK(5)])
                OP("dve", lambda e: e.tensor_tensor(posb, ps(5)[:, 0:64], cnt, op=ALU.add), reads=[PK(5), "cnt"], writes=["posb"])
                OP("dve", lambda e: e.tensor_tensor(posb, posb, eoff_t, op=ALU.add), reads=["posb", "eoff"], writes=["posb"])
                OP("dve", lambda e: e.tensor_tensor(cnt, cnt, ps(5)[:, 64:128], op=ALU.add), reads=["cnt", PK(5), "posb"], writes=["cnt"])
                for kk in range(2):
                    OP("dve", lambda e, kk=kk: e.tensor_tensor(t64, Ek[kk], posb, op=ALU.mult), reads=["Ek%d" % kk, "posb"], writes=["t64"])
                    OP("dve", lambda e, kk=kk: e.reduce_sum(dstf[:, kk:kk + 1], t64, axis=AX.X), reads=["t64"], writes=["dstf%d" % kk])
                    OP("dve", lambda e, kk=kk: e.tensor_scalar(dstf[:, kk:kk + 1], dstf[:, kk:kk + 1], float(N_EXP * 128 - 1), None, op0=ALU.min), reads=["dstf%d" % kk], writes=["dstf%d" % kk])
                    OP("dve", lambda e, kk=kk, tt=tt: e.tensor_copy(dest_i[:, tt, kk:kk + 1], dstf[:, kk:kk + 1]), reads=["dstf%d" % kk], writes=["dest%d_%d" % (tt, kk)])
                    sk = "xr.s%d_%d" % (tt, kk)
                    S.dma("pool", lambda e, kk=kk, tt=tt, hfb=hfb: e.indirect_dma_start(out=xr, out_offset=bass.IndirectOffsetOnAxis(ap=dest_i[:, tt, kk:kk + 1], axis=0), in_=hfb, in_offset=None),
                          reads=[hk, "dest%d_%d" % (tt, kk)] + (XR_Z if (tt == 0 and kk == 0) else []), writes=[sk] + (XR_Z if (tt == 0 and kk == 0) else []))
                    scat_keys.append(sk)
            if "d_dest" in dbg:
                dbg_dump("d_dest", dest_i, ["dest%d_%d" % (tt, kk) for tt in range(NT) for kk in range(2)])
                dbg_dump("d_gate", gate_t, ["gate%d" % tt for tt in range(NT)])

            if stop in (None, "E"):
                S.barrier()
                A.release(r_mark)
                WGU = [A.alloc((16, 1536), BF16) for _ in range(2)]
                WD = [A.alloc((6, D), BF16) for _ in range(2)]
                XE = [A.alloc((D,), BF16) for _ in range(2)]
                XET = [A.alloc((16, 128), BF16) for _ in range(2)]
                sg_e = A.alloc((D_EXP,), BF16)
                up_e = A.alloc((D_EXP,), BF16)
                hmid = A.alloc((D_EXP,), BF16)
                hT6 = A.alloc((6, 128), BF16)
                YE = [A.alloc((D,), BF16) for _ in range(2)]
                import os as _os
                ESTOP = float(_os.environ.get("K_ESTOP", 9))
            for ex in range(int(_os.environ.get("K_NEXP", N_EXP))):
                    pi = ex % 2
                    wgu, wd, xe, xet, ye = WGU[pi], WD[pi], XE[pi], XET[pi], YE[pi]
                    S.dma("pool", lambda e, wgu=wgu, ex=ex: e.dma_start(out=wgu[:, :, 0:768], in_=w_gate[ex].rearrange("(k p) f -> p k f", p=128)), writes=["wg%d" % pi])
                    S.dma("pool", lambda e, wgu=wgu, ex=ex: e.dma_start(out=wgu[:, :, 768:1536], in_=w_up[ex].rearrange("(k p) f -> p k f", p=128)), writes=["wu%d" % pi])
                    S.dma("pool", lambda e, wd=wd, ex=ex: e.dma_start(out=wd, in_=w_down[ex].rearrange("(k p) d -> p k d", p=128)), writes=["wd%d" % pi])
                    S.dma("sp", lambda e, xe=xe, ex=ex: e.dma_start(out=xe, in_=xr[ex * 128:(ex + 1) * 128, :]), reads=(scat_keys if ex == 0 else []) + XR_Z, writes=["xe%d" % pi])
                    for half in range(2):
                        for kk in range(8):
                            k = half * 8 + kk
                            OP("pe", lambda e, half=half, kk=kk, k=k, xe=xe: e.transpose(psb(half)[:, kk * 128:(kk + 1) * 128], xe[:, k * 128:(k + 1) * 128], ident_b), reads=["xe%d" % pi, "cm_b"], writes=[PK(half)])
                        OP("act", lambda e, xet=xet, half=half: e.activation(xet[:, half * 8:(half + 1) * 8, :], psb(half).rearrange("p (k t) -> p k t", t=128), AF.Copy), reads=[PK(half)], writes=["xet%d_%d" % (pi, half)])
                    for nb in range(3):
                        wkeys = [["wg%d" % pi], ["wg%d" % pi, "wu%d" % pi], ["wu%d" % pi]][nb]
                        for k in range(16):
                            OP("pe", lambda e, nb=nb, k=k, xet=xet, wgu=wgu: e.matmul(ps(2 + nb), xet[:, k, :], wgu[:, k, nb * 512:(nb + 1) * 512], start=(k == 0), stop=(k == 15)),
                               reads=["xet%d_%d" % (pi, k // 8)] + wkeys, writes=[PK(2 + nb)])
                    OP("act", lambda e: e.activation(sg_e[:, 0:512], ps(2), AF.Silu), reads=[PK(2)], writes=["sg_e0"])
                    OP("act", lambda e: e.activation(sg_e[:, 512:768], ps(3)[:, 0:256], AF.Silu), reads=[PK(3)], writes=["sg_e1"])
                    OP("act", lambda e: e.activation(up_e[:, 0:256], ps(3)[:, 256:512], AF.Copy), reads=[PK(3)], writes=["up_e0"])
                    OP("act", lambda e: e.activation(up_e[:, 256:768], ps(4), AF.Copy), reads=[PK(4)], writes=["up_e1"])
                    OP("dve", lambda e: e.tensor_tensor(hmid, sg_e, up_e, op=ALU.mult), reads=["sg_e0", "sg_e1", "up_e0", "up_e1"], writes=["hmid"])
                    for kf in range(6):
                        OP("pe", lambda e, kf=kf: e.transpose(psb(5)[:, kf * 128:(kf + 1) * 128], hmid[:, kf * 128:(kf + 1) * 128], ident_b), reads=["hmid", "cm_b"], writes=[PK(5)])
                    OP("act", lambda e: e.activation(hT6, psb(5)[:, 0:768].rearrange("p (k t) -> p k t", t=128), AF.Copy), reads=[PK(5)], writes=["hT6"])
                    for cb in range(4):
                        bk = 6 + cb % 2
                        for kf in range(6):
                            OP("pe", lambda e, bk=bk, kf=kf, cb=cb, wd=wd: e.matmul(ps(bk), hT6[:, kf, :], wd[:, kf, cb * 512:(cb + 1) * 512], start=(kf == 0), stop=(kf == 5)),
                               reads=["hT6", "wd%d" % pi], writes=[PK(bk)])
                        OP("act", lambda e, bk=bk, cb=cb, ye=ye: e.activation(ye[:, cb * 512:(cb + 1) * 512], ps(bk), AF.Copy), reads=[PK(bk)], writes=["ye%d_%d" % (pi, cb)])
                    S.dma("sp", lambda e, ye=ye, ex=ex: e.dma_start(out=yr[ex * 128:(ex + 1) * 128, :], in_=ye), reads=["ye%d_%d" % (pi, cb) for cb in range(4)], writes=["yr%d" % ex])

            if stop is None:
                S.barrier()
                A.release(r_mark)
                Y1 = [A.alloc((D,), BF16) for _ in range(3)]
                Y2 = [A.alloc((D,), BF16) for _ in range(3)]
                XC = [A.alloc((D,), F32) for _ in range(3)]
                YRK = ["yr%d" % ex for ex in range(N_EXP)]
                for tt in range(NT):
                    pi = tt % 3
                    S.dma("pool", lambda e, tt=tt, pi=pi: e.indirect_dma_start(out=Y1[pi], out_offset=None, in_=yr, in_offset=bass.IndirectOffsetOnAxis(ap=dest_i[:, tt, 0:1], axis=0)),
                          reads=(YRK if tt == 0 else []) + ["dest%d_0" % tt], writes=["y1_%d" % pi])
                    S.dma("pool", lambda e, tt=tt, pi=pi: e.indirect_dma_start(out=Y2[pi], out_offset=None, in_=yr, in_offset=bass.IndirectOffsetOnAxis(ap=dest_i[:, tt, 1:2], axis=0)),
                          reads=["dest%d_1" % tt], writes=["y2_%d" % pi])
                    S.dma("sp", lambda e, tt=tt, pi=pi: e.dma_start(out=XC[pi], in_=x1s[tt * 128:(tt + 1) * 128, :]), writes=["xc%d" % pi])
                    OP("dve", lambda e, tt=tt, pi=pi: e.scalar_tensor_tensor(XC[pi], Y1[pi], gate_t[:, tt, 0:1], XC[pi], op0=ALU.mult, op1=ALU.add), reads=["y1_%d" % pi, "xc%d" % pi, "gate%d" % tt], writes=["xc%d" % pi])
                    OP("dve", lambda e, tt=tt, pi=pi: e.scalar_tensor_tensor(XC[pi], Y2[pi], gate_t[:, tt, 1:2], XC[pi], op0=ALU.mult, op1=ALU.add), reads=["y2_%d" % pi, "xc%d" % pi, "gate%d" % tt], writes=["xc%d" % pi])
                    out_toks.append(S.dma("sp", lambda e, tt=tt, pi=pi: e.dma_start(out=out[tt * 128:(tt + 1) * 128, :], in_=XC[pi]), reads=["xc%d" % pi], writes=["out%d" % tt]))
        if not out_toks:
            pass
        S.barrier(["sp"])
        S.emit()
        print("kernel build: instrs~%d arena_peak=%d signalling=%s of %s" % (S.ninst, A.peak, S.nsig, S.cnt))
    return nc


def _consts():
    p = np.arange(128)
    ident = np.eye(128, dtype=np.float32)
    tri = (p[:, None] <= p[None, :]).astype(np.float32)
    stri = (p[:, None] < p[None, :]).astype(np.float32)
    sw = np.where((p % 64) < 32, p + 32, p - 32)
    swap = np.zeros((128, 128), np.float32)
    swap[sw, p] = 1.0
    bd = ((p[:, None] // 64) == (p[None, :] // 64)).astype(np.float32)
    gt = (p[:, None] > p[None, :]).astype(np.float32)
    cmat = np.ascontiguousarray(np.stack([ident, tri, stri, swap, bd, gt], axis=1))
    half = 32
    freqs = (10000.0 ** (-np.arange(half, dtype=np.float32) / half)).astype(np.float32)
    pos = np.arange(S_LEN, dtype=np.float32)
    ang = (pos[None, :] * freqs[p % 32][:, None]).astype(np.float32)
    cos = np.cos(ang).astype(np.float32)
    sin = np.sin(ang).astype(np.float32) * np.where((p % 64) < 32, -1.0, 1.0)[:, None].astype(np.float32)
    ctab = np.ascontiguousarray(np.stack([cos, sin], axis=1)).astype(np.float32)
    eoff = np.ascontiguousarray(np.broadcast_to((np.arange(64, dtype=np.float32) * 128.0)[None, :], (128, 64)))
    return cmat, ctab, eoff


def make_in_maps(inputs, cores, moe=True):
    f = lambda k: np.ascontiguousarray(np.asarray(inputs[k], dtype=np.float32)[0])
    rep = lambda v: np.ascontiguousarray(np.broadcast_to(v.reshape(1, -1), (128, v.size)))
    cmat, ctab, eoff = _consts()
    conv = f("conv_qk")
    convw = np.ascontiguousarray(conv.T.reshape(16, 128, 4).transpose(1, 0, 2).reshape(128, 64))
    gmixT = np.ascontiguousarray(f("g_mix").reshape(16, 128).T)
    gbias = rep(np.concatenate([f("b_igate"), f("b_fgate")]))
    gml = rep(f("g_mlstm").reshape(-1))
    gqk = np.ascontiguousarray(np.stack([np.tile(f("g_q"), 2), np.tile(f("g_k"), 2)], axis=1))
    sinks = rep(f("sinks"))
    gffn = rep(f("g_ffn"))
    wroute = np.ascontiguousarray(np.concatenate([f("w_group"), f("w_expert")], axis=1))
    broute = rep(np.concatenate([f("b_group"), f("b_expert")]))
    shared = dict(w_in=f("w_in"), convw=convw, gmixT=gmixT, gbias=gbias, gml=gml, gqk=gqk, sinks=sinks,
                  w_proj_m=f("w_proj_m"), w_proj_a=f("w_proj_a"), w_out=f("w_out"), gffn=gffn,
                  wroute=wroute, broute=broute, cmat=cmat, ctab=ctab, eoff=eoff)
    if moe:
        shared.update(w_gate=f("w_gate"), w_up=f("w_up"), w_down=f("w_down"))
    xs = np.asarray(inputs["x"], dtype=np.float32)
    return [dict(shared, x=np.ascontiguousarray(xs[c])) for c in cores]


def kernel(**inputs):
    nc = build_nc()
    in_maps = make_in_maps(inputs, list(range(8)))
    res = run_bass_kernel_spmd(nc, in_maps, core_ids=list(range(8)))
    return np.stack([np.asarray(r["out"]) for r in res.results], axis=0).astype(np.float32)
```

```python
import math
import numpy as np
import ml_dtypes
from contextlib import ExitStack
import concourse.bass as bass
import concourse.mybir as mybir
from concourse.bass_utils import run_bass_kernel_spmd

F32 = mybir.dt.float32
BF16 = mybir.dt.bfloat16
I32 = mybir.dt.int32
U8 = mybir.dt.uint8
AF = mybir.ActivationFunctionType
ALU = mybir.AluOpType
AX = mybir.AxisListType

S_LEN = 2048
D = 2048
D_IN = 9736
NT = 16
TB = 512
NTB = 4
EPS = 1e-6
N_EXP = 64
D_EXP = 768
C_MQ, C_MK, C_MV, C_MO, C_G, C_AQ, C_AK, C_AV, C_GM, C_GA = 0, 1024, 2048, 3072, 4096, 4104, 5128, 5384, 5640, 7688
LN16 = math.log(16.0)

ENGS = ("pe", "act", "dve", "pool", "sp")


class Sched:
    NDSEM = 20

    def __init__(self, nc, es):
        self.nc = nc
        self.q = {e: [] for e in ENGS}
        self.oprecs = {e: [] for e in ENGS}
        self.cnt = {e: 0 for e in ENGS}
        self.sem = {e: es.enter_context(nc.semaphore("s_" + e)) for e in ENGS}
        self.dsem, self.dcount, self.dnext = {}, {}, {}
        for e in ("sp", "act", "pool"):
            self.dsem[e] = [es.enter_context(nc.semaphore("d_%s%d" % (e, i))) for i in range(self.NDSEM)]
            self.dcount[e] = [0] * self.NDSEM
            self.dnext[e] = 0
        self.seen = {e: {} for e in ENGS}
        self.res = {}
        self.ninst = 0

    def _need(self, eng, key, val, waits):
        if eng == "pe" and key == "pe":
            return
        if self.seen[eng].get(key, 0) >= val:
            return
        self.seen[eng][key] = val
        waits[key] = max(waits.get(key, 0), val)
        if isinstance(key, str):
            self.oprecs[key][val - 1]["sig"] = True

    def _deps(self, eng, reads, writes):
        waits = {}
        for r in reads:
            ent = self.res.get(r)
            if ent and ent[0]:
                self._need(eng, ent[0][0], ent[0][1], waits)
            if ent and r.startswith("ps"):
                for k, v in ent[1].items():
                    if k != eng:
                        self._need(eng, k, v, waits)
        for r in writes:
            ent = self.res.get(r)
            if ent:
                if ent[0]:
                    self._need(eng, ent[0][0], ent[0][1], waits)
                for k, v in ent[1].items():
                    self._need(eng, k, v, waits)
        return waits

    def _commit(self, tok, reads, writes):
        for r in reads:
            ent = self.res.setdefault(r, [None, {}])
            ent[1][tok[0]] = max(ent[1].get(tok[0], 0), tok[1])
        for r in writes:
            self.res[r] = [tok, {}]

    def op(self, eng, fn, reads=(), writes=()):
        waits = self._deps(eng, reads, writes)
        self.cnt[eng] += 1
        tok = (eng, self.cnt[eng])
        self._commit(tok, reads, writes)
        rec = {"kind": "op", "fn": fn, "waits": waits, "sig": False}
        self.q[eng].append(rec)
        self.oprecs[eng].append(rec)
        self.ninst += 1 + len(waits)
        return tok

    def dma(self, eng, fn, reads=(), writes=()):
        waits = self._deps(eng, reads, writes)
        i = self.dnext[eng]
        self.dnext[eng] = (i + 1) % self.NDSEM
        key = (eng, i)
        prev = self.dcount[eng][i]
        if prev:
            self._need(eng, key, prev, waits)
        self.dcount[eng][i] = prev + 16
        tok = (key, prev + 16)
        self._commit(tok, reads, writes)
        self.q[eng].append({"kind": "dma", "fn": fn, "waits": waits, "dsem": self.dsem[eng][i]})
        self.ninst += 1 + len(waits)
        return tok

    def barrier(self, engines=ENGS):
        toks = [(e, self.cnt[e]) for e in ENGS if self.cnt[e]]
        for e in self.dsem:
            for i in range(self.NDSEM):
                if self.dcount[e][i]:
                    toks.append(((e, i), self.dcount[e][i]))
        for eng in engines:
            waits = {}
            for k, v in toks:
                self._need(eng, k, v, waits)
            self.q[eng].append({"kind": "wait", "waits": waits})

    def emit(self):
        nc = self.nc
        sigmap = {}
        for eng in ENGS:
            cum, m = 0, []
            for rec in self.oprecs[eng]:
                if rec["sig"]:
                    cum += 1
                m.append(cum)
            sigmap[eng] = m
        self.nsig = {e: (sigmap[e][-1] if sigmap[e] else 0) for e in ENGS}

        def replay(eng, e):
            sem = self.sem[eng]
            for rec in self.q[eng]:
                for k, v in rec["waits"].items():
                    if isinstance(k, str):
                        e.wait_ge(self.sem[k], sigmap[k][v - 1])
                    else:
                        e.wait_ge(self.dsem[k[0]][k[1]], v)
                if rec["kind"] == "op":
                    ins = rec["fn"](e)
                    if rec["sig"]:
                        ins.then_inc(sem, 1)
                elif rec["kind"] == "dma":
                    rec["fn"](e).then_inc(rec["dsem"], 16)

        with nc.Block() as block:
            @block.sync
            def _(e):
                replay("sp", e)

            @block.tensor
            def _(e):
                replay("pe", e)

            @block.scalar
            def _(e):
                replay("act", e)

            @block.vector
            def _(e):
                replay("dve", e)

            @block.gpsimd
            def _(e):
                replay("pool", e)


class Arena:
    def __init__(self, nc, es, nbytes):
        self.t = es.enter_context(nc.sbuf_tensor("arena", [128, nbytes], U8))
        self.off = 0
        self.cap = nbytes
        self.peak = 0

    def alloc(self, shape, dt):
        esz = {F32: 4, BF16: 2, I32: 4, U8: 1}[dt]
        n = esz
        for s in shape:
            n *= s
        off = (self.off + 63) // 64 * 64
        assert off + n <= self.cap, ("arena overflow", off, n, self.cap)
        self.off = off + n
        self.peak = max(self.peak, self.off)
        ap = self.t[:, off:off + n]
        if dt != U8:
            ap = ap.bitcast(dt)
        if len(shape) == 2:
            ap = ap.rearrange("p (a b) -> p a b", b=shape[1])
        elif len(shape) == 3:
            ap = ap.rearrange("p (a b c) -> p a b c", b=shape[1], c=shape[2])
        return ap

    def mark(self):
        return self.off

    def release(self, m):
        self.off = m


def build_nc(debug=None, stop=None, moe=True):
    nc = bass.Bass("TRN2", target_bir_lowering=False)
    dram = lambda name, shape, dt, kind="ExternalInput": nc.dram_tensor(name, list(shape), dt, kind=kind).ap()
    x = dram("x", [S_LEN, D], F32)
    w_in = dram("w_in", [D, D_IN], F32)
    convw = dram("convw", [128, 64], F32)
    gmixT = dram("gmixT", [128, 16], F32)
    gbias = dram("gbias", [128, 8], F32)
    gml = dram("gml", [128, 1024], F32)
    gqk = dram("gqk", [128, 2], F32)
    sinks = dram("sinks", [128, 16], F32)
    w_pm = dram("w_proj_m", [1024, D], F32)
    w_pa = dram("w_proj_a", [1024, D], F32)
    w_out = dram("w_out", [D, D], F32)
    gffn = dram("gffn", [128, D], F32)
    wroute = dram("wroute", [D, 72], F32)
    broute = dram("broute", [128, 72], F32)
    if moe:
        w_gate = dram("w_gate", [N_EXP, D, D_EXP], F32)
        w_up = dram("w_up", [N_EXP, D, D_EXP], F32)
        w_down = dram("w_down", [N_EXP, D_EXP, D], F32)
    cmat = dram("cmat", [128, 6, 128], F32)
    ctab = dram("ctab", [128, 2, S_LEN], F32)
    eoff = dram("eoff", [128, 64], F32)
    out = dram("out", [S_LEN, D], F32, kind="ExternalOutput")
    x1s = dram("x1s", [S_LEN, D], F32, kind="Internal")
    xr = dram("xr", [N_EXP * 128, D], BF16, kind="Internal")
    yr = dram("yr", [N_EXP * 128, D], BF16, kind="Internal")
    dbg = {}
    if debug:
        for name, shape, dt in debug:
            dbg[name] = dram(name, shape, dt, kind="ExternalOutput")

    if "d_x1" in dbg:
        x1s = dbg["d_x1"]
    with ExitStack() as es:
        S = Sched(nc, es)
        A = Arena(nc, es, 206 * 1024)
        PSA = es.enter_context(nc.psum_tensor("psA", [128, 8, 512], F32))
        ps = lambda b: PSA[:, b, :]
        psb = lambda b: PSA[:, b, :].bitcast(BF16)
        PK = lambda b: "ps%d" % b
        out_toks = []

        def OP(eng, fn, reads=(), writes=()):
            return S.op(eng, fn, reads, writes)

        def dbg_dump(name, src_ap, reads, dst=None):
            if name in dbg:
                d = dbg[name] if dst is None else dst
                out_toks.append(S.dma("sp", lambda e: e.dma_start(out=d, in_=src_ap), reads=reads, writes=["dbg." + name + str(len(out_toks))]))

        cm_f = A.alloc((6, 128), F32)
        cm_b = A.alloc((6, 128), BF16)
        ones_f = A.alloc((128,), F32)
        ones_b = A.alloc((128,), BF16)
        convw_t = A.alloc((16, 4), F32)
        gmixT_t = A.alloc((16,), F32)
        gbias_t = A.alloc((8,), F32)
        gqk_t = A.alloc((2,), F32)
        esk_t = A.alloc((16,), F32)
        ident_f, tri_f, stri_f = cm_f[:, 0, :], cm_f[:, 1, :], cm_f[:, 2, :]
        ident_b, tri_b, stri_b, swap_b, bd_b, gt_b = (cm_b[:, i, :] for i in range(6))
        S.dma("sp", lambda e: e.dma_start(out=cm_f, in_=cmat), writes=["cm_f"])
        S.dma("sp", lambda e: e.dma_start(out=convw_t, in_=convw.rearrange("p (c j) -> p c j", j=4)), writes=["convw"])
        S.dma("sp", lambda e: e.dma_start(out=gmixT_t, in_=gmixT), writes=["gmixT"])
        S.dma("sp", lambda e: e.dma_start(out=gbias_t, in_=gbias), writes=["gbias"])
        S.dma("sp", lambda e: e.dma_start(out=gqk_t, in_=gqk), writes=["gqk"])
        S.dma("sp", lambda e: e.dma_start(out=esk_t, in_=sinks), writes=["esk"])
        OP("dve", lambda e: e.tensor_copy(cm_b, cm_f), reads=["cm_f"], writes=["cm_b"])
        OP("dve", lambda e: e.memset(ones_f, 1.0), writes=["ones_f"])
        OP("dve", lambda e: e.memset(ones_b, 1.0), writes=["ones_b"])
        OP("act", lambda e: e.activation(esk_t, esk_t, AF.Exp), reads=["esk"], writes=["esk"])
        CONST_R = ["cm_f", "cm_b", "ones_f", "ones_b", "convw", "gmixT", "gbias", "gqk", "esk"]

        p1_mark = A.mark()
        XT = [A.alloc((D,), F32) for _ in range(2)]
        zt = XT[1].bitcast(BF16)
        xr_z = xr.rearrange("(a p c) d -> a p (c d)", p=128, c=2)
        XR_Z = ["xr.z%d" % a for a in range(32)]

        def zero_fill():
            OP("dve", lambda e: e.memset(zt, 0.0), writes=["xt1"])
            for a in range(32):
                S.dma("sp", lambda e, a=a: e.dma_start(out=xr_z[a], in_=zt), reads=["xt1"], writes=["xr.z%d" % a])
        gml_t = A.alloc((1024,), F32)
        S.dma("sp", lambda e: e.dma_start(out=gml_t, in_=gml), writes=["gml"])
        hb = A.alloc((D,), BF16)
        hT = A.alloc((16, TB), BF16)
        NW = 6
        WB = [A.alloc((8192,), U8) for _ in range(NW)]
        wstate = {"i": 0}
        ss_t = A.alloc((NT,), F32)
        rs_t = A.alloc((NT,), F32)
        ZC = [A.alloc((TB + 3,), F32) for _ in range(2)]
        ACC = [A.alloc((TB,), F32) for _ in range(2)]
        hal = A.alloc((16, 3), F32)
        mqkT = A.alloc((16, TB), BF16)
        v_ext = A.alloc((4, 4, 258), BF16)
        gs = A.alloc((4, 1024), BF16)
        graw = A.alloc((4, 8), F32)
        tab = A.alloc((2, TB), F32)
        aqT = A.alloc((8, TB), BF16)
        akT = A.alloc((2, 128 + TB), BF16)
        avd = A.alloc((5, 4, 128), BF16)
        hmT = A.alloc((8, TB), BF16)
        haT = A.alloc((8, TB), BF16)
        Cf = A.alloc((4, 2, 257), F32)
        Cb = A.alloc((4, 2, 258), BF16)
        f16 = lambda: A.alloc((16,), F32)
        v3 = lambda t: t.rearrange("p (a b) -> p a b", b=4)
        lfn, negB, u_t, euS, euE, dec, flr, dt1, dt2, dt3, dt4, Dg = (f16() for _ in range(12))
        NB_all = A.alloc((5, 4), F32)
        U_all = A.alloc((5, 4), F32)
        cm_s = A.alloc((1,), F32)
        kp = [A.alloc((256,), BF16) for _ in range(2)]
        PT = [A.alloc((128,), BF16) for _ in range(2)]
        msc = A.alloc((4, 8), F32)
        hsq = A.alloc((256,), BF16)
        ytok = [A.alloc((1024,), BF16) for _ in range(2)]
        sqb = A.alloc((TB,), BF16)
        rstd_a = ZC[0][:, 0:TB]
        xn_a = A.alloc((TB,), BF16)
        t1_a = ACC[0]
        t2_a = ACC[1]
        PTa = [A.alloc((TB,), BF16) for _ in range(2)]
        PTb = [A.alloc((TB,), BF16) for _ in range(2)]
        rd_a = [A.alloc((4, 128), F32) for _ in range(2)]
        sg_m, sg_a = PTa, PTb
        tm1 = ACC
        tm2 = [ZC[0][:, 0:TB], ZC[1][:, 0:TB]]
        mixT = mqkT
        XP = [A.alloc((256,), F32) for _ in range(3)]
        XO = [A.alloc((256,), F32) for _ in range(3)]

        OP("dve", lambda e: e.memset(hal, 0.0), writes=["hal"])
        OP("dve", lambda e: e.memset(v_ext, 1.0), writes=["v_ext"])
        OP("dve", lambda e: e.memset(Cf, 0.0), writes=["Cf"])
        OP("dve", lambda e: e.memset(Cb, 0.0), writes=["Cb"])
        OP("dve", lambda e: e.memset(NB_all, 0.0), writes=["NB_all"])
        OP("dve", lambda e: e.memset(U_all, 0.0), writes=["U_all"])
        OP("dve", lambda e: e.memset(ss_t, 0.0), writes=["ss"])
        OP("dve", lambda e: e.memset(akT, 0.0), writes=["akT"])
        OP("dve", lambda e: e.memset(avd, 0.0), writes=["avd"])

        def load_w(src2d, kch, ncols, runs=None):
            i = wstate["i"]
            wstate["i"] = (i + 1) % NW
            key = "wb%d" % i
            assert kch * ncols * 2 <= 8192
            view = WB[i][:, 0:kch * ncols * 2].bitcast(BF16).rearrange("p (k c) -> p k c", c=ncols)
            if runs is None:
                S.dma("pool", lambda e: e.dma_start(out=view, in_=src2d.rearrange("(k p) c -> p k c", p=128)), writes=[key])
            else:
                for (dc0, src) in runs:
                    n = src.shape[1]
                    S.dma("pool", lambda e, dc0=dc0, src=src, n=n: e.dma_start(out=view[:, :, dc0:dc0 + n], in_=src.rearrange("(k p) c -> p k c", p=128)), writes=[key])
            return view, key

        pbank = {"i": 0}

        def next_bank(lo=0, n=4):
            b = lo + pbank["i"] % n
            pbank["i"] += 1
            return b

        for tb in range(NTB):
            t0 = tb * TB
            for j in range(4):
                tt = tb * 4 + j
                xt = XT[tt % 2]
                xk = "xt%d" % (tt % 2)
                S.dma("sp", lambda e, xt=xt, tt=tt: e.dma_start(out=xt, in_=x[tt * 128:(tt + 1) * 128, :]), writes=[xk])
                OP("act", lambda e, xt=xt, tt=tt: e.activation(hb, xt, AF.Square, accum_out=ss_t[:, tt:tt + 1]), reads=[xk, "ss"], writes=["hb", "ss%d" % tt])
                OP("act", lambda e, tt=tt: e.activation(rs_t[:, tt:tt + 1], ss_t[:, tt:tt + 1], AF.Ln, scale=1.0 / D, bias=EPS), reads=["ss%d" % tt], writes=["rs%d" % tt])
                OP("act", lambda e, tt=tt: e.activation(rs_t[:, tt:tt + 1], rs_t[:, tt:tt + 1], AF.Exp, scale=-0.5), reads=["rs%d" % tt], writes=["rs%d" % tt])
                OP("dve", lambda e, xt=xt, tt=tt: e.tensor_scalar(hb, xt, rs_t[:, tt:tt + 1], None, op0=ALU.mult), reads=[xk, "rs%d" % tt], writes=["hb"])
                for half in range(2):
                    b = half
                    for kk in range(8):
                        k = half * 8 + kk
                        OP("pe", lambda e, b=b, kk=kk, k=k: e.transpose(psb(b)[:, kk * 128:(kk + 1) * 128], hb[:, k * 128:(k + 1) * 128], ident_b),
                           reads=["hb", "cm_b"], writes=[PK(b)])
                    OP("dve", lambda e, b=b, half=half, j=j: e.tensor_tensor(
                        hT[:, half * 8:(half + 1) * 8, j * 128:(j + 1) * 128],
                        psb(b).rearrange("p (k t) -> p k t", t=128),
                        gmixT_t[:, half * 8:(half + 1) * 8].unsqueeze(2).to_broadcast([128, 8, 128]), op=ALU.mult),
                        reads=[PK(b), "gmixT"], writes=["hT"])
            S.dma("sp", lambda e, t0=t0: e.dma_start(out=tab, in_=ctab[:, :, t0:t0 + TB]), writes=["tab"])

            def proj_fm(wv, wk, c0, ncol_chunk=128):
                b = next_bank(0, 4)
                for k in range(16):
                    OP("pe", lambda e, b=b, k=k, c0=c0: e.matmul(ps(b), wv[:, k, c0:c0 + 128], hT[:, k, :], start=(k == 0), stop=(k == 15)),
                       reads=[wk, "hT"], writes=[PK(b)])
                return b

            def proj_tm(wv, wk, j, ncols):
                b = next_bank(0, 4)
                for k in range(16):
                    OP("pe", lambda e, b=b, k=k, j=j: e.matmul(ps(b)[:, 0:ncols], hT[:, k, j * 128:(j + 1) * 128], wv[:, k, 0:ncols], start=(k == 0), stop=(k == 15)),
                       reads=[wk, "hT"], writes=[PK(b)])
                return b

            for g8 in range(8):
                wv, wk = load_w(w_in[:, g8 * 256:(g8 + 1) * 256], 16, 256)
                for c2 in range(2):
                    cj = g8 * 2 + c2
                    b = proj_fm(wv, wk, c2 * 128)
                    zc, zk = ZC[cj % 2], "zc%d" % (cj % 2)
                    acc, ak_ = ACC[cj % 2], "acc%d" % (cj % 2)
                    OP("act", lambda e, zc=zc, cj=cj: e.activation(zc[:, 0:3], hal[:, cj, :], AF.Copy), reads=["hal%d" % cj, "hal"], writes=[zk + "h"])
                    OP("act", lambda e, zc=zc, b=b: e.activation(zc[:, 3:TB + 3], ps(b), AF.Copy), reads=[PK(b)], writes=[zk])
                    OP("act", lambda e, zc=zc, cj=cj: e.activation(hal[:, cj, :], zc[:, TB:TB + 3], AF.Copy), reads=[zk], writes=["hal%d" % cj])
                    OP("dve", lambda e, zc=zc, acc=acc, cj=cj: e.tensor_scalar(acc, zc[:, 3:TB + 3], convw_t[:, cj, 3:4], None, op0=ALU.mult),
                       reads=[zk, zk + "h", "convw"], writes=[ak_])
                    for tap in (2, 1, 0):
                        OP("dve", lambda e, zc=zc, acc=acc, cj=cj, tap=tap: e.scalar_tensor_tensor(acc, zc[:, tap:tap + TB], convw_t[:, cj, tap:tap + 1], acc, op0=ALU.mult, op1=ALU.add),
                           reads=[zk, zk + "h", ak_], writes=[ak_])
                    OP("act", lambda e, acc=acc, cj=cj: e.activation(mqkT[:, cj, :], acc, AF.Silu), reads=[ak_], writes=["mqkT%d" % cj])
            for hh in range(4):
                wv, wk = load_w(w_in[:, C_MV + hh * 256:C_MV + (hh + 1) * 256], 16, 256)
                for j in range(4):
                    b = proj_tm(wv, wk, j, 256)
                    OP("act", lambda e, b=b, j=j, hh=hh: e.activation(v_ext[:, j, hh, 0:256], ps(b)[:, 0:256], AF.Copy), reads=[PK(b)], writes=["v%d_%d" % (j, hh)])
            for hh in range(4):
                wv, wk = load_w(w_in[:, C_MO + hh * 256:C_MO + (hh + 1) * 256], 16, 256)
                for j in range(4):
                    b = proj_tm(wv, wk, j, 256)
                    OP("act", lambda e, b=b, j=j, hh=hh: e.activation(gs[:, j, hh * 256:(hh + 1) * 256], ps(b)[:, 0:256], AF.Sigmoid), reads=[PK(b)], writes=["gs%d_%d" % (j, hh)])
                    OP("dve", lambda e, j=j, hh=hh: e.tensor_tensor(gs[:, j, hh * 256:(hh + 1) * 256], gs[:, j, hh * 256:(hh + 1) * 256], gml_t[:, hh * 256:(hh + 1) * 256], op=ALU.mult),
                       reads=["gs%d_%d" % (j, hh), "gml"], writes=["gs%d_%d" % (j, hh)])
            wv, wk = load_w(w_in[:, C_G:C_G + 8], 16, 8)
            for j in range(4):
                b = proj_tm(wv, wk, j, 8)
                OP("dve", lambda e, b=b, j=j: e.tensor_tensor(graw[:, j, :], ps(b)[:, 0:8], gbias_t, op=ALU.add), reads=[PK(b), "gbias"], writes=["graw%d" % j])

            def qk_post(b, gcol, dst, dkey, dsl):
                OP("act", lambda e: e.activation(sqb, ps(b), AF.Square), reads=[PK(b)], writes=["sqb"])
                OP("pe", lambda e: e.matmul(ps(4), bd_b, sqb, start=True, stop=True), reads=["sqb", "cm_b"], writes=[PK(4)])
                OP("act", lambda e: e.activation(rstd_a, ps(4), AF.Ln, scale=1.0 / 64, bias=EPS), reads=[PK(4)], writes=["zc0", "zc0h"])
                OP("act", lambda e: e.activation(rstd_a, rstd_a, AF.Exp, scale=-0.5), reads=["zc0"], writes=["zc0", "zc0h"])
                OP("dve", lambda e: e.scalar_tensor_tensor(xn_a, ps(b), gqk_t[:, gcol:gcol + 1], rstd_a, op0=ALU.mult, op1=ALU.mult),
                   reads=[PK(b), "gqk", "zc0"], writes=["xn_a"])
                OP("pe", lambda e: e.matmul(ps(5), swap_b, xn_a, start=True, stop=True), reads=["xn_a", "cm_b"], writes=[PK(5)])
                OP("dve", lambda e: e.tensor_tensor(t1_a, xn_a, tab[:, 0, :], op=ALU.mult), reads=["xn_a", "tab"], writes=["acc0"])
                OP("dve", lambda e: e.tensor_tensor(t2_a, ps(5), tab[:, 1, :], op=ALU.mult), reads=[PK(5), "tab"], writes=["acc1"])
                OP("dve", lambda e: e.tensor_tensor(dst, t1_a, t2_a, op=ALU.add), reads=["acc0", "acc1"], writes=[dkey])

            if tb > 0:
                OP("act", lambda e: e.activation(akT[:, :, 0:128], akT[:, :, TB:TB + 128], AF.Copy), reads=["akT"], writes=["akT"])
                OP("act", lambda e: e.activation(avd[:, 0, :, :], avd[:, 4, :, :], AF.Copy), reads=["avd"], writes=["avd"])
            wv, wk = load_w(w_in[:, C_AK:C_AK + 256], 16, 256)
            for i in range(2):
                b = proj_fm(wv, wk, i * 128)
                qk_post(b, 1, akT[:, i, 128:128 + TB], "akT", None)
            wv, wk = load_w(w_in[:, C_AV:C_AV + 256], 16, 256)
            for j in range(4):
                b = proj_tm(wv, wk, j, 256)
                for dup in range(2):
                    OP("act", lambda e, b=b, j=j, dup=dup: e.activation(avd[:, 1 + j, :, dup * 64:(dup + 1) * 64], ps(b)[:, 0:256].rearrange("p (g c) -> p g c", c=64), AF.Copy),
                       reads=[PK(b)], writes=["avd"])
            for a in range(4):
                runs = []
                for c2 in range(2):
                    ci = a * 2 + c2
                    i, r = ci // 4, ci % 4
                    h0, h1 = 8 * i + r, 8 * i + 4 + r
                    runs.append((c2 * 128, w_in[:, C_AQ + h0 * 64:C_AQ + (h0 + 1) * 64]))
                    runs.append((c2 * 128 + 64, w_in[:, C_AQ + h1 * 64:C_AQ + (h1 + 1) * 64]))
                wv, wk = load_w(None, 16, 256, runs=runs)
                for c2 in range(2):
                    ci = a * 2 + c2
                    b = proj_fm(wv, wk, c2 * 128)
                    qk_post(b, 0, aqT[:, ci, :], "aqT%d" % ci, None)

            if "d_mqkT" in dbg:
                dbg_dump("d_mqkT", mqkT, ["mqkT%d" % c for c in range(16)], dst=dbg["d_mqkT"][:, :, t0:t0 + TB])
            if "d_v" in dbg:
                dbg_dump("d_v", v_ext, ["v%d_%d" % (j, hh) for j in range(4) for hh in range(4)], dst=dbg["d_v"][:, tb * 4:(tb + 1) * 4, :, :])
            if "d_gs" in dbg:
                dbg_dump("d_gs", gs, ["gs%d_%d" % (j, hh) for j in range(4) for hh in range(4)], dst=dbg["d_gs"][:, tb * 4:(tb + 1) * 4, :])
            if "d_graw" in dbg:
                dbg_dump("d_graw", graw, ["graw%d" % j for j in range(4)], dst=dbg["d_graw"][:, tb * 4:(tb + 1) * 4, :])
            if "d_aqT" in dbg:
                dbg_dump("d_aqT", aqT, ["aqT%d" % c for c in range(8)], dst=dbg["d_aqT"][:, :, t0:t0 + TB])
            if "d_akT" in dbg:
                dbg_dump("d_akT", akT[:, :, 128:128 + TB], ["akT"], dst=dbg["d_akT"][:, :, t0:t0 + TB])
            if stop == "1b":
                continue

            if tb == 0:
                zero_fill()
            GR = ["graw%d" % j for j in range(4)]
            OP("act", lambda e: e.activation(v3(lfn), graw[:, :, 4:8], AF.Exp, scale=-1.0), reads=GR, writes=["lfn"])
            OP("act", lambda e: e.activation(lfn, lfn, AF.Ln, bias=1.0), reads=["lfn"], writes=["lfn"])
            OP("pe", lambda e: e.matmul(ps(4)[:, 0:16], tri_f, lfn, start=True, stop=True), reads=["lfn", "cm_f"], writes=[PK(4)])
            OP("pe", lambda e: e.matmul(ps(4)[:, 16:32], ones_f, lfn, start=True, stop=True), reads=["lfn", "ones_f"], writes=[PK(4)])
            if tb > 0:
                OP("dve", lambda e: e.tensor_copy(NB_all[:, 0, :], NB_all[:, 4, :]), reads=["NB_all"], writes=["NB_all"])
                OP("dve", lambda e: e.tensor_copy(U_all[:, 0, :], U_all[:, 4, :]), reads=["U_all"], writes=["U_all"])
            for j in range(4):
                OP("dve", lambda e, j=j: e.tensor_tensor(NB_all[:, j + 1, :], NB_all[:, j, :], ps(4)[:, 16 + j * 4:20 + j * 4], op=ALU.add), reads=["NB_all", PK(4)], writes=["NB_all"])
            OP("dve", lambda e: e.tensor_tensor(v3(negB), v3(ps(4)[:, 0:16]), NB_all[:, 0:4, :], op=ALU.add), reads=[PK(4), "NB_all"], writes=["negB"])
            OP("dve", lambda e: e.tensor_tensor(v3(u_t), graw[:, :, 0:4], v3(negB), op=ALU.add), reads=GR + ["negB"], writes=["u_t"])
            OP("pe", lambda e: e.transpose(ps(4)[0:16, 64:192], u_t, ident_f), reads=["u_t", "cm_f"], writes=[PK(4)])
            OP("dve", lambda e: e.reduce_max(cm_s[0:16, :], ps(4)[0:16, 64:192], axis=AX.X), reads=[PK(4)], writes=["cm_s"])
            OP("dve", lambda e: e.tensor_scalar(Dg[0:16, :], ident_f[0:16, 0:16], cm_s[0:16, 0:1], None, op0=ALU.mult), reads=["cm_s", "cm_f"], writes=["Dg"])
            OP("pe", lambda e: e.matmul(ps(4)[:, 32:48], ones_f[0:16, :], Dg[0:16, :], start=True, stop=True), reads=["Dg", "ones_f"], writes=[PK(4)])
            for j in range(4):
                OP("dve", lambda e, j=j: e.tensor_tensor(U_all[:, j + 1, :], U_all[:, j, :], ps(4)[:, 32 + j * 4:36 + j * 4], op=ALU.max), reads=["U_all", PK(4)], writes=["U_all"])
            Us, Ue = U_all[:, 0:4, :], U_all[:, 1:5, :]
            OP("dve", lambda e: e.scalar_tensor_tensor(v3(dt1), v3(u_t), -LN16, Us, op0=ALU.add, op1=ALU.subtract), reads=["u_t", "U_all"], writes=["dt1"])
            OP("act", lambda e: e.activation(euS, dt1, AF.Exp), reads=["dt1"], writes=["euS"])
            OP("dve", lambda e: e.scalar_tensor_tensor(v3(dt2), v3(u_t), -LN16, Ue, op0=ALU.add, op1=ALU.subtract), reads=["u_t", "U_all"], writes=["dt2"])
            OP("act", lambda e: e.activation(euE, dt2, AF.Exp), reads=["dt2"], writes=["euE"])
            OP("dve", lambda e: e.tensor_tensor(v3(dt3), Us, Ue, op=ALU.subtract), reads=["U_all"], writes=["dt3"])
            OP("act", lambda e: e.activation(dec, dt3, AF.Exp), reads=["dt3"], writes=["dec"])
            OP("dve", lambda e: e.tensor_tensor(v3(dt4), v3(negB), Us, op=ALU.subtract), reads=["negB", "U_all"], writes=["dt4"])
            OP("act", lambda e: e.activation(flr, dt4, AF.Exp), reads=["dt4"], writes=["flr"])

            def mlstm_unit(j, h):
                yt_, yk = ytok[j % 2], "ytok%d" % (j % 2)
                tsl = slice(j * 128, (j + 1) * 128)
                col = j * 4 + h
                qc = (2 * h, 2 * h + 1)
                kc = (8 + 2 * h, 9 + 2 * h)
                QR = ["mqkT%d" % c for c in qc]
                KR = ["mqkT%d" % c for c in kc]
                VK = "v%d_%d" % (j, h)
                kp_, kpk = kp[h % 2], "kp%d" % (h % 2)
                PT_, ptk = PT[h % 2], "PT%d" % (h % 2)
                nb = 2 + (h % 2)
                for half in range(2):
                    OP("pe", lambda e, half=half: e.transpose(psb(0)[:, half * 128:(half + 1) * 128], mqkT[:, kc[half], tsl], ident_b),
                       reads=[KR[half], "cm_b"], writes=[PK(0)])
                OP("act", lambda e: e.activation(kp_, psb(0)[:, 0:256], AF.Copy, scale=euE[:, col:col + 1]), reads=[PK(0), "euE"], writes=[kpk])
                for half in range(2):
                    OP("pe", lambda e, half=half: e.matmul(ps(1)[:, 0:128], mqkT[:, kc[half], tsl], mqkT[:, qc[half], tsl], start=(half == 0), stop=(half == 1)),
                       reads=[KR[half], QR[half]], writes=[PK(1)])
                OP("dve", lambda e: e.scalar_tensor_tensor(PT_, ps(1)[:, 0:128], euS[:, col:col + 1], tri_b, op0=ALU.mult, op1=ALU.mult),
                   reads=[PK(1), "euS", "cm_b"], writes=[ptk])
                for half in range(2):
                    OP("pe", lambda e, half=half: e.matmul(ps(nb)[:, 0:257], mqkT[:, qc[half], tsl], Cb[:, h, half, 0:257], start=(half == 0), stop=False),
                       reads=[QR[half], "Cb%d" % h], writes=[PK(nb)])
                OP("pe", lambda e: e.matmul(ps(nb)[:, 0:257], PT_, v_ext[:, j, h, 0:257], start=False, stop=True),
                   reads=[ptk, VK, "v_ext"], writes=[PK(nb)])
                for half in range(2):
                    OP("pe", lambda e, half=half: e.matmul(PSA[:, 4 + half, 0:257], kp_[:, half * 128:(half + 1) * 128], v_ext[:, j, h, 0:257], start=True, stop=True),
                       reads=[kpk, VK, "v_ext"], writes=[PK(4 + half)])
                OP("dve", lambda e: e.scalar_tensor_tensor(Cf[:, h, :, :], Cf[:, h, :, :], dec[:, col:col + 1], PSA[:, 4:6, 0:257], op0=ALU.mult, op1=ALU.add),
                   reads=["Cf", "Cf%d" % h, "dec", PK(4), PK(5)], writes=["Cf%d" % h])
                OP("act", lambda e: e.activation(Cb[:, h, :, 0:257], Cf[:, h, :, :], AF.Copy), reads=["Cf%d" % h, "Cb"], writes=["Cb%d" % h])
                m = msc[:, h, :]
                mk_ = "msc%d" % h
                OP("act", lambda e: e.activation(m[:, 0:1], ps(nb)[:, 256:257], AF.Abs), reads=[PK(nb)], writes=[mk_])
                OP("dve", lambda e: e.tensor_scalar(m[:, 0:1], m[:, 0:1], flr[:, col:col + 1], None, op0=ALU.max), reads=[mk_, "flr"], writes=[mk_])
                OP("dve", lambda e: e.reciprocal(m[:, 1:2], m[:, 0:1]), reads=[mk_], writes=[mk_])
                OP("dve", lambda e: e.memset(m[:, 2:3], 0.0), reads=[mk_], writes=[mk_])
                OP("act", lambda e: e.activation(hsq, ps(nb)[:, 0:256], AF.Square, scale=m[:, 1:2], accum_out=m[:, 2:3]), reads=[PK(nb), mk_], writes=[mk_, "hsq"])
                OP("act", lambda e: e.activation(m[:, 3:4], m[:, 2:3], AF.Ln, scale=1.0 / 256, bias=EPS), reads=[mk_], writes=[mk_])
                OP("act", lambda e: e.activation(m[:, 3:4], m[:, 3:4], AF.Exp, scale=-0.5), reads=[mk_], writes=[mk_])
                OP("dve", lambda e: e.tensor_tensor(m[:, 4:5], m[:, 1:2], m[:, 3:4], op=ALU.mult), reads=[mk_], writes=[mk_])
                OP("dve", lambda e: e.scalar_tensor_tensor(yt_[:, h * 256:(h + 1) * 256], ps(nb)[:, 0:256], m[:, 4:5], gs[:, j, h * 256:(h + 1) * 256], op0=ALU.mult, op1=ALU.mult),
                   reads=[PK(nb), mk_, "gs%d_%d" % (j, h)], writes=[yk])

            def mlstm_tile_end(j):
                yt_, yk = ytok[j % 2], "ytok%d" % (j % 2)
                tsl = slice(j * 128, (j + 1) * 128)
                for c in range(8):
                    OP("pe", lambda e, c=c: e.transpose(psb(0)[:, c * 128:(c + 1) * 128], yt_[:, c * 128:(c + 1) * 128], ident_b), reads=[yk, "cm_b"], writes=[PK(0)])
                OP("act", lambda e: e.activation(hmT[:, :, tsl], psb(0).rearrange("p (c t) -> p c t", t=128), AF.Copy), reads=[PK(0)], writes=["hmT"])

            AQR = ["aqT%d" % c for c in range(8)]

            def swa_unit(j, g):
                n = tb * 4 + j
                tsl = slice(j * 128, (j + 1) * 128)
                i, hf = g // 2, g % 2
                pr = slice(hf * 64, hf * 64 + 64)
                si = (j * 4 + g) % 2
                has_prev = n > 0
                pa_, pb_, rd = PTa[si], PTb[si], rd_a[si]
                pak, pbk, rdk = "PTa%d" % si, "PTb%d" % si, "rd%d" % si
                qv = aqT[pr, 4 * i:4 * i + 4, tsl]
                m3 = lambda t: t.rearrange("p (r q) -> p r q", q=128)
                if has_prev:
                    OP("pe", lambda e: e.matmul(ps(1), akT[pr, i, j * 128:(j + 1) * 128], qv, start=True, stop=True), reads=["akT"] + AQR, writes=[PK(1)])
                    OP("act", lambda e: e.activation(pa_, ps(1), AF.Exp, scale=0.125), reads=[PK(1)], writes=[pak])
                    OP("dve", lambda e: e.tensor_tensor(m3(pa_), m3(pa_), gt_b.unsqueeze(1).to_broadcast([128, 4, 128]), op=ALU.mult), reads=[pak, "cm_b"], writes=[pak])
                OP("pe", lambda e: e.matmul(ps(1), akT[pr, i, (j + 1) * 128:(j + 2) * 128], qv, start=True, stop=True), reads=["akT"] + AQR, writes=[PK(1)])
                OP("act", lambda e: e.activation(pb_, ps(1), AF.Exp, scale=0.125), reads=[PK(1)], writes=[pbk])
                OP("dve", lambda e: e.tensor_tensor(m3(pb_), m3(pb_), tri_b.unsqueeze(1).to_broadcast([128, 4, 128]), op=ALU.mult), reads=[pbk, "cm_b"], writes=[pbk])
                for (bk, lhs_fn, lk) in ((6, lambda jj: avd[:, jj, g, :], "avd"), (7, lambda jj: ones_b, "ones_b")):
                    if has_prev:
                        OP("pe", lambda e, bk=bk, lhs_fn=lhs_fn: e.matmul(ps(bk), lhs_fn(j), pa_, start=True, stop=False), reads=[lk, pak], writes=[PK(bk)])
                    OP("pe", lambda e, bk=bk, lhs_fn=lhs_fn: e.matmul(ps(bk), lhs_fn(j + 1), pb_, start=(not has_prev), stop=True), reads=[lk, pbk], writes=[PK(bk)])
                for r in range(4):
                    OP("act", lambda e, r=r: e.activation(rd[:, r, :], ps(7)[:, r * 128:(r + 1) * 128], AF.Ln, bias=esk_t[:, g * 4 + r:g * 4 + r + 1]), reads=[PK(7), "esk"], writes=[rdk])
                OP("act", lambda e: e.activation(rd, rd, AF.Exp, scale=-1.0), reads=[rdk], writes=[rdk])
                for par in range(2):
                    hp_ = slice(par * 64, par * 64 + 64)
                    OP("dve", lambda e, hp_=hp_, par=par: e.tensor_tensor(haT[hp_, 2 * g:2 * g + 2, tsl], m3(ps(6))[hp_, par::2, :], rd[hp_, par::2, :], op=ALU.mult),
                       reads=[PK(6), rdk], writes=["haT"])

            for j in range(4):
                import os as _os2
                _mm = _os2.environ.get("K_MIX", "both")
                for idx in range(4):
                    if _mm in ("both", "m"):
                        mlstm_unit(j, idx)
                    if _mm in ("both", "s"):
                        swa_unit(j, idx)
                if _mm in ("both", "m"):
                    mlstm_tile_end(j)
            if "d_hmT" in dbg:
                dbg_dump("d_hmT", hmT, ["hmT"], dst=dbg["d_hmT"][:, :, t0:t0 + TB])
            if "d_haT" in dbg:
                dbg_dump("d_haT", haT, ["haT"], dst=dbg["d_haT"][:, :, t0:t0 + TB])
            if stop in ("1c", "1d"):
                continue

            for pc in range(8):
                wgm, kgm = load_w(w_in[:, C_GM + pc * 256:C_GM + (pc + 1) * 256], 16, 256)
                wga, kga = load_w(w_in[:, C_GA + pc * 256:C_GA + (pc + 1) * 256], 16, 256)
                wpm, kpm = load_w(w_pm[:, pc * 256:(pc + 1) * 256], 8, 256)
                wpa, kpa = load_w(w_pa[:, pc * 256:(pc + 1) * 256], 8, 256)
                for c2 in range(2):
                    c = pc * 2 + c2
                    si = c % 2
                    bo = 4 * si
                    cs_ = slice(c2 * 128, (c2 + 1) * 128)
                    for (bk, wv_, wk_, src, skey, nk) in ((bo, wgm, kgm, hT, "hT", 16), (bo + 1, wga, kga, hT, "hT", 16),
                                                        (bo + 2, wpm, kpm, hmT, "hmT", 8), (bo + 3, wpa, kpa, haT, "haT", 8)):
                        for k in range(nk):
                            OP("pe", lambda e, bk=bk, wv_=wv_, k=k, cs_=cs_, src=src, nk=nk: e.matmul(ps(bk), wv_[:, k, cs_], src[:, k, :], start=(k == 0), stop=(k == nk - 1)),
                               reads=[wk_, skey], writes=[PK(bk)])
                    OP("act", lambda e, si=si, bo=bo: e.activation(sg_m[si], ps(bo), AF.Sigmoid), reads=[PK(bo)], writes=["PTa%d" % si])
                    OP("act", lambda e, si=si, bo=bo: e.activation(sg_a[si], ps(bo + 1), AF.Sigmoid), reads=[PK(bo + 1)], writes=["PTb%d" % si])
                    OP("dve", lambda e, si=si, bo=bo: e.tensor_tensor(tm1[si], sg_m[si], ps(bo + 2), op=ALU.mult), reads=["PTa%d" % si, PK(bo + 2)], writes=["acc%d" % si])
                    OP("dve", lambda e, si=si, bo=bo: e.tensor_tensor(tm2[si], sg_a[si], ps(bo + 3), op=ALU.mult), reads=["PTb%d" % si, PK(bo + 3)], writes=["zc%d" % si, "zc%dh" % si])
                    OP("dve", lambda e, si=si, c=c: e.tensor_tensor(mixT[:, c, :], tm1[si], tm2[si], op=ALU.add), reads=["acc%d" % si, "zc%d" % si], writes=["mqkT%d" % c])

            MXR = ["mqkT%d" % c for c in range(16)]
            for n8 in range(8):
                wo, wok = load_w(w_out[:, n8 * 256:(n8 + 1) * 256], 16, 256)
                for j in range(4):
                    tt = tb * 4 + j
                    b = next_bank(0, 8)
                    xi = (n8 * 4 + j) % 3
                    for k in range(16):
                        OP("pe", lambda e, b=b, k=k, j=j, wo=wo: e.matmul(ps(b)[:, 0:256], mixT[:, k, j * 128:(j + 1) * 128], wo[:, k, :], start=(k == 0), stop=(k == 15)),
                           reads=[wok] + MXR, writes=[PK(b)])
                    S.dma("sp", lambda e, xi=xi, tt=tt, n8=n8: e.dma_start(out=XP[xi], in_=x[tt * 128:(tt + 1) * 128, n8 * 256:(n8 + 1) * 256]), writes=["xp%d" % xi])
                    OP("dve", lambda e, xi=xi, b=b: e.tensor_tensor(XO[xi], ps(b)[:, 0:256], XP[xi], op=ALU.add), reads=[PK(b), "xp%d" % xi], writes=["xo%d" % xi])
                    tk = S.dma("sp", lambda e, xi=xi, tt=tt, n8=n8: e.dma_start(out=x1s[tt * 128:(tt + 1) * 128, n8 * 256:(n8 + 1) * 256], in_=XO[xi]), reads=["xo%d" % xi], writes=["x1s.%d.%d" % (tt, n8)])
                    if "d_x1" in dbg:
                        out_toks.append(tk)


        if stop in (None, "R", "E"):
            S.barrier()
            A.release(p1_mark)
            dest_i = A.alloc((NT, 2), I32)
            gate_t = A.alloc((NT, 2), F32)
            r_mark = A.mark()
            gffn_t = A.alloc((D,), F32)
            wr_t = A.alloc((16, 72), F32)
            br_t = A.alloc((72,), F32)
            eoff_t = A.alloc((64,), F32)
            cnt = A.alloc((64,), F32)
            ss2 = A.alloc((NT,), F32)
            rs2 = A.alloc((NT,), F32)
            X1T = [A.alloc((D,), F32) for _ in range(2)]
            HFB = [A.alloc((D,), BF16) for _ in range(2)]
            hfT = A.alloc((16, 128), F32)
            lg = A.alloc((72,), F32)
            sm = A.alloc((16,), F32)
            goh = A.alloc((8,), F32)
            gex = A.alloc((8,), F32)
            esel = A.alloc((8,), F32)
            esel2 = A.alloc((8,), F32)
            oh = [A.alloc((8,), F32) for _ in range(2)]
            t64 = A.alloc((64,), F32)
            Ek = [A.alloc((64,), F32) for _ in range(2)]
            Mb = A.alloc((64,), BF16)
            posb = A.alloc((64,), F32)
            dstf = A.alloc((2,), F32)
            S.dma("sp", lambda e: e.dma_start(out=gffn_t, in_=gffn), writes=["gffn"])
            S.dma("sp", lambda e: e.dma_start(out=wr_t, in_=wroute.rearrange("(k p) c -> p k c", p=128)), writes=["wr"])
            S.dma("sp", lambda e: e.dma_start(out=br_t, in_=broute), writes=["br"])
            S.dma("sp", lambda e: e.dma_start(out=eoff_t, in_=eoff), writes=["eoff"])
            OP("dve", lambda e: e.memset(cnt, 0.0), writes=["cnt"])
            OP("dve", lambda e: e.memset(ss2, 0.0), writes=["ss2"])
            g3 = lambda t: t.rearrange("p (g e) -> p g e", e=8)
            scat_keys = []
            for tt in range(NT):
                xt, xk = X1T[tt % 2], "x1t%d" % (tt % 2)
                hfb, hk = HFB[tt % 2], "hfb%d" % (tt % 2)
                S.dma("sp", lambda e, xt=xt, tt=tt: e.dma_start(out=xt, in_=x1s[tt * 128:(tt + 1) * 128, :]), reads=["x1s.%d.%d" % (tt, n8) for n8 in range(8)], writes=[xk])
                OP("act", lambda e, xt=xt, tt=tt, hfb=hfb: e.activation(hfb, xt, AF.Square, accum_out=ss2[:, tt:tt + 1]), reads=[xk, "ss2"], writes=[hk, "ss2_%d" % tt])
                OP("act", lambda e, tt=tt: e.activation(rs2[:, tt:tt + 1], ss2[:, tt:tt + 1], AF.Ln, scale=1.0 / D, bias=EPS), reads=["ss2_%d" % tt], writes=["rs2_%d" % tt])
                OP("act", lambda e, tt=tt: e.activation(rs2[:, tt:tt + 1], rs2[:, tt:tt + 1], AF.Exp, scale=-0.5), reads=["rs2_%d" % tt], writes=["rs2_%d" % tt])
                OP("dve", lambda e, xt=xt, tt=tt: e.scalar_tensor_tensor(xt, xt, rs2[:, tt:tt + 1], gffn_t, op0=ALU.mult, op1=ALU.mult), reads=[xk, "rs2_%d" % tt, "gffn"], writes=[xk])
                OP("act", lambda e, xt=xt, hfb=hfb: e.activation(hfb, xt, AF.Copy), reads=[xk], writes=[hk])
                for q4 in range(4):
                    for kk in range(4):
                        k = q4 * 4 + kk
                        OP("pe", lambda e, q4=q4, kk=kk, k=k, xt=xt: e.transpose(ps(q4)[:, kk * 128:(kk + 1) * 128], xt[:, k * 128:(k + 1) * 128], ident_f), reads=[xk, "cm_f"], writes=[PK(q4)])
                    eng = "act" if q4 % 2 == 0 else "dve"
                    if eng == "act":
                        OP("act", lambda e, q4=q4: e.activation(hfT[:, q4 * 4:(q4 + 1) * 4, :], ps(q4).rearrange("p (k t) -> p k t", t=128), AF.Copy), reads=[PK(q4)], writes=["hfT%d" % q4])
                    else:
                        OP("dve", lambda e, q4=q4: e.tensor_copy(hfT[:, q4 * 4:(q4 + 1) * 4, :], ps(q4).rearrange("p (k t) -> p k t", t=128)), reads=[PK(q4)], writes=["hfT%d" % q4])
                for k in range(16):
                    OP("pe", lambda e, k=k: e.matmul(ps(4)[:, 0:72], hfT[:, k, :], wr_t[:, k, :], start=(k == 0), stop=(k == 15)), reads=["hfT%d" % (k // 4), "wr"], writes=[PK(4)])
                OP("dve", lambda e: e.tensor_tensor(lg, ps(4)[:, 0:72], br_t, op=ALU.add), reads=[PK(4), "br"], writes=["lg"])
                OP("dve", lambda e: e.reduce_max(sm[:, 0:1], lg[:, 0:8], axis=AX.X), reads=["lg"], writes=["sm0"])
                OP("dve", lambda e: e.tensor_scalar(goh, lg[:, 0:8], sm[:, 0:1], None, op0=ALU.is_equal), reads=["lg", "sm0"], writes=["goh"])
                OP("dve", lambda e: e.tensor_scalar(sm[:, 1:2], sm[:, 0:1], -1.0, None, op0=ALU.mult), reads=["sm0"], writes=["sm1"])
                OP("dve", lambda e: e.memset(sm[:, 2:3], 0.0), writes=["sm2"])
                OP("act", lambda e: e.activation(gex, lg[:, 0:8], AF.Exp, bias=sm[:, 1:2], accum_out=sm[:, 2:3]), reads=["lg", "sm1", "sm2"], writes=["gex", "sm2"])
                OP("dve", lambda e: e.reciprocal(sm[:, 3:4], sm[:, 2:3]), reads=["sm2"], writes=["sm3"])
                OP("dve", lambda e: e.tensor_tensor(g3(t64), g3(lg[:, 8:72]), goh.unsqueeze(2).to_broadcast([128, 8, 8]), op=ALU.mult), reads=["lg", "goh"], writes=["t64"])
                OP("dve", lambda e: e.reduce_sum(esel, t64.rearrange("p (g e) -> p e g", e=8), axis=AX.X), reads=["t64"], writes=["esel"])
                OP("dve", lambda e: e.reduce_max(sm[:, 4:5], esel, axis=AX.X), reads=["esel"], writes=["sm4"])
                OP("dve", lambda e: e.tensor_scalar(oh[0], esel, sm[:, 4:5], None, op0=ALU.is_equal), reads=["esel", "sm4"], writes=["oh0"])
                OP("dve", lambda e: e.scalar_tensor_tensor(esel2, oh[0], -1e30, esel, op0=ALU.mult, op1=ALU.add), reads=["oh0", "esel"], writes=["esel2"])
                OP("dve", lambda e: e.reduce_max(sm[:, 5:6], esel2, axis=AX.X), reads=["esel2"], writes=["sm5"])
                OP("dve", lambda e: e.tensor_scalar(oh[1], esel2, sm[:, 5:6], None, op0=ALU.is_equal), reads=["esel2", "sm5"], writes=["oh1"])
                OP("dve", lambda e: e.tensor_tensor(sm[:, 6:7], sm[:, 4:5], sm[:, 5:6], op=ALU.subtract), reads=["sm4", "sm5"], writes=["sm6"])
                OP("act", lambda e: e.activation(sm[:, 7:8], sm[:, 6:7], AF.Sigmoid), reads=["sm6"], writes=["sm7"])
                OP("dve", lambda e: e.tensor_scalar(sm[:, 8:9], sm[:, 7:8], -1.0, 1.0, op0=ALU.mult, op1=ALU.add), reads=["sm7"], writes=["sm8"])
                OP("dve", lambda e, tt=tt: e.tensor_tensor(gate_t[:, tt, 0:1], sm[:, 7:8], sm[:, 3:4], op=ALU.mult), reads=["sm7", "sm3"], writes=["gate%d" % tt])
                OP("dve", lambda e, tt=tt: e.tensor_tensor(gate_t[:, tt, 1:2], sm[:, 8:9], sm[:, 3:4], op=ALU.mult), reads=["sm8", "sm3", "gate%d" % tt], writes=["gate%d" % tt])
                for kk in range(2):
                    OP("dve", lambda e, kk=kk: e.tensor_tensor(g3(Ek[kk]), goh.unsqueeze(2).to_broadcast([128, 8, 8]), oh[kk].unsqueeze(1).to_broadcast([128, 8, 8]), op=ALU.mult),
                       reads=["goh", "oh%d" % kk], writes=["Ek%d" % kk])
                OP("dve", lambda e: e.tensor_tensor(Mb, Ek[0], Ek[1], op=ALU.add), reads=["Ek0", "Ek1"], writes=["Mb"])
                OP("pe", lambda e: e.matmul(ps(5)[:, 0:64], stri_b, Mb, start=True, stop=True), reads=["Mb", "cm_b"], writes=[PK(5)])
                OP("pe", lambda e: e.matmul(ps(5)[:, 64:128], ones_b, Mb, start=True, stop=True), reads=["Mb", "ones_b"], writes=[PK(5)])
                OP("dve", lambda e: e.tensor_tensor(posb, ps(5)[:, 0:64], cnt, op=ALU.add), reads=[PK(5), "cnt"], writes=["posb"])
                OP("dve", lambda e: e.tensor_tensor(posb, posb, eoff_t, op=ALU.add), reads=["posb", "eoff"], writes=["posb"])
                OP("dve", lambda e: e.tensor_tensor(cnt, cnt, ps(5)[:, 64:128], op=ALU.add), reads=["cnt", PK(5), "posb"], writes=["cnt"])
                for kk in range(2):
                    OP("dve", lambda e, kk=kk: e.tensor_tensor(t64, Ek[kk], posb, op=ALU.mult), reads=["Ek%d" % kk, "posb"], writes=["t64"])
                    OP("dve", lambda e, kk=kk: e.reduce_sum(dstf[:, kk:kk + 1], t64, axis=AX.X), reads=["t64"], writes=["dstf%d" % kk])
                    OP("dve", lambda e, kk=kk: e.tensor_scalar(dstf[:, kk:kk + 1], dstf[:, kk:kk + 1], float(N_EXP * 128 - 1), None, op0=ALU.min), reads=["dstf%d" % kk], writes=["dstf%d" % kk])
                    OP("dve", lambda e, kk=kk, tt=tt: e.tensor_copy(dest_i[:, tt, kk:kk + 1], dstf[:, kk:kk + 1]), reads=["dstf%d" % kk], writes=["dest%d_%d" % (tt, kk)])
                    sk = "xr.s%d_%d" % (tt, kk)
                    S.dma("pool", lambda e, kk=kk, tt=tt, hfb=hfb: e.indirect_dma_start(out=xr, out_offset=bass.IndirectOffsetOnAxis(ap=dest_i[:, tt, kk:kk + 1], axis=0), in_=hfb, in_offset=None),
                          reads=[hk, "dest%d_%d" % (tt, kk)] + (XR_Z if (tt == 0 and kk == 0) else []), writes=[sk] + (XR_Z if (tt == 0 and kk == 0) else []))
                    scat_keys.append(sk)
            if "d_dest" in dbg:
                dbg_dump("d_dest", dest_i, ["dest%d_%d" % (tt, kk) for tt in range(NT) for kk in range(2)])
                dbg_dump("d_gate", gate_t, ["gate%d" % tt for tt in range(NT)])

            if stop in (None, "E"):
                S.barrier()
                A.release(r_mark)
                WGU = [A.alloc((16, 1536), BF16) for _ in range(2)]
                WD = [A.alloc((6, D), BF16) for _ in range(2)]
                XE = [A.alloc((D,), BF16) for _ in range(2)]
                XET = [A.alloc((16, 128), BF16) for _ in range(1)]
                NSTG = 4
                STG = [A.alloc((2, D_EXP), F32) for _ in range(NSTG)]
                stg_i = {"i": 0}
                sg_e = A.alloc((D_EXP,), BF16)
                up_e = A.alloc((D_EXP,), BF16)
                hmid = A.alloc((D_EXP,), BF16)
                hT6 = A.alloc((6, 128), BF16)
                YE = [A.alloc((D,), BF16) for _ in range(2)]
                import os as _os
                ESTOP = float(_os.environ.get("K_ESTOP", 9))
            NEXP_RUN = int(_os.environ.get("K_NEXP", N_EXP))

            def load_expert(ex):
                    pi = ex % 2
                    wgu, wd, xe = WGU[pi], WD[pi], XE[pi]
                    S.dma("pool", lambda e: e.dma_start(out=wgu[:, :, 0:768], in_=w_gate[ex].rearrange("(k p) f -> p k f", p=128)), writes=["wg%d" % pi])
                    S.dma("pool", lambda e: e.dma_start(out=wd, in_=w_down[ex].rearrange("(k p) d -> p k d", p=128)), writes=["wd%d" % pi])
                    for kp in range(8):
                        si_ = stg_i["i"]
                        stg_i["i"] = (si_ + 1) % NSTG
                        S.dma("sp", lambda e, kp=kp, si_=si_: e.dma_start(out=STG[si_], in_=w_up[ex, kp * 256:(kp + 1) * 256, :].rearrange("(k p) f -> p k f", p=128)), writes=["stg%d" % si_])
                        OP("dve", lambda e, kp=kp, si_=si_: e.tensor_copy(wgu[:, 2 * kp:2 * kp + 2, 768:1536], STG[si_]), reads=["stg%d" % si_], writes=["wu%d_%d" % (pi, kp)])
                    S.dma("sp", lambda e: e.dma_start(out=xe, in_=xr[ex * 128:(ex + 1) * 128, :]), reads=(scat_keys if ex == 0 else []) + XR_Z, writes=["xe%d" % pi])

            load_expert(0)
            for ex in range(NEXP_RUN):
                    if ex + 1 < NEXP_RUN:
                        load_expert(ex + 1)
                    pi = ex % 2
                    wgu, wd, xe, xet, ye = WGU[pi], WD[pi], XE[pi], XET[0], YE[pi]
                    for half in range(2):
                        for kk in range(8):
                            k = half * 8 + kk
                            OP("pe", lambda e, half=half, kk=kk, k=k, xe=xe: e.transpose(psb(half)[:, kk * 128:(kk + 1) * 128], xe[:, k * 128:(k + 1) * 128], ident_b), reads=["xe%d" % pi, "cm_b"], writes=[PK(half)])
                        OP("act", lambda e, xet=xet, half=half: e.activation(xet[:, half * 8:(half + 1) * 8, :], psb(half).rearrange("p (k t) -> p k t", t=128), AF.Copy), reads=[PK(half)], writes=["xet0_%d" % half])
                    for nb in range(3):
                        for k in range(16):
                            wkeys = [["wg%d" % pi], ["wg%d" % pi, "wu%d_%d" % (pi, k // 2)], ["wu%d_%d" % (pi, k // 2)]][nb]
                            OP("pe", lambda e, nb=nb, k=k, xet=xet, wgu=wgu: e.matmul(ps(2 + nb), xet[:, k, :], wgu[:, k, nb * 512:(nb + 1) * 512], start=(k == 0), stop=(k == 15)),
                               reads=["xet0_%d" % (k // 8)] + wkeys, writes=[PK(2 + nb)])
                    OP("act", lambda e: e.activation(sg_e[:, 0:512], ps(2), AF.Silu), reads=[PK(2)], writes=["sg_e0"])
                    OP("act", lambda e: e.activation(sg_e[:, 512:768], ps(3)[:, 0:256], AF.Silu), reads=[PK(3)], writes=["sg_e1"])
                    OP("act", lambda e: e.activation(up_e[:, 0:256], ps(3)[:, 256:512], AF.Copy), reads=[PK(3)], writes=["up_e0"])
                    OP("act", lambda e: e.activation(up_e[:, 256:768], ps(4), AF.Copy), reads=[PK(4)], writes=["up_e1"])
                    OP("pool", lambda e: e.tensor_tensor(hmid, sg_e, up_e, op=ALU.mult), reads=["sg_e0", "sg_e1", "up_e0", "up_e1"], writes=["hmid"])
                    for kf in range(6):
                        OP("pe", lambda e, kf=kf: e.transpose(psb(5)[:, kf * 128:(kf + 1) * 128], hmid[:, kf * 128:(kf + 1) * 128], ident_b), reads=["hmid", "cm_b"], writes=[PK(5)])
                    OP("act", lambda e: e.activation(hT6, psb(5)[:, 0:768].rearrange("p (k t) -> p k t", t=128), AF.Copy), reads=[PK(5)], writes=["hT6"])
                    for cb in range(4):
                        bk = 6 + cb % 2
                        for kf in range(6):
                            OP("pe", lambda e, bk=bk, kf=kf, cb=cb, wd=wd: e.matmul(ps(bk), hT6[:, kf, :], wd[:, kf, cb * 512:(cb + 1) * 512], start=(kf == 0), stop=(kf == 5)),
                               reads=["hT6", "wd%d" % pi], writes=[PK(bk)])
                        OP("act", lambda e, bk=bk, cb=cb, ye=ye: e.activation(ye[:, cb * 512:(cb + 1) * 512], ps(bk), AF.Copy), reads=[PK(bk)], writes=["ye%d_%d" % (pi, cb)])
                    S.dma("sp", lambda e, ye=ye, ex=ex: e.dma_start(out=yr[ex * 128:(ex + 1) * 128, :], in_=ye), reads=["ye%d_%d" % (pi, cb) for cb in range(4)], writes=["yr%d" % ex])

            if stop is None:
                S.barrier()
                A.release(r_mark)
                Y1 = [A.alloc((D,), BF16) for _ in range(3)]
                Y2 = [A.alloc((D,), BF16) for _ in range(3)]
                XC = [A.alloc((D,), F32) for _ in range(3)]
                YRK = ["yr%d" % ex for ex in range(N_EXP)]
                for tt in range(NT):
                    pi = tt % 3
                    S.dma("pool", lambda e, tt=tt, pi=pi: e.indirect_dma_start(out=Y1[pi], out_offset=None, in_=yr, in_offset=bass.IndirectOffsetOnAxis(ap=dest_i[:, tt, 0:1], axis=0)),
                          reads=(YRK if tt == 0 else []) + ["dest%d_0" % tt], writes=["y1_%d" % pi])
                    S.dma("pool", lambda e, tt=tt, pi=pi: e.indirect_dma_start(out=Y2[pi], out_offset=None, in_=yr, in_offset=bass.IndirectOffsetOnAxis(ap=dest_i[:, tt, 1:2], axis=0)),
                          reads=["dest%d_1" % tt], writes=["y2_%d" % pi])
                    S.dma("sp", lambda e, tt=tt, pi=pi: e.dma_start(out=XC[pi], in_=x1s[tt * 128:(tt + 1) * 128, :]), writes=["xc%d" % pi])
                    OP("dve", lambda e, tt=tt, pi=pi: e.scalar_tensor_tensor(XC[pi], Y1[pi], gate_t[:, tt, 0:1], XC[pi], op0=ALU.mult, op1=ALU.add), reads=["y1_%d" % pi, "xc%d" % pi, "gate%d" % tt], writes=["xc%d" % pi])
                    OP("dve", lambda e, tt=tt, pi=pi: e.scalar_tensor_tensor(XC[pi], Y2[pi], gate_t[:, tt, 1:2], XC[pi], op0=ALU.mult, op1=ALU.add), reads=["y2_%d" % pi, "xc%d" % pi, "gate%d" % tt], writes=["xc%d" % pi])
                    out_toks.append(S.dma("sp", lambda e, tt=tt, pi=pi: e.dma_start(out=out[tt * 128:(tt + 1) * 128, :], in_=XC[pi]), reads=["xc%d" % pi], writes=["out%d" % tt]))
        if not out_toks:
            pass
        S.barrier(["sp"])
        S.emit()
        print("kernel build: instrs~%d arena_peak=%d signalling=%s of %s" % (S.ninst, A.peak, S.nsig, S.cnt))
    return nc


def _consts():
    p = np.arange(128)
    ident = np.eye(128, dtype=np.float32)
    tri = (p[:, None] <= p[None, :]).astype(np.float32)
    stri = (p[:, None] < p[None, :]).astype(np.float32)
    sw = np.where((p % 64) < 32, p + 32, p - 32)
    swap = np.zeros((128, 128), np.float32)
    swap[sw, p] = 1.0
    bd = ((p[:, None] // 64) == (p[None, :] // 64)).astype(np.float32)
    gt = (p[:, None] > p[None, :]).astype(np.float32)
    cmat = np.ascontiguousarray(np.stack([ident, tri, stri, swap, bd, gt], axis=1))
    half = 32
    freqs = (10000.0 ** (-np.arange(half, dtype=np.float32) / half)).astype(np.float32)
    pos = np.arange(S_LEN, dtype=np.float32)
    ang = (pos[None, :] * freqs[p % 32][:, None]).astype(np.float32)
    cos = np.cos(ang).astype(np.float32)
    sin = np.sin(ang).astype(np.float32) * np.where((p % 64) < 32, -1.0, 1.0)[:, None].astype(np.float32)
    ctab = np.ascontiguousarray(np.stack([cos, sin], axis=1)).astype(np.float32)
    eoff = np.ascontiguousarray(np.broadcast_to((np.arange(64, dtype=np.float32) * 128.0)[None, :], (128, 64)))
    return cmat, ctab, eoff


def make_in_maps(inputs, cores, moe=True):
    f = lambda k: np.ascontiguousarray(np.asarray(inputs[k], dtype=np.float32)[0])
    rep = lambda v: np.ascontiguousarray(np.broadcast_to(v.reshape(1, -1), (128, v.size)))
    cmat, ctab, eoff = _consts()
    conv = f("conv_qk")
    convw = np.ascontiguousarray(conv.T.reshape(16, 128, 4).transpose(1, 0, 2).reshape(128, 64))
    gmixT = np.ascontiguousarray(f("g_mix").reshape(16, 128).T)
    gbias = rep(np.concatenate([f("b_igate"), f("b_fgate")]))
    gml = rep(f("g_mlstm").reshape(-1))
    gqk = np.ascontiguousarray(np.stack([np.tile(f("g_q"), 2), np.tile(f("g_k"), 2)], axis=1))
    sinks = rep(f("sinks"))
    gffn = rep(f("g_ffn"))
    wroute = np.ascontiguousarray(np.concatenate([f("w_group"), f("w_expert")], axis=1))
    broute = rep(np.concatenate([f("b_group"), f("b_expert")]))
    shared = dict(w_in=f("w_in"), convw=convw, gmixT=gmixT, gbias=gbias, gml=gml, gqk=gqk, sinks=sinks,
                  w_proj_m=f("w_proj_m"), w_proj_a=f("w_proj_a"), w_out=f("w_out"), gffn=gffn,
                  wroute=wroute, broute=broute, cmat=cmat, ctab=ctab, eoff=eoff)
    if moe:
        shared.update(w_gate=f("w_gate"), w_up=f("w_up"), w_down=f("w_down"))
    xs = np.asarray(inputs["x"], dtype=np.float32)
    return [dict(shared, x=np.ascontiguousarray(xs[c])) for c in cores]


def kernel(**inputs):
    nc = build_nc()
    in_maps = make_in_maps(inputs, list(range(8)))
    res = run_bass_kernel_spmd(nc, in_maps, core_ids=list(range(8)))
    return np.stack([np.asarray(r["out"]) for r in res.results], axis=0).astype(np.float32)
```

```python
import math
import numpy as np
import ml_dtypes
from contextlib import ExitStack
import concourse.bass as bass
import concourse.mybir as mybir
from concourse.bass_utils import run_bass_kernel_spmd

F32 = mybir.dt.float32
BF16 = mybir.dt.bfloat16
I32 = mybir.dt.int32
U8 = mybir.dt.uint8
AF = mybir.ActivationFunctionType
ALU = mybir.AluOpType
AX = mybir.AxisListType

S_LEN = 2048
D = 2048
D_IN = 9736
NT = 16
TB = 512
NTB = 4
EPS = 1e-6
N_EXP = 64
D_EXP = 768
C_MQ, C_MK, C_MV, C_MO, C_G, C_AQ, C_AK, C_AV, C_GM, C_GA = 0, 1024, 2048, 3072, 4096, 4104, 5128, 5384, 5640, 7688
LN16 = math.log(16.0)

ENGS = ("pe", "act", "dve", "pool", "sp")


class Sched:
    NDSEM = 20

    def __init__(self, nc, es):
        self.nc = nc
        self.q = {e: [] for e in ENGS}
        self.oprecs = {e: [] for e in ENGS}
        self.cnt = {e: 0 for e in ENGS}
        self.sem = {e: es.enter_context(nc.semaphore("s_" + e)) for e in ENGS}
        self.dsem, self.dcount, self.dnext = {}, {}, {}
        for e in ("sp", "act", "pool"):
            self.dsem[e] = [es.enter_context(nc.semaphore("d_%s%d" % (e, i))) for i in range(self.NDSEM)]
            self.dcount[e] = [0] * self.NDSEM
            self.dnext[e] = 0
        self.seen = {e: {} for e in ENGS}
        self.res = {}
        self.ninst = 0

    def _need(self, eng, key, val, waits):
        if eng == "pe" and key == "pe":
            return
        if self.seen[eng].get(key, 0) >= val:
            return
        self.seen[eng][key] = val
        waits[key] = max(waits.get(key, 0), val)
        if isinstance(key, str):
            self.oprecs[key][val - 1]["sig"] = True

    def _deps(self, eng, reads, writes):
        waits = {}
        for r in reads:
            ent = self.res.get(r)
            if ent and ent[0]:
                self._need(eng, ent[0][0], ent[0][1], waits)
            if ent and r.startswith("ps"):
                for k, v in ent[1].items():
                    if k != eng:
                        self._need(eng, k, v, waits)
        for r in writes:
            ent = self.res.get(r)
            if ent:
                if ent[0]:
                    self._need(eng, ent[0][0], ent[0][1], waits)
                for k, v in ent[1].items():
                    self._need(eng, k, v, waits)
        return waits

    def _commit(self, tok, reads, writes):
        for r in reads:
            ent = self.res.setdefault(r, [None, {}])
            ent[1][tok[0]] = max(ent[1].get(tok[0], 0), tok[1])
        for r in writes:
            self.res[r] = [tok, {}]

    def op(self, eng, fn, reads=(), writes=()):
        waits = self._deps(eng, reads, writes)
        self.cnt[eng] += 1
        tok = (eng, self.cnt[eng])
        self._commit(tok, reads, writes)
        rec = {"kind": "op", "fn": fn, "waits": waits, "sig": False}
        self.q[eng].append(rec)
        self.oprecs[eng].append(rec)
        self.ninst += 1 + len(waits)
        return tok

    def dma(self, eng, fn, reads=(), writes=()):
        waits = self._deps(eng, reads, writes)
        i = self.dnext[eng]
        self.dnext[eng] = (i + 1) % self.NDSEM
        key = (eng, i)
        prev = self.dcount[eng][i]
        if prev:
            self._need(eng, key, prev, waits)
        self.dcount[eng][i] = prev + 16
        tok = (key, prev + 16)
        self._commit(tok, reads, writes)
        self.q[eng].append({"kind": "dma", "fn": fn, "waits": waits, "dsem": self.dsem[eng][i]})
        self.ninst += 1 + len(waits)
        return tok

    def barrier(self, engines=ENGS):
        toks = [(e, self.cnt[e]) for e in ENGS if self.cnt[e]]
        for e in self.dsem:
            for i in range(self.NDSEM):
                if self.dcount[e][i]:
                    toks.append(((e, i), self.dcount[e][i]))
        for eng in engines:
            waits = {}
            for k, v in toks:
                self._need(eng, k, v, waits)
            self.q[eng].append({"kind": "wait", "waits": waits})

    def emit(self):
        nc = self.nc
        sigmap = {}
        for eng in ENGS:
            cum, m = 0, []
            for rec in self.oprecs[eng]:
                if rec["sig"]:
                    cum += 1
                m.append(cum)
            sigmap[eng] = m
        self.nsig = {e: (sigmap[e][-1] if sigmap[e] else 0) for e in ENGS}

        def replay(eng, e):
            sem = self.sem[eng]
            for rec in self.q[eng]:
                for k, v in rec["waits"].items():
                    if isinstance(k, str):
                        e.wait_ge(self.sem[k], sigmap[k][v - 1])
                    else:
                        e.wait_ge(self.dsem[k[0]][k[1]], v)
                if rec["kind"] == "op":
                    ins = rec["fn"](e)
                    if rec["sig"]:
                        ins.then_inc(sem, 1)
                elif rec["kind"] == "dma":
                    rec["fn"](e).then_inc(rec["dsem"], 16)

        with nc.Block() as block:
            @block.sync
            def _(e):
                replay("sp", e)

            @block.tensor
            def _(e):
                replay("pe", e)

            @block.scalar
            def _(e):
                replay("act", e)

            @block.vector
            def _(e):
                replay("dve", e)

            @block.gpsimd
            def _(e):
                replay("pool", e)


class Arena:
    def __init__(self, nc, es, nbytes):
        self.t = es.enter_context(nc.sbuf_tensor("arena", [128, nbytes], U8))
        self.off = 0
        self.cap = nbytes
        self.peak = 0

    def alloc(self, shape, dt):
        esz = {F32: 4, BF16: 2, I32: 4, U8: 1}[dt]
        n = esz
        for s in shape:
            n *= s
        off = (self.off + 63) // 64 * 64
        assert off + n <= self.cap, ("arena overflow", off, n, self.cap)
        self.off = off + n
        self.peak = max(self.peak, self.off)
        ap = self.t[:, off:off + n]
        if dt != U8:
            ap = ap.bitcast(dt)
        if len(shape) == 2:
            ap = ap.rearrange("p (a b) -> p a b", b=shape[1])
        elif len(shape) == 3:
            ap = ap.rearrange("p (a b c) -> p a b c", b=shape[1], c=shape[2])
        return ap

    def mark(self):
        return self.off

    def release(self, m):
        self.off = m


def build_nc(debug=None, stop=None, moe=True):
    nc = bass.Bass("TRN2", target_bir_lowering=False)
    dram = lambda name, shape, dt, kind="ExternalInput": nc.dram_tensor(name, list(shape), dt, kind=kind).ap()
    x = dram("x", [S_LEN, D], F32)
    w_in = dram("w_in", [D, D_IN], F32)
    convw = dram("convw", [128, 64], F32)
    gmixT = dram("gmixT", [128, 16], F32)
    gbias = dram("gbias", [128, 8], F32)
    gml = dram("gml", [128, 1024], F32)
    gqk = dram("gqk", [128, 2], F32)
    sinks = dram("sinks", [128, 16], F32)
    w_pm = dram("w_proj_m", [1024, D], F32)
    w_pa = dram("w_proj_a", [1024, D], F32)
    w_out = dram("w_out", [D, D], F32)
    gffn = dram("gffn", [128, D], F32)
    wroute = dram("wroute", [D, 72], F32)
    broute = dram("broute", [128, 72], F32)
    if moe:
        w_gate = dram("w_gate", [N_EXP, D, D_EXP], F32)
        w_up = dram("w_up", [N_EXP, D, D_EXP], F32)
        w_down = dram("w_down", [N_EXP, D_EXP, D], F32)
    cmat = dram("cmat", [128, 6, 128], F32)
    ctab = dram("ctab", [128, 2, S_LEN], F32)
    eoff = dram("eoff", [128, 64], F32)
    out = dram("out", [S_LEN, D], F32, kind="ExternalOutput")
    x1s = dram("x1s", [S_LEN, D], F32, kind="Internal")
    xr = dram("xr", [N_EXP * 128, D], BF16, kind="Internal")
    yr = dram("yr", [N_EXP * 128, D], BF16, kind="Internal")
    dbg = {}
    if debug:
        for name, shape, dt in debug:
            dbg[name] = dram(name, shape, dt, kind="ExternalOutput")

    if "d_x1" in dbg:
        x1s = dbg["d_x1"]
    with ExitStack() as es:
        S = Sched(nc, es)
        A = Arena(nc, es, 206 * 1024)
        PSA = es.enter_context(nc.psum_tensor("psA", [128, 8, 512], F32))
        ps = lambda b: PSA[:, b, :]
        psb = lambda b: PSA[:, b, :].bitcast(BF16)
        PK = lambda b: "ps%d" % b
        out_toks = []

        def OP(eng, fn, reads=(), writes=()):
            return S.op(eng, fn, reads, writes)

        def dbg_dump(name, src_ap, reads, dst=None):
            if name in dbg:
                d = dbg[name] if dst is None else dst
                out_toks.append(S.dma("sp", lambda e: e.dma_start(out=d, in_=src_ap), reads=reads, writes=["dbg." + name + str(len(out_toks))]))

        cm_f = A.alloc((6, 128), F32)
        cm_b = A.alloc((6, 128), BF16)
        ones_f = A.alloc((128,), F32)
        ones_b = A.alloc((128,), BF16)
        convw_t = A.alloc((16, 4), F32)
        gmixT_t = A.alloc((16,), F32)
        gbias_t = A.alloc((8,), F32)
        gqk_t = A.alloc((2,), F32)
        esk_t = A.alloc((16,), F32)
        ident_f, tri_f, stri_f = cm_f[:, 0, :], cm_f[:, 1, :], cm_f[:, 2, :]
        ident_b, tri_b, stri_b, swap_b, bd_b, gt_b = (cm_b[:, i, :] for i in range(6))
        S.dma("sp", lambda e: e.dma_start(out=cm_f, in_=cmat), writes=["cm_f"])
        S.dma("sp", lambda e: e.dma_start(out=convw_t, in_=convw.rearrange("p (c j) -> p c j", j=4)), writes=["convw"])
        S.dma("sp", lambda e: e.dma_start(out=gmixT_t, in_=gmixT), writes=["gmixT"])
        S.dma("sp", lambda e: e.dma_start(out=gbias_t, in_=gbias), writes=["gbias"])
        S.dma("sp", lambda e: e.dma_start(out=gqk_t, in_=gqk), writes=["gqk"])
        S.dma("sp", lambda e: e.dma_start(out=esk_t, in_=sinks), writes=["esk"])
        OP("dve", lambda e: e.tensor_copy(cm_b, cm_f), reads=["cm_f"], writes=["cm_b"])
        OP("dve", lambda e: e.memset(ones_f, 1.0), writes=["ones_f"])
        OP("dve", lambda e: e.memset(ones_b, 1.0), writes=["ones_b"])
        OP("act", lambda e: e.activation(esk_t, esk_t, AF.Exp), reads=["esk"], writes=["esk"])
        CONST_R = ["cm_f", "cm_b", "ones_f", "ones_b", "convw", "gmixT", "gbias", "gqk", "esk"]

        p1_mark = A.mark()
        XT = [A.alloc((D,), F32) for _ in range(2)]
        zt = XT[1].bitcast(BF16)
        xr_z = xr.rearrange("(a p c) d -> a p (c d)", p=128, c=2)
        XR_Z = ["xr.z%d" % a for a in range(32)]

        def zero_fill():
            OP("dve", lambda e: e.memset(zt, 0.0), writes=["xt1"])
            for a in range(32):
                S.dma("sp", lambda e, a=a: e.dma_start(out=xr_z[a], in_=zt), reads=["xt1"], writes=["xr.z%d" % a])
        gml_t = A.alloc((1024,), F32)
        S.dma("sp", lambda e: e.dma_start(out=gml_t, in_=gml), writes=["gml"])
        hb = A.alloc((D,), BF16)
        hT = A.alloc((16, TB), BF16)
        NW = 6
        WB = [A.alloc((8192,), U8) for _ in range(NW)]
        wstate = {"i": 0}
        ss_t = A.alloc((NT,), F32)
        rs_t = A.alloc((NT,), F32)
        ZC = [A.alloc((TB + 3,), F32) for _ in range(2)]
        ACC = [A.alloc((TB,), F32) for _ in range(2)]
        hal = A.alloc((16, 3), F32)
        mqkT = A.alloc((16, TB), BF16)
        v_ext = A.alloc((4, 4, 258), BF16)
        gs = A.alloc((4, 1024), BF16)
        graw = A.alloc((4, 8), F32)
        tab = A.alloc((2, TB), F32)
        aqT = A.alloc((8, TB), BF16)
        akT = A.alloc((2, 128 + TB), BF16)
        avd = A.alloc((5, 4, 128), BF16)
        hmT = A.alloc((8, TB), BF16)
        haT = A.alloc((8, TB), BF16)
        Cf = A.alloc((4, 2, 257), F32)
        Cb = A.alloc((4, 2, 258), BF16)
        f16 = lambda: A.alloc((16,), F32)
        v3 = lambda t: t.rearrange("p (a b) -> p a b", b=4)
        lfn, negB, u_t, euS, euE, dec, flr, dt1, dt2, dt3, dt4, Dg = (f16() for _ in range(12))
        NB_all = A.alloc((5, 4), F32)
        U_all = A.alloc((5, 4), F32)
        cm_s = A.alloc((1,), F32)
        kp = [A.alloc((256,), BF16) for _ in range(2)]
        PT = [A.alloc((128,), BF16) for _ in range(2)]
        msc = A.alloc((4, 8), F32)
        hsq = A.alloc((256,), BF16)
        ytok = [A.alloc((1024,), BF16) for _ in range(2)]
        sqb = A.alloc((TB,), BF16)
        rstd_a = ZC[0][:, 0:TB]
        xn_a = A.alloc((TB,), BF16)
        t1_a = ACC[0]
        t2_a = ACC[1]
        PTa = [A.alloc((TB,), BF16) for _ in range(2)]
        PTb = [A.alloc((TB,), BF16) for _ in range(2)]
        rd_a = [A.alloc((4, 128), F32) for _ in range(2)]
        sg_m, sg_a = PTa, PTb
        tm1 = ACC
        tm2 = [ZC[0][:, 0:TB], ZC[1][:, 0:TB]]
        mixT = mqkT
        XP = [A.alloc((256,), F32) for _ in range(3)]
        XO = [A.alloc((256,), F32) for _ in range(3)]

        OP("dve", lambda e: e.memset(hal, 0.0), writes=["hal"])
        OP("dve", lambda e: e.memset(v_ext, 1.0), writes=["v_ext"])
        OP("dve", lambda e: e.memset(Cf, 0.0), writes=["Cf"])
        OP("dve", lambda e: e.memset(Cb, 0.0), writes=["Cb"])
        OP("dve", lambda e: e.memset(NB_all, 0.0), writes=["NB_all"])
        OP("dve", lambda e: e.memset(U_all, 0.0), writes=["U_all"])
        OP("dve", lambda e: e.memset(ss_t, 0.0), writes=["ss"])
        OP("dve", lambda e: e.memset(akT, 0.0), writes=["akT"])
        OP("dve", lambda e: e.memset(avd, 0.0), writes=["avd"])

        def load_w(src2d, kch, ncols, runs=None):
            i = wstate["i"]
            wstate["i"] = (i + 1) % NW
            key = "wb%d" % i
            assert kch * ncols * 2 <= 8192
            view = WB[i][:, 0:kch * ncols * 2].bitcast(BF16).rearrange("p (k c) -> p k c", c=ncols)
            if runs is None:
                S.dma("pool", lambda e: e.dma_start(out=view, in_=src2d.rearrange("(k p) c -> p k c", p=128)), writes=[key])
            else:
                for (dc0, src) in runs:
                    n = src.shape[1]
                    S.dma("pool", lambda e, dc0=dc0, src=src, n=n: e.dma_start(out=view[:, :, dc0:dc0 + n], in_=src.rearrange("(k p) c -> p k c", p=128)), writes=[key])
            return view, key

        pbank = {"i": 0}

        def next_bank(lo=0, n=4):
            b = lo + pbank["i"] % n
            pbank["i"] += 1
            return b

        for tb in range(NTB):
            t0 = tb * TB
            for j in range(4):
                tt = tb * 4 + j
                xt = XT[tt % 2]
                xk = "xt%d" % (tt % 2)
                S.dma("sp", lambda e, xt=xt, tt=tt: e.dma_start(out=xt, in_=x[tt * 128:(tt + 1) * 128, :]), writes=[xk])
                OP("act", lambda e, xt=xt, tt=tt: e.activation(hb, xt, AF.Square, accum_out=ss_t[:, tt:tt + 1]), reads=[xk, "ss"], writes=["hb", "ss%d" % tt])
                OP("act", lambda e, tt=tt: e.activation(rs_t[:, tt:tt + 1], ss_t[:, tt:tt + 1], AF.Ln, scale=1.0 / D, bias=EPS), reads=["ss%d" % tt], writes=["rs%d" % tt])
                OP("act", lambda e, tt=tt: e.activation(rs_t[:, tt:tt + 1], rs_t[:, tt:tt + 1], AF.Exp, scale=-0.5), reads=["rs%d" % tt], writes=["rs%d" % tt])
                OP("dve", lambda e, xt=xt, tt=tt: e.tensor_scalar(hb, xt, rs_t[:, tt:tt + 1], None, op0=ALU.mult), reads=[xk, "rs%d" % tt], writes=["hb"])
                for half in range(2):
                    b = half
                    for kk in range(8):
                        k = half * 8 + kk
                        OP("pe", lambda e, b=b, kk=kk, k=k: e.transpose(psb(b)[:, kk * 128:(kk + 1) * 128], hb[:, k * 128:(k + 1) * 128], ident_b),
                           reads=["hb", "cm_b"], writes=[PK(b)])
                    OP("dve", lambda e, b=b, half=half, j=j: e.tensor_tensor(
                        hT[:, half * 8:(half + 1) * 8, j * 128:(j + 1) * 128],
                        psb(b).rearrange("p (k t) -> p k t", t=128),
                        gmixT_t[:, half * 8:(half + 1) * 8].unsqueeze(2).to_broadcast([128, 8, 128]), op=ALU.mult),
                        reads=[PK(b), "gmixT"], writes=["hT"])
            S.dma("sp", lambda e, t0=t0: e.dma_start(out=tab, in_=ctab[:, :, t0:t0 + TB]), writes=["tab"])

            def proj_fm(wv, wk, c0, ncol_chunk=128):
                b = next_bank(0, 4)
                for k in range(16):
                    OP("pe", lambda e, b=b, k=k, c0=c0: e.matmul(ps(b), wv[:, k, c0:c0 + 128], hT[:, k, :], start=(k == 0), stop=(k == 15)),
                       reads=[wk, "hT"], writes=[PK(b)])
                return b

            def proj_tm(wv, wk, j, ncols):
                b = next_bank(0, 4)
                for k in range(16):
                    OP("pe", lambda e, b=b, k=k, j=j: e.matmul(ps(b)[:, 0:ncols], hT[:, k, j * 128:(j + 1) * 128], wv[:, k, 0:ncols], start=(k == 0), stop=(k == 15)),
                       reads=[wk, "hT"], writes=[PK(b)])
                return b

            for g8 in range(8):
                wv, wk = load_w(w_in[:, g8 * 256:(g8 + 1) * 256], 16, 256)
                for c2 in range(2):
                    cj = g8 * 2 + c2
                    b = proj_fm(wv, wk, c2 * 128)
                    zc, zk = ZC[cj % 2], "zc%d" % (cj % 2)
                    acc, ak_ = ACC[cj % 2], "acc%d" % (cj % 2)
                    OP("act", lambda e, zc=zc, cj=cj: e.activation(zc[:, 0:3], hal[:, cj, :], AF.Copy), reads=["hal%d" % cj, "hal"], writes=[zk + "h"])
                    OP("act", lambda e, zc=zc, b=b: e.activation(zc[:, 3:TB + 3], ps(b), AF.Copy), reads=[PK(b)], writes=[zk])
                    OP("act", lambda e, zc=zc, cj=cj: e.activation(hal[:, cj, :], zc[:, TB:TB + 3], AF.Copy), reads=[zk], writes=["hal%d" % cj])
                    OP("dve", lambda e, zc=zc, acc=acc, cj=cj: e.tensor_scalar(acc, zc[:, 3:TB + 3], convw_t[:, cj, 3:4], None, op0=ALU.mult),
                       reads=[zk, zk + "h", "convw"], writes=[ak_])
                    for tap in (2, 1, 0):
                        OP("dve", lambda e, zc=zc, acc=acc, cj=cj, tap=tap: e.scalar_tensor_tensor(acc, zc[:, tap:tap + TB], convw_t[:, cj, tap:tap + 1], acc, op0=ALU.mult, op1=ALU.add),
                           reads=[zk, zk + "h", ak_], writes=[ak_])
                    OP("act", lambda e, acc=acc, cj=cj: e.activation(mqkT[:, cj, :], acc, AF.Silu), reads=[ak_], writes=["mqkT%d" % cj])
            for hh in range(4):
                wv, wk = load_w(w_in[:, C_MV + hh * 256:C_MV + (hh + 1) * 256], 16, 256)
                for j in range(4):
                    b = proj_tm(wv, wk, j, 256)
                    OP("act", lambda e, b=b, j=j, hh=hh: e.activation(v_ext[:, j, hh, 0:256], ps(b)[:, 0:256], AF.Copy), reads=[PK(b)], writes=["v%d_%d" % (j, hh)])
            for hh in range(4):
                wv, wk = load_w(w_in[:, C_MO + hh * 256:C_MO + (hh + 1) * 256], 16, 256)
                for j in range(4):
                    b = proj_tm(wv, wk, j, 256)
                    OP("act", lambda e, b=b, j=j, hh=hh: e.activation(gs[:, j, hh * 256:(hh + 1) * 256], ps(b)[:, 0:256], AF.Sigmoid), reads=[PK(b)], writes=["gs%d_%d" % (j, hh)])
                    OP("dve", lambda e, j=j, hh=hh: e.tensor_tensor(gs[:, j, hh * 256:(hh + 1) * 256], gs[:, j, hh * 256:(hh + 1) * 256], gml_t[:, hh * 256:(hh + 1) * 256], op=ALU.mult),
                       reads=["gs%d_%d" % (j, hh), "gml"], writes=["gs%d_%d" % (j, hh)])
            wv, wk = load_w(w_in[:, C_G:C_G + 8], 16, 8)
            for j in range(4):
                b = proj_tm(wv, wk, j, 8)
                OP("dve", lambda e, b=b, j=j: e.tensor_tensor(graw[:, j, :], ps(b)[:, 0:8], gbias_t, op=ALU.add), reads=[PK(b), "gbias"], writes=["graw%d" % j])

            def qk_post(b, gcol, dst, dkey, dsl):
                OP("act", lambda e: e.activation(sqb, ps(b), AF.Square), reads=[PK(b)], writes=["sqb"])
                OP("pe", lambda e: e.matmul(ps(4), bd_b, sqb, start=True, stop=True), reads=["sqb", "cm_b"], writes=[PK(4)])
                OP("act", lambda e: e.activation(rstd_a, ps(4), AF.Ln, scale=1.0 / 64, bias=EPS), reads=[PK(4)], writes=["zc0", "zc0h"])
                OP("act", lambda e: e.activation(rstd_a, rstd_a, AF.Exp, scale=-0.5), reads=["zc0"], writes=["zc0", "zc0h"])
                OP("dve", lambda e: e.scalar_tensor_tensor(xn_a, ps(b), gqk_t[:, gcol:gcol + 1], rstd_a, op0=ALU.mult, op1=ALU.mult),
                   reads=[PK(b), "gqk", "zc0"], writes=["xn_a"])
                OP("pe", lambda e: e.matmul(ps(5), swap_b, xn_a, start=True, stop=True), reads=["xn_a", "cm_b"], writes=[PK(5)])
                OP("dve", lambda e: e.tensor_tensor(t1_a, xn_a, tab[:, 0, :], op=ALU.mult), reads=["xn_a", "tab"], writes=["acc0"])
                OP("dve", lambda e: e.tensor_tensor(t2_a, ps(5), tab[:, 1, :], op=ALU.mult), reads=[PK(5), "tab"], writes=["acc1"])
                OP("dve", lambda e: e.tensor_tensor(dst, t1_a, t2_a, op=ALU.add), reads=["acc0", "acc1"], writes=[dkey])

            if tb > 0:
                OP("act", lambda e: e.activation(akT[:, :, 0:128], akT[:, :, TB:TB + 128], AF.Copy), reads=["akT"], writes=["akT"])
                OP("act", lambda e: e.activation(avd[:, 0, :, :], avd[:, 4, :, :], AF.Copy), reads=["avd"], writes=["avd"])
            wv, wk = load_w(w_in[:, C_AK:C_AK + 256], 16, 256)
            for i in range(2):
                b = proj_fm(wv, wk, i * 128)
                qk_post(b, 1, akT[:, i, 128:128 + TB], "akT", None)
            wv, wk = load_w(w_in[:, C_AV:C_AV + 256], 16, 256)
            for j in range(4):
                b = proj_tm(wv, wk, j, 256)
                for dup in range(2):
                    OP("act", lambda e, b=b, j=j, dup=dup: e.activation(avd[:, 1 + j, :, dup * 64:(dup + 1) * 64], ps(b)[:, 0:256].rearrange("p (g c) -> p g c", c=64), AF.Copy),
                       reads=[PK(b)], writes=["avd"])
            for a in range(4):
                runs = []
                for c2 in range(2):
                    ci = a * 2 + c2
                    i, r = ci // 4, ci % 4
                    h0, h1 = 8 * i + r, 8 * i + 4 + r
                    runs.append((c2 * 128, w_in[:, C_AQ + h0 * 64:C_AQ + (h0 + 1) * 64]))
                    runs.append((c2 * 128 + 64, w_in[:, C_AQ + h1 * 64:C_AQ + (h1 + 1) * 64]))
                wv, wk = load_w(None, 16, 256, runs=runs)
                for c2 in range(2):
                    ci = a * 2 + c2
                    b = proj_fm(wv, wk, c2 * 128)
                    qk_post(b, 0, aqT[:, ci, :], "aqT%d" % ci, None)

            if "d_mqkT" in dbg:
                dbg_dump("d_mqkT", mqkT, ["mqkT%d" % c for c in range(16)], dst=dbg["d_mqkT"][:, :, t0:t0 + TB])
            if "d_v" in dbg:
                dbg_dump("d_v", v_ext, ["v%d_%d" % (j, hh) for j in range(4) for hh in range(4)], dst=dbg["d_v"][:, tb * 4:(tb + 1) * 4, :, :])
            if "d_gs" in dbg:
                dbg_dump("d_gs", gs, ["gs%d_%d" % (j, hh) for j in range(4) for hh in range(4)], dst=dbg["d_gs"][:, tb * 4:(tb + 1) * 4, :])
            if "d_graw" in dbg:
                dbg_dump("d_graw", graw, ["graw%d" % j for j in range(4)], dst=dbg["d_graw"][:, tb * 4:(tb + 1) * 4, :])
            if "d_aqT" in dbg:
                dbg_dump("d_aqT", aqT, ["aqT%d" % c for c in range(8)], dst=dbg["d_aqT"][:, :, t0:t0 + TB])
            if "d_akT" in dbg:
                dbg_dump("d_akT", akT[:, :, 128:128 + TB], ["akT"], dst=dbg["d_akT"][:, :, t0:t0 + TB])
            if stop == "1b":
                continue

            if tb == 0:
                zero_fill()
            GR = ["graw%d" % j for j in range(4)]
            OP("act", lambda e: e.activation(v3(lfn), graw[:, :, 4:8], AF.Exp, scale=-1.0), reads=GR, writes=["lfn"])
            OP("act", lambda e: e.activation(lfn, lfn, AF.Ln, bias=1.0), reads=["lfn"], writes=["lfn"])
            OP("pe", lambda e: e.matmul(ps(4)[:, 0:16], tri_f, lfn, start=True, stop=True), reads=["lfn", "cm_f"], writes=[PK(4)])
            OP("pe", lambda e: e.matmul(ps(4)[:, 16:32], ones_f, lfn, start=True, stop=True), reads=["lfn", "ones_f"], writes=[PK(4)])
            if tb > 0:
                OP("dve", lambda e: e.tensor_copy(NB_all[:, 0, :], NB_all[:, 4, :]), reads=["NB_all"], writes=["NB_all"])
                OP("dve", lambda e: e.tensor_copy(U_all[:, 0, :], U_all[:, 4, :]), reads=["U_all"], writes=["U_all"])
            for j in range(4):
                OP("dve", lambda e, j=j: e.tensor_tensor(NB_all[:, j + 1, :], NB_all[:, j, :], ps(4)[:, 16 + j * 4:20 + j * 4], op=ALU.add), reads=["NB_all", PK(4)], writes=["NB_all"])
            OP("dve", lambda e: e.tensor_tensor(v3(negB), v3(ps(4)[:, 0:16]), NB_all[:, 0:4, :], op=ALU.add), reads=[PK(4), "NB_all"], writes=["negB"])
            OP("dve", lambda e: e.tensor_tensor(v3(u_t), graw[:, :, 0:4], v3(negB), op=ALU.add), reads=GR + ["negB"], writes=["u_t"])
            OP("pe", lambda e: e.transpose(ps(4)[0:16, 64:192], u_t, ident_f), reads=["u_t", "cm_f"], writes=[PK(4)])
            OP("dve", lambda e: e.reduce_max(cm_s[0:16, :], ps(4)[0:16, 64:192], axis=AX.X), reads=[PK(4)], writes=["cm_s"])
            OP("dve", lambda e: e.tensor_scalar(Dg[0:16, :], ident_f[0:16, 0:16], cm_s[0:16, 0:1], None, op0=ALU.mult), reads=["cm_s", "cm_f"], writes=["Dg"])
            OP("pe", lambda e: e.matmul(ps(4)[:, 32:48], ones_f[0:16, :], Dg[0:16, :], start=True, stop=True), reads=["Dg", "ones_f"], writes=[PK(4)])
            for j in range(4):
                OP("dve", lambda e, j=j: e.tensor_tensor(U_all[:, j + 1, :], U_all[:, j, :], ps(4)[:, 32 + j * 4:36 + j * 4], op=ALU.max), reads=["U_all", PK(4)], writes=["U_all"])
            Us, Ue = U_all[:, 0:4, :], U_all[:, 1:5, :]
            OP("dve", lambda e: e.scalar_tensor_tensor(v3(dt1), v3(u_t), -LN16, Us, op0=ALU.add, op1=ALU.subtract), reads=["u_t", "U_all"], writes=["dt1"])
            OP("act", lambda e: e.activation(euS, dt1, AF.Exp), reads=["dt1"], writes=["euS"])
            OP("dve", lambda e: e.scalar_tensor_tensor(v3(dt2), v3(u_t), -LN16, Ue, op0=ALU.add, op1=ALU.subtract), reads=["u_t", "U_all"], writes=["dt2"])
            OP("act", lambda e: e.activation(euE, dt2, AF.Exp), reads=["dt2"], writes=["euE"])
            OP("dve", lambda e: e.tensor_tensor(v3(dt3), Us, Ue, op=ALU.subtract), reads=["U_all"], writes=["dt3"])
            OP("act", lambda e: e.activation(dec, dt3, AF.Exp), reads=["dt3"], writes=["dec"])
            OP("dve", lambda e: e.tensor_tensor(v3(dt4), v3(negB), Us, op=ALU.subtract), reads=["negB", "U_all"], writes=["dt4"])
            OP("act", lambda e: e.activation(flr, dt4, AF.Exp), reads=["dt4"], writes=["flr"])

            def mlstm_unit(j, h):
                yt_, yk = ytok[j % 2], "ytok%d" % (j % 2)
                tsl = slice(j * 128, (j + 1) * 128)
                col = j * 4 + h
                qc = (2 * h, 2 * h + 1)
                kc = (8 + 2 * h, 9 + 2 * h)
                QR = ["mqkT%d" % c for c in qc]
                KR = ["mqkT%d" % c for c in kc]
                VK = "v%d_%d" % (j, h)
                kp_, kpk = kp[h % 2], "kp%d" % (h % 2)
                PT_, ptk = PT[h % 2], "PT%d" % (h % 2)
                nb = 2 + (h % 2)
                for half in range(2):
                    OP("pe", lambda e, half=half: e.transpose(psb(0)[:, half * 128:(half + 1) * 128], mqkT[:, kc[half], tsl], ident_b),
                       reads=[KR[half], "cm_b"], writes=[PK(0)])
                OP("act", lambda e: e.activation(kp_, psb(0)[:, 0:256], AF.Copy, scale=euE[:, col:col + 1]), reads=[PK(0), "euE"], writes=[kpk])
                for half in range(2):
                    OP("pe", lambda e, half=half: e.matmul(ps(1)[:, 0:128], mqkT[:, kc[half], tsl], mqkT[:, qc[half], tsl], start=(half == 0), stop=(half == 1)),
                       reads=[KR[half], QR[half]], writes=[PK(1)])
                OP("dve", lambda e: e.scalar_tensor_tensor(PT_, ps(1)[:, 0:128], euS[:, col:col + 1], tri_b, op0=ALU.mult, op1=ALU.mult),
                   reads=[PK(1), "euS", "cm_b"], writes=[ptk])
                for half in range(2):
                    OP("pe", lambda e, half=half: e.matmul(ps(nb)[:, 0:257], mqkT[:, qc[half], tsl], Cb[:, h, half, 0:257], start=(half == 0), stop=False),
                       reads=[QR[half], "Cb%d" % h], writes=[PK(nb)])
                OP("pe", lambda e: e.matmul(ps(nb)[:, 0:257], PT_, v_ext[:, j, h, 0:257], start=False, stop=True),
                   reads=[ptk, VK, "v_ext"], writes=[PK(nb)])
                for half in range(2):
                    OP("pe", lambda e, half=half: e.matmul(PSA[:, 4 + half, 0:257], kp_[:, half * 128:(half + 1) * 128], v_ext[:, j, h, 0:257], start=True, stop=True),
                       reads=[kpk, VK, "v_ext"], writes=[PK(4 + half)])
                OP("dve", lambda e: e.scalar_tensor_tensor(Cf[:, h, :, :], Cf[:, h, :, :], dec[:, col:col + 1], PSA[:, 4:6, 0:257], op0=ALU.mult, op1=ALU.add),
                   reads=["Cf", "Cf%d" % h, "dec", PK(4), PK(5)], writes=["Cf%d" % h])
                OP("act", lambda e: e.activation(Cb[:, h, :, 0:257], Cf[:, h, :, :], AF.Copy), reads=["Cf%d" % h, "Cb"], writes=["Cb%d" % h])
                m = msc[:, h, :]
                mk_ = "msc%d" % h

                def stage_b():
                    mlstm_out(j, h, nb, m, mk_, yt_, yk)
                return stage_b

            def mlstm_out(j, h, nb, m, mk_, yt_, yk):
                col = j * 4 + h
                OP("act", lambda e: e.activation(m[:, 0:1], ps(nb)[:, 256:257], AF.Abs), reads=[PK(nb)], writes=[mk_])
                OP("dve", lambda e: e.tensor_scalar(m[:, 0:1], m[:, 0:1], flr[:, col:col + 1], None, op0=ALU.max), reads=[mk_, "flr"], writes=[mk_])
                OP("dve", lambda e: e.reciprocal(m[:, 1:2], m[:, 0:1]), reads=[mk_], writes=[mk_])
                OP("dve", lambda e: e.memset(m[:, 2:3], 0.0), reads=[mk_], writes=[mk_])
                OP("act", lambda e: e.activation(hsq, ps(nb)[:, 0:256], AF.Square, scale=m[:, 1:2], accum_out=m[:, 2:3]), reads=[PK(nb), mk_], writes=[mk_, "hsq"])
                OP("act", lambda e: e.activation(m[:, 3:4], m[:, 2:3], AF.Ln, scale=1.0 / 256, bias=EPS), reads=[mk_], writes=[mk_])
                OP("act", lambda e: e.activation(m[:, 3:4], m[:, 3:4], AF.Exp, scale=-0.5), reads=[mk_], writes=[mk_])
                OP("dve", lambda e: e.tensor_tensor(m[:, 4:5], m[:, 1:2], m[:, 3:4], op=ALU.mult), reads=[mk_], writes=[mk_])
                OP("dve", lambda e: e.scalar_tensor_tensor(yt_[:, h * 256:(h + 1) * 256], ps(nb)[:, 0:256], m[:, 4:5], gs[:, j, h * 256:(h + 1) * 256], op0=ALU.mult, op1=ALU.mult),
                   reads=[PK(nb), mk_, "gs%d_%d" % (j, h)], writes=[yk])

            def mlstm_tile_end(j):
                yt_, yk = ytok[j % 2], "ytok%d" % (j % 2)
                tsl = slice(j * 128, (j + 1) * 128)
                for c in range(8):
                    OP("pe", lambda e, c=c: e.transpose(psb(0)[:, c * 128:(c + 1) * 128], yt_[:, c * 128:(c + 1) * 128], ident_b), reads=[yk, "cm_b"], writes=[PK(0)])
                OP("act", lambda e: e.activation(hmT[:, :, tsl], psb(0).rearrange("p (c t) -> p c t", t=128), AF.Copy), reads=[PK(0)], writes=["hmT"])

            AQR = ["aqT%d" % c for c in range(8)]

            def swa_unit(j, g):
                n = tb * 4 + j
                tsl = slice(j * 128, (j + 1) * 128)
                i, hf = g // 2, g % 2
                pr = slice(hf * 64, hf * 64 + 64)
                si = (j * 4 + g) % 2
                has_prev = n > 0
                pa_, pb_, rd = PTa[si], PTb[si], rd_a[si]
                pak, pbk, rdk = "PTa%d" % si, "PTb%d" % si, "rd%d" % si
                qv = aqT[pr, 4 * i:4 * i + 4, tsl]
                m3 = lambda t: t.rearrange("p (r q) -> p r q", q=128)
                if has_prev:
                    OP("pe", lambda e: e.matmul(ps(1), akT[pr, i, j * 128:(j + 1) * 128], qv, start=True, stop=True), reads=["akT"] + AQR, writes=[PK(1)])
                    OP("act", lambda e: e.activation(pa_, ps(1), AF.Exp, scale=0.125), reads=[PK(1)], writes=[pak])
                    OP("dve", lambda e: e.tensor_tensor(m3(pa_), m3(pa_), gt_b.unsqueeze(1).to_broadcast([128, 4, 128]), op=ALU.mult), reads=[pak, "cm_b"], writes=[pak])
                OP("pe", lambda e: e.matmul(ps(1), akT[pr, i, (j + 1) * 128:(j + 2) * 128], qv, start=True, stop=True), reads=["akT"] + AQR, writes=[PK(1)])
                OP("act", lambda e: e.activation(pb_, ps(1), AF.Exp, scale=0.125), reads=[PK(1)], writes=[pbk])
                OP("dve", lambda e: e.tensor_tensor(m3(pb_), m3(pb_), tri_b.unsqueeze(1).to_broadcast([128, 4, 128]), op=ALU.mult), reads=[pbk, "cm_b"], writes=[pbk])
                for (bk, lhs_fn, lk) in ((6, lambda jj: avd[:, jj, g, :], "avd"), (7, lambda jj: ones_b, "ones_b")):
                    if has_prev:
                        OP("pe", lambda e, bk=bk, lhs_fn=lhs_fn: e.matmul(ps(bk), lhs_fn(j), pa_, start=True, stop=False), reads=[lk, pak], writes=[PK(bk)])
                    OP("pe", lambda e, bk=bk, lhs_fn=lhs_fn: e.matmul(ps(bk), lhs_fn(j + 1), pb_, start=(not has_prev), stop=True), reads=[lk, pbk], writes=[PK(bk)])
                def stage_b():
                    swa_out(j, g, rd, rdk, tsl, m3)
                return stage_b

            def swa_out(j, g, rd, rdk, tsl, m3):
                for r in range(4):
                    OP("act", lambda e, r=r: e.activation(rd[:, r, :], ps(7)[:, r * 128:(r + 1) * 128], AF.Ln, bias=esk_t[:, g * 4 + r:g * 4 + r + 1]), reads=[PK(7), "esk"], writes=[rdk])
                OP("act", lambda e: e.activation(rd, rd, AF.Exp, scale=-1.0), reads=[rdk], writes=[rdk])
                for par in range(2):
                    hp_ = slice(par * 64, par * 64 + 64)
                    OP("dve", lambda e, hp_=hp_, par=par: e.tensor_tensor(haT[hp_, 2 * g:2 * g + 2, tsl], m3(ps(6))[hp_, par::2, :], rd[hp_, par::2, :], op=ALU.mult),
                       reads=[PK(6), rdk], writes=["haT"])

            for j in range(4):
                import os as _os2
                _mm = _os2.environ.get("K_MIX", "both")
                pend_m, pend_s = None, None
                for idx in range(4):
                    mb = mlstm_unit(j, idx)
                    if pend_m:
                        pend_m()
                    if pend_s:
                        pend_s()
                    sb_ = swa_unit(j, idx)
                    pend_m, pend_s = mb, sb_
                pend_m()
                pend_s()
                mlstm_tile_end(j)
            if "d_hmT" in dbg:
                dbg_dump("d_hmT", hmT, ["hmT"], dst=dbg["d_hmT"][:, :, t0:t0 + TB])
            if "d_haT" in dbg:
                dbg_dump("d_haT", haT, ["haT"], dst=dbg["d_haT"][:, :, t0:t0 + TB])
            if stop in ("1c", "1d"):
                continue

            for pc in range(8):
                wgm, kgm = load_w(w_in[:, C_GM + pc * 256:C_GM + (pc + 1) * 256], 16, 256)
                wga, kga = load_w(w_in[:, C_GA + pc * 256:C_GA + (pc + 1) * 256], 16, 256)
                wpm, kpm = load_w(w_pm[:, pc * 256:(pc + 1) * 256], 8, 256)
                wpa, kpa = load_w(w_pa[:, pc * 256:(pc + 1) * 256], 8, 256)
                for c2 in range(2):
                    c = pc * 2 + c2
                    si = c % 2
                    bo = 4 * si
                    cs_ = slice(c2 * 128, (c2 + 1) * 128)
                    for (bk, wv_, wk_, src, skey, nk) in ((bo, wgm, kgm, hT, "hT", 16), (bo + 1, wga, kga, hT, "hT", 16),
                                                        (bo + 2, wpm, kpm, hmT, "hmT", 8), (bo + 3, wpa, kpa, haT, "haT", 8)):
                        for k in range(nk):
                            OP("pe", lambda e, bk=bk, wv_=wv_, k=k, cs_=cs_, src=src, nk=nk: e.matmul(ps(bk), wv_[:, k, cs_], src[:, k, :], start=(k == 0), stop=(k == nk - 1)),
                               reads=[wk_, skey], writes=[PK(bk)])
                    OP("act", lambda e, si=si, bo=bo: e.activation(sg_m[si], ps(bo), AF.Sigmoid), reads=[PK(bo)], writes=["PTa%d" % si])
                    OP("act", lambda e, si=si, bo=bo: e.activation(sg_a[si], ps(bo + 1), AF.Sigmoid), reads=[PK(bo + 1)], writes=["PTb%d" % si])
                    OP("dve", lambda e, si=si, bo=bo: e.tensor_tensor(tm1[si], sg_m[si], ps(bo + 2), op=ALU.mult), reads=["PTa%d" % si, PK(bo + 2)], writes=["acc%d" % si])
                    OP("dve", lambda e, si=si, bo=bo: e.tensor_tensor(tm2[si], sg_a[si], ps(bo + 3), op=ALU.mult), reads=["PTb%d" % si, PK(bo + 3)], writes=["zc%d" % si, "zc%dh" % si])
                    OP("dve", lambda e, si=si, c=c: e.tensor_tensor(mixT[:, c, :], tm1[si], tm2[si], op=ALU.add), reads=["acc%d" % si, "zc%d" % si], writes=["mqkT%d" % c])

            MXR = ["mqkT%d" % c for c in range(16)]
            for n8 in range(8):
                wo, wok = load_w(w_out[:, n8 * 256:(n8 + 1) * 256], 16, 256)
                for j in range(4):
                    tt = tb * 4 + j
                    b = next_bank(0, 8)
                    xi = (n8 * 4 + j) % 3
                    for k in range(16):
                        OP("pe", lambda e, b=b, k=k, j=j, wo=wo: e.matmul(ps(b)[:, 0:256], mixT[:, k, j * 128:(j + 1) * 128], wo[:, k, :], start=(k == 0), stop=(k == 15)),
                           reads=[wok] + MXR, writes=[PK(b)])
                    S.dma("sp", lambda e, xi=xi, tt=tt, n8=n8: e.dma_start(out=XP[xi], in_=x[tt * 128:(tt + 1) * 128, n8 * 256:(n8 + 1) * 256]), writes=["xp%d" % xi])
                    OP("dve", lambda e, xi=xi, b=b: e.tensor_tensor(XO[xi], ps(b)[:, 0:256], XP[xi], op=ALU.add), reads=[PK(b), "xp%d" % xi], writes=["xo%d" % xi])
                    tk = S.dma("sp", lambda e, xi=xi, tt=tt, n8=n8: e.dma_start(out=x1s[tt * 128:(tt + 1) * 128, n8 * 256:(n8 + 1) * 256], in_=XO[xi]), reads=["xo%d" % xi], writes=["x1s.%d.%d" % (tt, n8)])
                    if "d_x1" in dbg:
                        out_toks.append(tk)


        if stop in (None, "R", "E"):
            S.barrier()
            A.release(p1_mark)
            dest_i = A.alloc((NT, 2), I32)
            gate_t = A.alloc((NT, 2), F32)
            r_mark = A.mark()
            gffn_t = A.alloc((D,), F32)
            wr_t = A.alloc((16, 72), F32)
            br_t = A.alloc((72,), F32)
            eoff_t = A.alloc((64,), F32)
            cnt = A.alloc((64,), F32)
            ss2 = A.alloc((NT,), F32)
            rs2 = A.alloc((NT,), F32)
            X1T = [A.alloc((D,), F32) for _ in range(2)]
            HFB = [A.alloc((D,), BF16) for _ in range(2)]
            hfT = A.alloc((16, 128), F32)
            lg = A.alloc((72,), F32)
            sm = A.alloc((16,), F32)
            goh = A.alloc((8,), F32)
            gex = A.alloc((8,), F32)
            esel = A.alloc((8,), F32)
            esel2 = A.alloc((8,), F32)
            oh = [A.alloc((8,), F32) for _ in range(2)]
            t64 = A.alloc((64,), F32)
            Ek = [A.alloc((64,), F32) for _ in range(2)]
            Mb = A.alloc((64,), BF16)
            posb = A.alloc((64,), F32)
            dstf = A.alloc((2,), F32)
            S.dma("sp", lambda e: e.dma_start(out=gffn_t, in_=gffn), writes=["gffn"])
            S.dma("sp", lambda e: e.dma_start(out=wr_t, in_=wroute.rearrange("(k p) c -> p k c", p=128)), writes=["wr"])
            S.dma("sp", lambda e: e.dma_start(out=br_t, in_=broute), writes=["br"])
            S.dma("sp", lambda e: e.dma_start(out=eoff_t, in_=eoff), writes=["eoff"])
            OP("dve", lambda e: e.memset(cnt, 0.0), writes=["cnt"])
            OP("dve", lambda e: e.memset(ss2, 0.0), writes=["ss2"])
            g3 = lambda t: t.rearrange("p (g e) -> p g e", e=8)
            scat_keys = []
            for tt in range(NT):
                xt, xk = X1T[tt % 2], "x1t%d" % (tt % 2)
                hfb, hk = HFB[tt % 2], "hfb%d" % (tt % 2)
                S.dma("sp", lambda e, xt=xt, tt=tt: e.dma_start(out=xt, in_=x1s[tt * 128:(tt + 1) * 128, :]), reads=["x1s.%d.%d" % (tt, n8) for n8 in range(8)], writes=[xk])
                OP("act", lambda e, xt=xt, tt=tt, hfb=hfb: e.activation(hfb, xt, AF.Square, accum_out=ss2[:, tt:tt + 1]), reads=[xk, "ss2"], writes=[hk, "ss2_%d" % tt])
                OP("act", lambda e, tt=tt: e.activation(rs2[:, tt:tt + 1], ss2[:, tt:tt + 1], AF.Ln, scale=1.0 / D, bias=EPS), reads=["ss2_%d" % tt], writes=["rs2_%d" % tt])
                OP("act", lambda e, tt=tt: e.activation(rs2[:, tt:tt + 1], rs2[:, tt:tt + 1], AF.Exp, scale=-0.5), reads=["rs2_%d" % tt], writes=["rs2_%d" % tt])
                OP("dve", lambda e, xt=xt, tt=tt: e.scalar_tensor_tensor(xt, xt, rs2[:, tt:tt + 1], gffn_t, op0=ALU.mult, op1=ALU.mult), reads=[xk, "rs2_%d" % tt, "gffn"], writes=[xk])
                OP("act", lambda e, xt=xt, hfb=hfb: e.activation(hfb, xt, AF.Copy), reads=[xk], writes=[hk])
                for q4 in range(4):
                    for kk in range(4):
                        k = q4 * 4 + kk
                        OP("pe", lambda e, q4=q4, kk=kk, k=k, xt=xt: e.transpose(ps(q4)[:, kk * 128:(kk + 1) * 128], xt[:, k * 128:(k + 1) * 128], ident_f), reads=[xk, "cm_f"], writes=[PK(q4)])
                    eng = "act" if q4 % 2 == 0 else "dve"
                    if eng == "act":
                        OP("act", lambda e, q4=q4: e.activation(hfT[:, q4 * 4:(q4 + 1) * 4, :], ps(q4).rearrange("p (k t) -> p k t", t=128), AF.Copy), reads=[PK(q4)], writes=["hfT%d" % q4])
                    else:
                        OP("dve", lambda e, q4=q4: e.tensor_copy(hfT[:, q4 * 4:(q4 + 1) * 4, :], ps(q4).rearrange("p (k t) -> p k t", t=128)), reads=[PK(q4)], writes=["hfT%d" % q4])
                for k in range(16):
                    OP("pe", lambda e, k=k: e.matmul(ps(4)[:, 0:72], hfT[:, k, :], wr_t[:, k, :], start=(k == 0), stop=(k == 15)), reads=["hfT%d" % (k // 4), "wr"], writes=[PK(4)])
                OP("dve", lambda e: e.tensor_tensor(lg, ps(4)[:, 0:72], br_t, op=ALU.add), reads=[PK(4), "br"], writes=["lg"])
                OP("dve", lambda e: e.reduce_max(sm[:, 0:1], lg[:, 0:8], axis=AX.X), reads=["lg"], writes=["sm0"])
                OP("dve", lambda e: e.tensor_scalar(goh, lg[:, 0:8], sm[:, 0:1], None, op0=ALU.is_equal), reads=["lg", "sm0"], writes=["goh"])
                OP("dve", lambda e: e.tensor_scalar(sm[:, 1:2], sm[:, 0:1], -1.0, None, op0=ALU.mult), reads=["sm0"], writes=["sm1"])
                OP("dve", lambda e: e.memset(sm[:, 2:3], 0.0), writes=["sm2"])
                OP("act", lambda e: e.activation(gex, lg[:, 0:8], AF.Exp, bias=sm[:, 1:2], accum_out=sm[:, 2:3]), reads=["lg", "sm1", "sm2"], writes=["gex", "sm2"])
                OP("dve", lambda e: e.reciprocal(sm[:, 3:4], sm[:, 2:3]), reads=["sm2"], writes=["sm3"])
                OP("dve", lambda e: e.tensor_tensor(g3(t64), g3(lg[:, 8:72]), goh.unsqueeze(2).to_broadcast([128, 8, 8]), op=ALU.mult), reads=["lg", "goh"], writes=["t64"])
                OP("dve", lambda e: e.reduce_sum(esel, t64.rearrange("p (g e) -> p e g", e=8), axis=AX.X), reads=["t64"], writes=["esel"])
                OP("dve", lambda e: e.reduce_max(sm[:, 4:5], esel, axis=AX.X), reads=["esel"], writes=["sm4"])
                OP("dve", lambda e: e.tensor_scalar(oh[0], esel, sm[:, 4:5], None, op0=ALU.is_equal), reads=["esel", "sm4"], writes=["oh0"])
                OP("dve", lambda e: e.scalar_tensor_tensor(esel2, oh[0], -1e30, esel, op0=ALU.mult, op1=ALU.add), reads=["oh0", "esel"], writes=["esel2"])
                OP("dve", lambda e: e.reduce_max(sm[:, 5:6], esel2, axis=AX.X), reads=["esel2"], writes=["sm5"])
                OP("dve", lambda e: e.tensor_scalar(oh[1], esel2, sm[:, 5:6], None, op0=ALU.is_equal), reads=["esel2", "sm5"], writes=["oh1"])
                OP("dve", lambda e: e.tensor_tensor(sm[:, 6:7], sm[:, 4:5], sm[:, 5:6], op=ALU.subtract), reads=["sm4", "sm5"], writes=["sm6"])
                OP("act", lambda e: e.activation(sm[:, 7:8], sm[:, 6:7], AF.Sigmoid), reads=["sm6"], writes=["sm7"])
                OP("dve", lambda e: e.tensor_scalar(sm[:, 8:9], sm[:, 7:8], -1.0, 1.0, op0=ALU.mult, op1=ALU.add), reads=["sm7"], writes=["sm8"])
                OP("dve", lambda e, tt=tt: e.tensor_tensor(gate_t[:, tt, 0:1], sm[:, 7:8], sm[:, 3:4], op=ALU.mult), reads=["sm7", "sm3"], writes=["gate%d" % tt])
                OP("dve", lambda e, tt=tt: e.tensor_tensor(gate_t[:, tt, 1:2], sm[:, 8:9], sm[:, 3:4], op=ALU.mult), reads=["sm8", "sm3", "gate%d" % tt], writes=["gate%d" % tt])
                for kk in range(2):
                    OP("dve", lambda e, kk=kk: e.tensor_tensor(g3(Ek[kk]), goh.unsqueeze(2).to_broadcast([128, 8, 8]), oh[kk].unsqueeze(1).to_broadcast([128, 8, 8]), op=ALU.mult),
                       reads=["goh", "oh%d" % kk], writes=["Ek%d" % kk])
                OP("dve", lambda e: e.tensor_tensor(Mb, Ek[0], Ek[1], op=ALU.add), reads=["Ek0", "Ek1"], writes=["Mb"])
                OP("pe", lambda e: e.matmul(ps(5)[:, 0:64], stri_b, Mb, start=True, stop=True), reads=["Mb", "cm_b"], writes=[PK(5)])
                OP("pe", lambda e: e.matmul(ps(5)[:, 64:128], ones_b, Mb, start=True, stop=True), reads=["Mb", "ones_b"], writes=[PK(5)])
                OP("dve", lambda e: e.tensor_tensor(posb, ps(5)[:, 0:64], cnt, op=ALU.add), reads=[PK(5), "cnt"], writes=["posb"])
                OP("dve", lambda e: e.tensor_tensor(posb, posb, eoff_t, op=ALU.add), reads=["posb", "eoff"], writes=["posb"])
                OP("dve", lambda e: e.tensor_tensor(cnt, cnt, ps(5)[:, 64:128], op=ALU.add), reads=["cnt", PK(5), "posb"], writes=["cnt"])
                for kk in range(2):
                    OP("dve", lambda e, kk=kk: e.tensor_tensor(t64, Ek[kk], posb, op=ALU.mult), reads=["Ek%d" % kk, "posb"], writes=["t64"])
                    OP("dve", lambda e, kk=kk: e.reduce_sum(dstf[:, kk:kk + 1], t64, axis=AX.X), reads=["t64"], writes=["dstf%d" % kk])
                    OP("dve", lambda e, kk=kk: e.tensor_scalar(dstf[:, kk:kk + 1], dstf[:, kk:kk + 1], float(N_EXP * 128 - 1), None, op0=ALU.min), reads=["dstf%d" % kk], writes=["dstf%d" % kk])
                    OP("dve", lambda e, kk=kk, tt=tt: e.tensor_copy(dest_i[:, tt, kk:kk + 1], dstf[:, kk:kk + 1]), reads=["dstf%d" % kk], writes=["dest%d_%d" % (tt, kk)])
                    sk = "xr.s%d_%d" % (tt, kk)
                    S.dma("pool", lambda e, kk=kk, tt=tt, hfb=hfb: e.indirect_dma_start(out=xr, out_offset=bass.IndirectOffsetOnAxis(ap=dest_i[:, tt, kk:kk + 1], axis=0), in_=hfb, in_offset=None),
                          reads=[hk, "dest%d_%d" % (tt, kk)] + (XR_Z if (tt == 0 and kk == 0) else []), writes=[sk] + (XR_Z if (tt == 0 and kk == 0) else []))
                    scat_keys.append(sk)
            if "d_dest" in dbg:
                dbg_dump("d_dest", dest_i, ["dest%d_%d" % (tt, kk) for tt in range(NT) for kk in range(2)])
                dbg_dump("d_gate", gate_t, ["gate%d" % tt for tt in range(NT)])

            if stop in (None, "E"):
                S.barrier()
                A.release(r_mark)
                WGU = [A.alloc((16, 1536), BF16) for _ in range(2)]
                WD = [A.alloc((6, D), BF16) for _ in range(2)]
                XE = [A.alloc((D,), BF16) for _ in range(2)]
                XET = [A.alloc((16, 128), BF16) for _ in range(1)]
                NSTG = 4
                STG = [A.alloc((2, D_EXP), F32) for _ in range(NSTG)]
                stg_i = {"i": 0}
                sg_e = A.alloc((D_EXP,), BF16)
                up_e = A.alloc((D_EXP,), BF16)
                hmid = A.alloc((D_EXP,), BF16)
                hT6 = A.alloc((6, 128), BF16)
                YE = [A.alloc((D,), BF16) for _ in range(2)]
                import os as _os
                ESTOP = float(_os.environ.get("K_ESTOP", 9))
            NEXP_RUN = int(_os.environ.get("K_NEXP", N_EXP))

            def load_expert(ex):
                    pi = ex % 2
                    wgu, wd, xe = WGU[pi], WD[pi], XE[pi]
                    S.dma("pool", lambda e: e.dma_start(out=wgu[:, :, 0:768], in_=w_gate[ex].rearrange("(k p) f -> p k f", p=128)), writes=["wg%d" % pi])
                    S.dma("pool", lambda e: e.dma_start(out=wd, in_=w_down[ex].rearrange("(k p) d -> p k d", p=128)), writes=["wd%d" % pi])
                    for kp in range(8):
                        si_ = stg_i["i"]
                        stg_i["i"] = (si_ + 1) % NSTG
                        S.dma("sp", lambda e, kp=kp, si_=si_: e.dma_start(out=STG[si_], in_=w_up[ex, kp * 256:(kp + 1) * 256, :].rearrange("(k p) f -> p k f", p=128)), writes=["stg%d" % si_])
                        OP("dve", lambda e, kp=kp, si_=si_: e.tensor_copy(wgu[:, 2 * kp:2 * kp + 2, 768:1536], STG[si_]), reads=["stg%d" % si_], writes=["wu%d_%d" % (pi, kp)])
                    S.dma("sp", lambda e: e.dma_start(out=xe, in_=xr[ex * 128:(ex + 1) * 128, :]), reads=(scat_keys if ex == 0 else []) + XR_Z, writes=["xe%d" % pi])

            load_expert(0)
            for ex in range(NEXP_RUN):
                    if ex + 1 < NEXP_RUN:
                        load_expert(ex + 1)
                    pi = ex % 2
                    wgu, wd, xe, xet, ye = WGU[pi], WD[pi], XE[pi], XET[0], YE[pi]
                    for half in range(2):
                        for kk in range(8):
                            k = half * 8 + kk
                            OP("pe", lambda e, half=half, kk=kk, k=k, xe=xe: e.transpose(psb(half)[:, kk * 128:(kk + 1) * 128], xe[:, k * 128:(k + 1) * 128], ident_b), reads=["xe%d" % pi, "cm_b"], writes=[PK(half)])
                        OP("act", lambda e, xet=xet, half=half: e.activation(xet[:, half * 8:(half + 1) * 8, :], psb(half).rearrange("p (k t) -> p k t", t=128), AF.Copy), reads=[PK(half)], writes=["xet0_%d" % half])
                    for nb in range(3):
                        for k in range(16):
                            wkeys = [["wg%d" % pi], ["wg%d" % pi, "wu%d_%d" % (pi, k // 2)], ["wu%d_%d" % (pi, k // 2)]][nb]
                            OP("pe", lambda e, nb=nb, k=k, xet=xet, wgu=wgu: e.matmul(ps(2 + nb), xet[:, k, :], wgu[:, k, nb * 512:(nb + 1) * 512], start=(k == 0), stop=(k == 15)),
                               reads=["xet0_%d" % (k // 8)] + wkeys, writes=[PK(2 + nb)])
                    OP("act", lambda e: e.activation(sg_e[:, 0:512], ps(2), AF.Silu), reads=[PK(2)], writes=["sg_e0"])
                    OP("act", lambda e: e.activation(sg_e[:, 512:768], ps(3)[:, 0:256], AF.Silu), reads=[PK(3)], writes=["sg_e1"])
                    OP("act", lambda e: e.activation(up_e[:, 0:256], ps(3)[:, 256:512], AF.Copy), reads=[PK(3)], writes=["up_e0"])
                    OP("act", lambda e: e.activation(up_e[:, 256:768], ps(4), AF.Copy), reads=[PK(4)], writes=["up_e1"])
                    OP("pool", lambda e: e.tensor_tensor(hmid, sg_e, up_e, op=ALU.mult), reads=["sg_e0", "sg_e1", "up_e0", "up_e1"], writes=["hmid"])
                    for kf in range(6):
                        OP("pe", lambda e, kf=kf: e.transpose(psb(5)[:, kf * 128:(kf + 1) * 128], hmid[:, kf * 128:(kf + 1) * 128], ident_b), reads=["hmid", "cm_b"], writes=[PK(5)])
                    OP("act", lambda e: e.activation(hT6, psb(5)[:, 0:768].rearrange("p (k t) -> p k t", t=128), AF.Copy), reads=[PK(5)], writes=["hT6"])
                    for cb in range(4):
                        bk = 6 + cb % 2
                        for kf in range(6):
                            OP("pe", lambda e, bk=bk, kf=kf, cb=cb, wd=wd: e.matmul(ps(bk), hT6[:, kf, :], wd[:, kf, cb * 512:(cb + 1) * 512], start=(kf == 0), stop=(kf == 5)),
                               reads=["hT6", "wd%d" % pi], writes=[PK(bk)])
                        OP("act", lambda e, bk=bk, cb=cb, ye=ye: e.activation(ye[:, cb * 512:(cb + 1) * 512], ps(bk), AF.Copy), reads=[PK(bk)], writes=["ye%d_%d" % (pi, cb)])
                    S.dma("sp", lambda e, ye=ye, ex=ex: e.dma_start(out=yr[ex * 128:(ex + 1) * 128, :], in_=ye), reads=["ye%d_%d" % (pi, cb) for cb in range(4)], writes=["yr%d" % ex])

            if stop is None:
                S.barrier()
                A.release(r_mark)
                Y1 = [A.alloc((D,), BF16) for _ in range(3)]
                Y2 = [A.alloc((D,), BF16) for _ in range(3)]
                XC = [A.alloc((D,), F32) for _ in range(3)]
                YRK = ["yr%d" % ex for ex in range(N_EXP)]
                for tt in range(NT):
                    pi = tt % 3
                    S.dma("pool", lambda e, tt=tt, pi=pi: e.indirect_dma_start(out=Y1[pi], out_offset=None, in_=yr, in_offset=bass.IndirectOffsetOnAxis(ap=dest_i[:, tt, 0:1], axis=0)),
                          reads=(YRK if tt == 0 else []) + ["dest%d_0" % tt], writes=["y1_%d" % pi])
                    S.dma("pool", lambda e, tt=tt, pi=pi: e.indirect_dma_start(out=Y2[pi], out_offset=None, in_=yr, in_offset=bass.IndirectOffsetOnAxis(ap=dest_i[:, tt, 1:2], axis=0)),
                          reads=["dest%d_1" % tt], writes=["y2_%d" % pi])
                    S.dma("sp", lambda e, tt=tt, pi=pi: e.dma_start(out=XC[pi], in_=x1s[tt * 128:(tt + 1) * 128, :]), writes=["xc%d" % pi])
                    OP("dve", lambda e, tt=tt, pi=pi: e.scalar_tensor_tensor(XC[pi], Y1[pi], gate_t[:, tt, 0:1], XC[pi], op0=ALU.mult, op1=ALU.add), reads=["y1_%d" % pi, "xc%d" % pi, "gate%d" % tt], writes=["xc%d" % pi])
                    OP("dve", lambda e, tt=tt, pi=pi: e.scalar_tensor_tensor(XC[pi], Y2[pi], gate_t[:, tt, 1:2], XC[pi], op0=ALU.mult, op1=ALU.add), reads=["y2_%d" % pi, "xc%d" % pi, "gate%d" % tt], writes=["xc%d" % pi])
                    out_toks.append(S.dma("sp", lambda e, tt=tt, pi=pi: e.dma_start(out=out[tt * 128:(tt + 1) * 128, :], in_=XC[pi]), reads=["xc%d" % pi], writes=["out%d" % tt]))
        if not out_toks:
            pass
        S.barrier(["sp"])
        S.emit()
        print("kernel build: instrs~%d arena_peak=%d signalling=%s of %s" % (S.ninst, A.peak, S.nsig, S.cnt))
    return nc


def _consts():
    p = np.arange(128)
    ident = np.eye(128, dtype=np.float32)
    tri = (p[:, None] <= p[None, :]).astype(np.float32)
    stri = (p[:, None] < p[None, :]).astype(np.float32)
    sw = np.where((p % 64) < 32, p + 32, p - 32)
    swap = np.zeros((128, 128), np.float32)
    swap[sw, p] = 1.0
    bd = ((p[:, None] // 64) == (p[None, :] // 64)).astype(np.float32)
    gt = (p[:, None] > p[None, :]).astype(np.float32)
    cmat = np.ascontiguousarray(np.stack([ident, tri, stri, swap, bd, gt], axis=1))
    half = 32
    freqs = (10000.0 ** (-np.arange(half, dtype=np.float32) / half)).astype(np.float32)
    pos = np.arange(S_LEN, dtype=np.float32)
    ang = (pos[None, :] * freqs[p % 32][:, None]).astype(np.float32)
    cos = np.cos(ang).astype(np.float32)
    sin = np.sin(ang).astype(np.float32) * np.where((p % 64) < 32, -1.0, 1.0)[:, None].astype(np.float32)
    ctab = np.ascontiguousarray(np.stack([cos, sin], axis=1)).astype(np.float32)
    eoff = np.ascontiguousarray(np.broadcast_to((np.arange(64, dtype=np.float32) * 128.0)[None, :], (128, 64)))
    return cmat, ctab, eoff


def make_in_maps(inputs, cores, moe=True):
    f = lambda k: np.ascontiguousarray(np.asarray(inputs[k], dtype=np.float32)[0])
    rep = lambda v: np.ascontiguousarray(np.broadcast_to(v.reshape(1, -1), (128, v.size)))
    cmat, ctab, eoff = _consts()
    conv = f("conv_qk")
    convw = np.ascontiguousarray(conv.T.reshape(16, 128, 4).transpose(1, 0, 2).reshape(128, 64))
    gmixT = np.ascontiguousarray(f("g_mix").reshape(16, 128).T)
    gbias = rep(np.concatenate([f("b_igate"), f("b_fgate")]))
    gml = rep(f("g_mlstm").reshape(-1))
    gqk = np.ascontiguousarray(np.stack([np.tile(f("g_q"), 2), np.tile(f("g_k"), 2)], axis=1))
    sinks = rep(f("sinks"))
    gffn = rep(f("g_ffn"))
    wroute = np.ascontiguousarray(np.concatenate([f("w_group"), f("w_expert")], axis=1))
    broute = rep(np.concatenate([f("b_group"), f("b_expert")]))
    shared = dict(w_in=f("w_in"), convw=convw, gmixT=gmixT, gbias=gbias, gml=gml, gqk=gqk, sinks=sinks,
                  w_proj_m=f("w_proj_m"), w_proj_a=f("w_proj_a"), w_out=f("w_out"), gffn=gffn,
                  wroute=wroute, broute=broute, cmat=cmat, ctab=ctab, eoff=eoff)
    if moe:
        shared.update(w_gate=f("w_gate"), w_up=f("w_up"), w_down=f("w_down"))
    xs = np.asarray(inputs["x"], dtype=np.float32)
    return [dict(shared, x=np.ascontiguousarray(xs[c])) for c in cores]


def kernel(**inputs):
    nc = build_nc()
    in_maps = make_in_maps(inputs, list(range(8)))
    res = run_bass_kernel_spmd(nc, in_maps, core_ids=list(range(8)))
    return np.stack([np.asarray(r["out"]) for r in res.results], axis=0).astype(np.float32)
```
